# Optimizing a Trainium2 kernel written in Bass

```python
import math
import jax
import jax.numpy as jnp
from jax import lax
import numpy as np

D_MODEL = 2048
BATCH = 2
SEQ = 4096
DEPTH = 4

GRID_W = 64
CTX_LEN = 256
EPS = 1e-6

GDN_HEADS = 6
GDN_DK = 128
GDN_DV = 128
GDN_W = GDN_HEADS * GDN_DV
GDN_CHUNK = 64
QKV_CONV = 3
SC_GROUPS = 4
SC_GROUP_DIM = 128
SC_W = SC_GROUPS * SC_GROUP_DIM
SC_CONV = 3
DIFF_HEADS = 6
DIFF_DH = 64
DIFF_DV = 2 * DIFF_DH
DIFF_W = DIFF_HEADS * DIFF_DV
Q_BLOCK = 128
ROPE_BASE = 10000.0
ROPE_PAIRS = DIFF_DH // 4

MIX_W = GDN_W + SC_W + DIFF_W
A_IN = 4 * GDN_W + 4 * GDN_HEADS
B_IN = 3 * SC_W
C_IN = 3 * DIFF_W
N_IN = A_IN + B_IN + C_IN

N_EXPERTS = 16
EXPERT_FF = 1024
EC_CAPACITY = 2

kernel_name = 'hybrid_gdn_shortconv_diffattn_ecmoe_dit'


def rmsnorm(x, g):
    xf = x.astype(jnp.float32)
    y = xf * lax.rsqrt(jnp.mean(xf * xf, -1, keepdims=True) + EPS)
    return (y * g.astype(jnp.float32)).astype(x.dtype)


def l2norm(x):
    xf = x.astype(jnp.float32)
    return xf * lax.rsqrt(jnp.sum(xf * xf, -1, keepdims=True) + EPS)


def modulate(h, shift, scale):
    return h * (1 + scale) + shift


def depthwise_conv(x, w):
    k = w.shape[0]
    return lax.conv_general_dilated(
        x, w[:, None, :].astype(x.dtype), window_strides=(1,),
        padding=[((k - 1) // 2, (k - 1) // 2)],
        dimension_numbers=('NWC', 'WIO', 'NWC'), feature_group_count=x.shape[-1])


def axial_rope_tables(t):
    rows = t // GRID_W
    row = jnp.repeat(jnp.arange(rows, dtype=jnp.float32), GRID_W)
    col = jnp.tile(jnp.arange(GRID_W, dtype=jnp.float32), rows)
    inv = ROPE_BASE ** (-jnp.arange(ROPE_PAIRS, dtype=jnp.float32) / ROPE_PAIRS)
    ang = jnp.stack([row[:, None] * inv, col[:, None] * inv], 1)
    return jnp.cos(ang), jnp.sin(ang)


def axial_rope(x, cos, sin):
    shp = x.shape
    xr = x.astype(jnp.float32).reshape(shp[:-1] + (2, 2, ROPE_PAIRS))
    x1, x2 = xr[..., 0, :], xr[..., 1, :]
    c = cos[None, :, None, None]
    s = sin[None, :, None, None]
    y = jnp.stack([x1 * c - x2 * s, x2 * c + x1 * s], -2)
    return y.reshape(shp).astype(x.dtype)


def gdn_chunked(q, k, v, g, beta, s0):
    bsz, t = q.shape[:2]
    n = t // GDN_CHUNK
    dv = v.shape[-1]

    def to_chunks(a):
        return jnp.moveaxis(a.reshape((bsz, n, GDN_CHUNK) + a.shape[2:]), 3, 1)

    q, k, v, g, beta = (to_chunks(a) for a in (q, k, v, g, beta))
    gc = jnp.cumsum(g, -1)
    idx = jnp.arange(GDN_CHUNK)
    incl = idx[:, None] >= idx[None, :]
    strict = idx[:, None] > idx[None, :]
    decay = jnp.exp(jnp.where(incl, gc[..., :, None] - gc[..., None, :], -jnp.inf))
    kb = k * beta[..., None]
    low = jnp.where(strict, jnp.einsum('bhnid,bhnjd->bhnij', kb, k) * decay, 0.0)
    a_mat = low + jnp.eye(GDN_CHUNK, dtype=low.dtype)
    rhs = jnp.concatenate([v * beta[..., None], kb * jnp.exp(gc)[..., None]], -1)
    sol = lax.linalg.triangular_solve(a_mat, rhs, left_side=True, lower=True, unit_diagonal=True)
    u, w = sol[..., :dv], sol[..., dv:]
    attn = jnp.einsum('bhnid,bhnjd->bhnij', q, k) * decay
    q_dec = q * jnp.exp(gc)[..., None]
    k_dec = k * jnp.exp(gc[..., -1:] - gc)[..., None]
    g_last = jnp.exp(gc[..., -1])
    xs = tuple(jnp.moveaxis(a, 2, 0) for a in (u, w, attn, q_dec, k_dec, g_last))

    def step(s, inp):
        u_n, w_n, a_n, qd_n, kd_n, gl_n = inp
        v_new = u_n - jnp.einsum('bhcd,bhde->bhce', w_n, s)
        o = jnp.einsum('bhcd,bhde->bhce', qd_n, s) + jnp.einsum('bhij,bhje->bhie', a_n, v_new)
        s = s * gl_n[..., None, None] + jnp.einsum('bhcd,bhce->bhde', kd_n, v_new)
        return s, o

    s_final, o = lax.scan(step, s0, xs)
    o = jnp.moveaxis(jnp.moveaxis(o, 0, 2), 1, 3).reshape(bsz, t, GDN_HEADS, dv)
    return o, s_final


def gdn_mixer(uc, ux, conv_w, a_log, dt_bias, norm_g, with_ctx):
    def prep(u):
        bsz, t = u.shape[:2]
        qkv, z, b, a = jnp.split(u, [3 * GDN_W, 4 * GDN_W, 4 * GDN_W + 2 * GDN_HEADS], -1)
        qkv = jax.nn.silu(depthwise_conv(qkv, conv_w))
        q, k, v = (m.reshape(bsz, t, GDN_HEADS, -1) for m in jnp.split(qkv, 3, -1))
        q = l2norm(q) * GDN_DK ** -0.5
        k = l2norm(k)
        v = v.astype(jnp.float32)
        beta = jax.nn.sigmoid(b.astype(jnp.float32)).reshape(bsz, t, 2, GDN_HEADS)
        g = -jnp.exp(a_log.astype(jnp.float32)) * jax.nn.softplus(
            a.astype(jnp.float32).reshape(bsz, t, 2, GDN_HEADS) + dt_bias.astype(jnp.float32))
        return q, k, v, g, beta, z

    qc, kc, vc, gcx, bc, zc = prep(uc)
    qx, kx, vx, gx, bx, zx = prep(ux)
    s0 = jnp.zeros((ux.shape[0], GDN_HEADS, GDN_DK, GDN_DV), jnp.float32)
    oc = 0.0
    ox = 0.0
    for d in range(2):
        def rev(a):
            return jnp.flip(a, 1) if d == 1 else a
        o_c, s_ctx = gdn_chunked(rev(qc), rev(kc), rev(vc), rev(gcx[:, :, d]), rev(bc[:, :, d]), s0)
        o_x, _ = gdn_chunked(rev(qx), rev(kx), rev(vx), rev(gx[:, :, d]), rev(bx[:, :, d]), s_ctx)
        oc = oc + rev(o_c)
        ox = ox + rev(o_x)

    def out(o, z):
        bsz, t = z.shape[:2]
        y = rmsnorm(o, norm_g) * jax.nn.silu(z.astype(jnp.float32)).reshape(bsz, t, GDN_HEADS, GDN_DV)
        return y.reshape(bsz, t, GDN_W).astype(z.dtype)

    return (out(oc, zc) if with_ctx else None), out(ox, zx)


def short_conv_mixer(u, conv_w):
    b, cg, xin = jnp.split(u, 3, -1)
    return b * depthwise_conv(cg * xin, conv_w)


def diff_attend(q, k, v, lam):
    s = jnp.einsum('bqhmd,bkhmd->bhmqk', q, k).astype(jnp.float32) * DIFF_DH ** -0.5
    p = jax.nn.softmax(s, -1)
    wts = p[:, :, 0] - lam * p[:, :, 1]
    return jnp.einsum('bhqk,bkhd->bqhd', wts.astype(v.dtype), v)


def diff_attn_mixer(uc, ux, lam_params, norm_g, lam_init, cos, sin, with_ctx):
    def heads(u):
        bsz, t = u.shape[:2]
        q, k, v = jnp.split(u, 3, -1)
        return (q.reshape(bsz, t, DIFF_HEADS, 2, DIFF_DH), k.reshape(bsz, t, DIFF_HEADS, 2, DIFF_DH),
                v.reshape(bsz, t, DIFF_HEADS, DIFF_DV))

    qc, kc, vc = heads(uc)
    qx, kx, vx = heads(ux)
    qx = axial_rope(qx, cos, sin)
    kx = axial_rope(kx, cos, sin)
    lp = lam_params.astype(jnp.float32)
    lam = jnp.exp(jnp.sum(lp[0] * lp[1])) - jnp.exp(jnp.sum(lp[2] * lp[3])) + lam_init
    k_all = jnp.concatenate([kc, kx], 1)
    v_all = jnp.concatenate([vc, vx], 1)
    bsz, t = ux.shape[:2]
    nb = t // Q_BLOCK
    q_blocks = jnp.swapaxes(qx.reshape(bsz, nb, Q_BLOCK, DIFF_HEADS, 2, DIFF_DH), 0, 1)
    o_blocks = lax.map(lambda qb: diff_attend(qb, k_all, v_all, lam), q_blocks)
    ox = jnp.swapaxes(o_blocks, 0, 1).reshape(bsz, t, DIFF_HEADS, DIFF_DV)

    def out(o):
        return (rmsnorm(o, norm_g) * (1.0 - lam_init)).reshape(o.shape[0], o.shape[1], DIFF_W)

    oc = out(diff_attend(qc, kc, vc, lam)) if with_ctx else None
    return oc, out(ox)


def expert_choice_ffn(h, w_router, w_gate, w_up, w_down):
    bsz, t, d = h.shape
    cap = EC_CAPACITY * t // N_EXPERTS
    aff = jax.nn.softmax((h @ w_router).astype(jnp.float32), -1)
    gate, idx = lax.top_k(jnp.swapaxes(aff, 1, 2), cap)
    xs = jax.vmap(lambda hb, ib: hb[ib])(h, idx)
    hid = jax.nn.silu(jnp.einsum('becd,edf->becf', xs, w_gate)) * jnp.einsum('becd,edf->becf', xs, w_up)
    y = jnp.einsum('becf,efd->becd', hid, w_down) * gate[..., None].astype(h.dtype)
    return jax.vmap(lambda ib, yb: jnp.zeros((t, d), yb.dtype).at[ib.reshape(-1)].add(yb.reshape(-1, d)))(idx, y)


def setup_inputs(seed: int = 0) -> dict:
    key = jax.random.key(seed)
    ks = jax.random.split(key, 24)
    f32 = jnp.float32

    def nrm(k, shape, scale):
        return jax.random.normal(k, shape, f32) * scale

    dt = jnp.exp(jax.random.uniform(ks[11], (DEPTH, 2, GDN_HEADS), f32, math.log(1e-3), math.log(1e-1)))
    return {
        'x': nrm(ks[0], (BATCH, SEQ, D_MODEL), 1.0),
        'c': nrm(ks[1], (BATCH, D_MODEL), 1.0),
        'ctx': nrm(ks[2], (BATCH, CTX_LEN, D_MODEL), 1.0),
        'c_ctx': nrm(ks[3], (D_MODEL,), 1.0),
        'w_mod': nrm(ks[4], (DEPTH, D_MODEL, 6 * D_MODEL), 0.5 * D_MODEL ** -0.5),
        'b_mod': nrm(ks[5], (DEPTH, 6 * D_MODEL), 0.02),
        'norm1_g': 1.0 + nrm(ks[6], (DEPTH, D_MODEL), 0.02),
        'norm2_g': 1.0 + nrm(ks[7], (DEPTH, D_MODEL), 0.02),
        'w_in': nrm(ks[8], (DEPTH, D_MODEL, N_IN), D_MODEL ** -0.5),
        'gdn_conv_w': nrm(ks[9], (DEPTH, QKV_CONV, 3 * GDN_W), QKV_CONV ** -0.5),
        'gdn_a_log': jnp.log(jax.random.uniform(ks[10], (DEPTH, 2, GDN_HEADS), f32, 1.0, 16.0)),
        'gdn_dt_bias': dt + jnp.log(-jnp.expm1(-dt)),
        'gdn_norm_g': 1.0 + nrm(ks[12], (DEPTH, GDN_DV), 0.02),
        'sc_conv_w': nrm(ks[13], (DEPTH, SC_CONV, SC_W), SC_CONV ** -0.5),
        'diff_lambda': nrm(ks[14], (DEPTH, 4, DIFF_DH), 0.1),
        'diff_norm_g': 1.0 + nrm(ks[15], (DEPTH, DIFF_DV), 0.02),
        'w_out': nrm(ks[16], (DEPTH, MIX_W, D_MODEL), MIX_W ** -0.5),
        'w_router': nrm(ks[17], (DEPTH, D_MODEL, N_EXPERTS), D_MODEL ** -0.5),
        'w_e_gate': nrm(ks[18], (DEPTH, N_EXPERTS, D_MODEL, EXPERT_FF), D_MODEL ** -0.5),
        'w_e_up': nrm(ks[19], (DEPTH, N_EXPERTS, D_MODEL, EXPERT_FF), D_MODEL ** -0.5),
        'w_e_down': nrm(ks[20], (DEPTH, N_EXPERTS, EXPERT_FF, D_MODEL), EXPERT_FF ** -0.5),
        'final_norm_g': 1.0 + nrm(ks[21], (D_MODEL,), 0.02),
    }


def reference(x, c, ctx, c_ctx, w_mod, b_mod, norm1_g, norm2_g, w_in, gdn_conv_w, gdn_a_log,
              gdn_dt_bias, gdn_norm_g, sc_conv_w, diff_lambda, diff_norm_g, w_out, w_router,
              w_e_gate, w_e_up, w_e_down, final_norm_g):
    t = x.shape[1]
    tc = ctx.shape[1]
    cos, sin = axial_rope_tables(t)
    sc_x = jax.nn.silu(c)
    sc_c = jax.nn.silu(c_ctx)
    hx, hc = x, ctx
    for l in range(DEPTH):
        with_ctx = l < DEPTH - 1
        lam_init = 0.8 - 0.6 * math.exp(-0.3 * l)
        mx = jnp.split((sc_x @ w_mod[l] + b_mod[l])[:, None, :], 6, -1)
        mc = jnp.split(sc_c @ w_mod[l] + b_mod[l], 6, -1)
        ax = modulate(rmsnorm(hx, norm1_g[l]), mx[0], mx[1])
        ac = modulate(rmsnorm(hc, norm1_g[l]), mc[0], mc[1])
        u = jnp.concatenate([ac, ax], 1) @ w_in[l]
        uc, ux = u[:, :tc], u[:, tc:]
        uc_a, uc_b, uc_c = jnp.split(uc, [A_IN, A_IN + B_IN], -1)
        ux_a, ux_b, ux_c = jnp.split(ux, [A_IN, A_IN + B_IN], -1)
        oc_a, ox_a = gdn_mixer(uc_a, ux_a, gdn_conv_w[l], gdn_a_log[l], gdn_dt_bias[l], gdn_norm_g[l], with_ctx)
        ox_b = short_conv_mixer(ux_b, sc_conv_w[l])
        oc_c, ox_c = diff_attn_mixer(uc_c, ux_c, diff_lambda[l], diff_norm_g[l], lam_init, cos, sin, with_ctx)
        hx = hx + mx[2] * (jnp.concatenate([ox_a, ox_b, ox_c], -1) @ w_out[l])
        hx = hx + mx[5] * expert_choice_ffn(modulate(rmsnorm(hx, norm2_g[l]), mx[3], mx[4]),
                                            w_router[l], w_e_gate[l], w_e_up[l], w_e_down[l])
        if with_ctx:
            oc_b = short_conv_mixer(uc_b, sc_conv_w[l])
            hc = hc + mc[2] * (jnp.concatenate([oc_a, oc_b, oc_c], -1) @ w_out[l])
            hc = hc + mc[5] * expert_choice_ffn(modulate(rmsnorm(hc, norm2_g[l]), mc[3], mc[4]),
                                                w_router[l], w_e_gate[l], w_e_up[l], w_e_down[l])
    return rmsnorm(hx, final_norm_g)
```

```python
import math
import numpy as np
import concourse.bass as bass
import concourse.mybir as mybir
from concourse.bass_utils import run_bass_kernel_spmd

F32 = mybir.dt.float32
BF16 = mybir.dt.bfloat16
I32 = mybir.dt.int32
U32 = mybir.dt.uint32
AF = mybir.ActivationFunctionType
ALU = mybir.AluOpType
AX = mybir.AxisListType


class T:
    __slots__ = ("ap", "w", "r", "name", "psum")

    def __init__(self, ap, name="", psum=False):
        self.ap = ap
        self.psum = psum
        self.w = None
        self.r = {}
        self.name = name

    def __getitem__(self, idx):
        return self.ap[idx]

    def view(self, idx, name=""):
        return T(self.ap[idx], name or self.name, self.psum)


class P:
    NDS = 8

    def __init__(self, nc):
        self.nc = nc
        self.eng = {"pe": nc.tensor, "act": nc.scalar, "dve": nc.vector, "pool": nc.gpsimd, "sp": nc.sync}
        self.sems = {}
        self.cnt = {}
        self._ctx = []
        for e in ("pe", "act", "dve", "pool"):
            self._mksem(e)
        for q in ("sp", "act", "pool"):
            for i in range(self.NDS):
                self._mksem(("d", q, i))
        self.dma_i = {"sp": 0, "act": 0, "pool": 0}
        self.seen = {e: {} for e in self.eng}
        self.n_instr = 0
        self.n_wait = 0

    def _mksem(self, key):
        name = "s_" + ("_".join(str(k) for k in key) if isinstance(key, tuple) else key)
        cm = self.nc.semaphore(name)
        s = cm.__enter__()
        self._ctx.append(cm)
        self.sems[key] = s
        self.cnt[key] = 0

    def _uniq(self, name):
        self._uid = getattr(self, "_uid", 0) + 1
        return f"{name}_{self._uid}"

    def sb(self, name, shape, dt=F32):
        name = self._uniq(name)
        cm = self.nc.sbuf_tensor(name, list(shape), dt)
        t = cm.__enter__()
        self._ctx.append(cm)
        return T(t, name)

    def ps(self, name, shape, dt=F32):
        name = self._uniq(name)
        cm = self.nc.psum_tensor(name, list(shape), dt)
        t = cm.__enter__()
        self._ctx.append(cm)
        return T(t, name, True)

    def dram(self, name, shape, dt, kind="Internal"):
        t = self.nc.dram_tensor(name, list(shape), dt, kind=kind)
        return T(t.ap(), name)

    def _wait(self, e, key, val):
        if val <= 0:
            return
        if self.seen[e].get(key, 0) >= val:
            return
        self.eng[e].wait_ge(self.sems[key], val)
        self.seen[e][key] = val
        self.n_wait += 1

    def _deps(self, e, mykey, reads, writes):
        for t in reads:
            if t.w is not None:
                self._wait(e, *t.w)
        for t in writes:
            if t.w is not None:
                self._wait(e, *t.w)
            for k, v in t.r.items():
                self._wait(e, k, v)

    def _mark(self, key, val, reads, writes):
        for t in reads:
            if t.r.get(key, 0) < val:
                t.r[key] = val
        for t in writes:
            t.w = (key, val)
            t.r = {}

    def op(self, e, reads, writes, fn):
        rp = [t for t in reads if t.psum and t not in writes]
        if rp:
            writes = list(writes) + rp
        self._deps(e, e, reads, writes)
        ins = fn(self.eng[e])
        self.cnt[e] += 1
        ins.then_inc(self.sems[e], 1)
        self._mark(e, self.cnt[e], reads, writes)
        self.n_instr += 1
        return ins

    def dma(self, q, out_t, out_ap, in_t, in_ap, **kw):
        i = self.dma_i[q] % self.NDS
        self.dma_i[q] += 1
        key = ("d", q, i)
        self._wait(q, key, self.cnt[key])
        self._deps(q, key, [in_t], [out_t])
        ins = self.eng[q].dma_start(out=out_ap, in_=in_ap, **kw)
        self.cnt[key] += 16
        ins.then_inc(self.sems[key], 16)
        self._mark(key, self.cnt[key], [in_t], [out_t])
        self.n_instr += 1
        return ins

    def _dma_generic(self, q, out_t, in_ts, issue):
        i = self.dma_i[q] % self.NDS
        self.dma_i[q] += 1
        key = ("d", q, i)
        self._wait(q, key, self.cnt[key])
        self._deps(q, key, in_ts, [out_t])
        ins = issue(self.eng[q])
        self.cnt[key] += 16
        ins.then_inc(self.sems[key], 16)
        self._mark(key, self.cnt[key], in_ts, [out_t])
        self.n_instr += 1
        return ins

    def dma_indirect_gather(self, out_t, out_ap, src_t, src_ap, idx_t, idx_ap):
        return self._dma_generic("pool", out_t, [src_t, idx_t], lambda g: g.indirect_dma_start(
            out=out_ap, out_offset=None, in_=src_ap, in_offset=bass.IndirectOffsetOnAxis(ap=idx_ap, axis=0)))

    def dma_indirect_scatter_add(self, dst_t, dst_ap, src_t, src_ap, idx_t, idx_ap):
        return self._dma_generic("pool", dst_t, [src_t, idx_t], lambda g: g.indirect_dma_start(
            out=dst_ap, out_offset=bass.IndirectOffsetOnAxis(ap=idx_ap, axis=0), in_=src_ap, in_offset=None,
            compute_op=ALU.add))

    def finish(self, out_ts, e="sp"):
        for t in out_ts:
            if t.w is not None:
                self._wait(e, *t.w)
        for key, v in self.cnt.items():
            self._wait(e, key, v)

    def mark(self):
        return len(self._ctx)

    def release(self, mark):
        while len(self._ctx) > mark:
            self._ctx.pop().__exit__(None, None, None)

    def barrier(self):
        for e in ("pe", "act", "dve", "pool", "sp"):
            for key, v in self.cnt.items():
                self._wait(e, key, v)

    def close(self):
        for cm in reversed(self._ctx):
            cm.__exit__(None, None, None)
        self._ctx = []


D = 2048
DEPTH = 4
EPS = 1e-6
N_IN = 6936
_PROGS = {}
N_LAUNCH = [0]


def get_prog(name, builder):
    if name not in _PROGS:
        _PROGS[name] = builder()
    return _PROGS[name]


def launch(nc, maps):
    N_LAUNCH[0] += 1
    res = run_bass_kernel_spmd(nc, maps, core_ids=list(range(len(maps))))
    return res.results


def rep128(v):
    return np.ascontiguousarray(np.broadcast_to(np.asarray(v, np.float32).reshape(1, -1), (128, v.size)))
TC = 256
TL = 4096
TA = TC + TL
NT = TA // 128
GH = 6
NCH = TA // 64
QKV0, Z0, B0c, A0c = 0, 2304, 3072, 3084
SC0 = 3096
CQ0, CK0, CV0 = 4632, 5400, 6168
NE = 16
FF = 1024
NEG = -1.0e30


class Ctx:
    pass


def declare_io(p, depth):
    C = Ctx()
    C.depth = depth
    ein = lambda n, s: p.dram(n, s, F32, kind="ExternalInput")
    C.x = ein("x", [TL, D]); C.ctx = ein("ctx", [TC, D])
    C.cT = ein("cT", [128, 16, 2])
    C.w_mod = ein("w_mod", [depth, D, 6 * D]); C.b_mod = ein("b_mod", [depth, 2, 6 * D])
    C.norm1 = ein("norm1", [depth, D]); C.norm2 = ein("norm2", [depth, D])
    C.w_in = ein("w_in", [depth, D, N_IN])
    C.gconv = ein("gconv", [depth, 128, 18, 3])
    C.galog = ein("galog", [depth, 64, 12]); C.gdtb = ein("gdtb", [depth, 64, 12])
    C.gnorm = ein("gnorm", [depth, 128, 1])
    C.sconv = ein("sconv", [depth, 128, 4, 3])
    C.dlam = ein("dlam", [depth, 128, 4, 64]); C.dnorm = ein("dnorm", [depth, 128, 1])
    C.w_out = ein("w_out", [depth, D, D]); C.w_r = ein("w_r", [depth, D, NE])
    C.w_eg = ein("w_eg", [depth, NE, D, FF]); C.w_eu = ein("w_eu", [depth, NE, D, FF]); C.w_ed = ein("w_ed", [depth, NE, FF, D])
    C.fnorm = ein("fnorm", [1, D])
    C.ident = ein("ident", [128, 128])
    C.cosT = ein("cosT", [128, TL]); C.sinT = ein("sinT", [128, TL])
    C.rotm = ein("rotm", [128, 128])
    C.tri = ein("tri", [2, 64, 64])
    C.nmask = ein("nmask", [4, 64, 64])
    C.out = p.dram("out", [TL, D], F32, kind="ExternalOutput")
    return C


def declare_scratch(p, C):
    C.h = p.dram("h_s", [TA, D], F32)
    C.hv = [C.h.view((slice(i * 128, (i + 1) * 128), slice(None))) for i in range(NT)]
    C.mod = p.dram("mod_s", [C.depth, 2, 6 * D], F32)
    C.uT = [p.dram(f"uT{c}", [128, TA], F32) for c in range(48)]
    C.uTv = [[t.view((slice(None), slice(hf * 2176, (hf + 1) * 2176))) for hf in range(2)] for t in C.uT]
    C.ba = p.dram("ba_s", [64, NCH, 24], F32)
    C.bav = [C.ba.view((slice(None), slice(hf * 34, (hf + 1) * 34), slice(None))) for hf in range(2)]
    C.cv = p.dram("cv_s", [TA, 768], BF16)
    C.cvv = [C.cv.view((slice(i * 128, (i + 1) * 128), slice(None))) for i in range(NT)]
    C.mixT = [p.dram(f"mixT{c}", [128, TA], BF16) for c in range(16)]


def bc_load(p, q, dst_t, dst_ap, src_t, src_row_ap, nparts=128):
    p.dma(q, dst_t, dst_ap, src_t, src_row_ap.partition_broadcast(nparts))


def stage_M(p, C):
    mk = p.mark()
    cT = p.sb("cT_s", [128, 16, 2]); sg = p.sb("sg", [128, 16, 2])
    bm = p.sb("bm_s", [2, 6 * D]); res = p.sb("resM", [2, 6 * D])
    wts = [p.sb(f"wtM{i}", [128, 4, 1024]) for i in range(3)]
    pss = [p.ps(f"psM{i}", [2, 512]) for i in range(4)]
    p.dma("sp", cT, cT[:], C.cT, C.cT[:])
    p.op("act", [cT], [sg], lambda e: e.activation(out=sg[:], in_=cT[:], func=AF.Sigmoid))
    p.op("dve", [cT, sg], [sg], lambda e: e.tensor_tensor(out=sg[:], in0=cT[:], in1=sg[:], op=ALU.mult))
    wi = 0
    for l in range(C.depth):
        p.dma("sp", bm, bm[:], C.b_mod, C.b_mod.ap[l])
        for ng in range(12):
            ps4 = pss[(ng % 2) * 2:(ng % 2) * 2 + 2]
            for kq in range(4):
                wt = wts[wi % 3]; wi += 1
                p.dma("sp" if wi % 2 else "act", wt, wt[:], C.w_mod,
                      C.w_mod.ap[l, kq * 512:(kq + 1) * 512, ng * 1024:(ng + 1) * 1024].rearrange("(k p) n -> p k n", p=128))
                for n in range(2):
                    for k in range(4):
                        p.op("pe", [sg, wt], [ps4[n]], lambda e: e.matmul(
                            ps4[n][:], lhsT=sg[:, kq * 4 + k, :], rhs=wt[:, k, n * 512:(n + 1) * 512],
                            start=(kq == 0 and k == 0), stop=(kq == 3 and k == 3)))
            for n in range(2):
                c0 = ng * 1024 + n * 512
                p.op("dve", [ps4[n], bm], [res], lambda e: e.tensor_tensor(
                    out=res[:, c0:c0 + 512], in0=ps4[n][:], in1=bm[:, c0:c0 + 512], op=ALU.add))
        p.dma("sp", C.mod, C.mod.ap[l], res, res[:])
    p.barrier()
    p.release(mk)


def modrow(C, l, kind, i):
    r = 1 if kind == 0 else 0
    return C.mod.ap[l, r:r + 1, i * D:(i + 1) * D]


def stage_A(p, C, l):
    mk = p.mark()
    gs = p.sb("gsA", [128, 2, D]); sh = p.sb("shA", [128, 2, D]); g1 = p.sb("g1A", [128, D])
    idf = p.sb("idfA", [128, 128]); idb = p.sb("idbA", [128, 128], BF16)
    aT = p.sb("aT", [128, 16, 2176], BF16)
    aTv = [aT.view((slice(None), slice(None), slice(i * 128, (i + 1) * 128))) for i in range(17)]
    p.dma("sp", idf, idf[:], C.ident, C.ident[:])
    p.op("dve", [idf], [idb], lambda e: e.tensor_copy(out=idb[:], in_=idf[:]))
    bc_load(p, "sp", g1, g1[:], C.norm1, C.norm1.ap[l:l + 1, :])
    for kind in range(2):
        bc_load(p, "act", gs, gs[:, kind, :], C.mod, modrow(C, l, kind, 1))
        bc_load(p, "sp", sh, sh[:, kind, :], C.mod, modrow(C, l, kind, 0))
    for kind in range(2):
        p.op("dve", [gs, g1], [gs], lambda e: e.scalar_tensor_tensor(
            out=gs[:, kind, :], in0=gs[:, kind, :], scalar=1.0, in1=g1[:], op0=ALU.add, op1=ALU.mult))
    for hf in range(2):
        mk1 = p.mark()
        hb = [p.sb(f"hbA{i}", [128, D]) for i in range(2)]
        yb = [p.sb(f"ybA{i}", [128, D]) for i in range(2)]
        ab = [p.sb(f"abA{i}", [128, D], BF16) for i in range(2)]
        ss = [p.sb(f"ssA{i}", [128, 2]) for i in range(2)]
        ptr = [p.ps(f"ptrA{i}", [128, 4, 128], BF16) for i in range(4)]
        tcount = 0
        for tt in range(17):
            ti = hf * 17 + tt
            kind = 0 if ti < 2 else 1
            ht = hb[tt % 2]; y_ = yb[tt % 2]; a_ = ab[tt % 2]; s_ = ss[tt % 2]
            p.dma("sp", ht, ht[:], C.hv[ti], C.hv[ti][:])
            p.op("pool", [], [s_], lambda e: e.memset(s_[:], 0.0))
            p.op("act", [ht], [y_, s_], lambda e: e.activation(out=y_[:], in_=ht[:], func=AF.Square, accum_out=s_[:, 0:1]))
            p.op("dve", [s_], [s_], lambda e: e.tensor_scalar(out=s_[:, 1:2], in0=s_[:, 0:1], scalar1=1.0 / D, scalar2=EPS, op0=ALU.mult, op1=ALU.add))
            p.op("act", [s_], [s_], lambda e: e.sqrt(out=s_[:, 1:2], in_=s_[:, 1:2]))
            p.op("dve", [s_], [s_], lambda e: e.reciprocal(out=s_[:, 1:2], in_=s_[:, 1:2]))
            p.op("dve", [ht, s_, gs], [y_], lambda e: e.scalar_tensor_tensor(
                out=y_[:], in0=ht[:], scalar=s_[:, 1:2], in1=gs[:, kind, :], op0=ALU.mult, op1=ALU.mult))
            p.op("pool", [y_, sh], [a_], lambda e: e.tensor_tensor(out=a_[:], in0=y_[:], in1=sh[:, kind, :], op=ALU.add))
            for k4 in range(4):
                pt = ptr[tcount % 4]; tcount += 1
                for kk in range(4):
                    k = k4 * 4 + kk
                    p.op("pe", [a_, idb], [pt], lambda e: e.transpose(out=pt[:, kk, :], in_=a_[:, k * 128:(k + 1) * 128], identity=idb[:]))
                p.op("act", [pt], [aTv[tt]], lambda e: e.copy(out=aT[:, k4 * 4:(k4 + 1) * 4, tt * 128:(tt + 1) * 128], in_=pt[:]))
        p.barrier()
        p.release(mk1)
        aTall = [aT] + aTv
        mk2 = p.mark()
        wb = [p.sb(f"wbA{i}", [128, 16, 512], BF16) for i in range(2)]
        wbv = [(w.view((slice(None), slice(0, 8), slice(None))), w.view((slice(None), slice(8, 16), slice(None)))) for w in wb]
        stg = [p.sb(f"stgA{i}", [128, 2176]) for i in range(4)]
        pu = [p.ps(f"puA{i}", [128, 512]) for i in range(8)]
        wcount = 0; setc = 0; scount = 0; ec = 0

        def load_w(c0, nw):
            nonlocal wcount
            wv = wbv[wcount % 2]; wcount += 1
            for half in range(2):
                p.dma("pool", wv[half], wv[half][:, :, :nw], C.w_in,
                      C.w_in.ap[l, half * 1024:(half + 1) * 1024, c0:c0 + nw].rearrange("(k p) n -> p k n", p=128))
            return wv

        def evac(pp, dst_t, dst_ap, src_ap):
            nonlocal ec
            ec += 1
            if ec % 2:
                p.op("act", [pp], [dst_t], lambda e: e.copy(out=dst_ap, in_=src_ap))
            else:
                p.op("dve", [pp], [dst_t], lambda e: e.tensor_copy(out=dst_ap, in_=src_ap))
        fm_cols = [QKV0 + 128 * i for i in range(24)] + [SC0 + 128 * i for i in range(12)] + [CQ0 + 128 * i for i in range(12)]
        for g4 in range(12):
            c0 = fm_cols[g4 * 4]
            wv = load_w(c0, 512)
            sts = [stg[cc] for cc in range(4)]
            for cc in range(4):
                bank = pu[(setc % 2) * 4:(setc % 2) * 4 + 4]; setc += 1
                for k in range(16):
                    for tb in range(4):
                        p.op("pe", aTall + [wv[k // 8]], [bank[tb]], lambda e: e.matmul(
                            bank[tb][:, :], lhsT=wv[k // 8][:, k % 8, cc * 128:(cc + 1) * 128], rhs=aT[:, k, tb * 512:(tb + 1) * 512], start=(k == 0), stop=(k == 15)))
                for tb in range(4):
                    evac(bank[tb], sts[cc], sts[cc][:, tb * 512:(tb + 1) * 512], bank[tb][:, :])
            bank = pu[(setc % 2) * 4:(setc % 2) * 4 + 4]; setc += 1
            for k in range(16):
                for cc in range(4):
                    p.op("pe", aTall + [wv[k // 8]], [bank[cc]], lambda e: e.matmul(
                        bank[cc][:, :128], lhsT=wv[k // 8][:, k % 8, cc * 128:(cc + 1) * 128], rhs=aT[:, k, 2048:2176], start=(k == 0), stop=(k == 15)))
            for cc in range(4):
                evac(bank[cc], sts[cc], sts[cc][:, 2048:2176], bank[cc][:, :128])
                ch = g4 * 4 + cc
                p.dma("sp" if cc % 2 else "act", C.uTv[ch][hf], C.uTv[ch][hf][:], sts[cc], sts[cc][:])
        for vg in range(2):
            nw = 512 if vg == 0 else 256
            wv = load_w(CV0 + vg * 512, nw)
            for t4 in range(0, 17, 4):
                tl = list(range(t4, min(t4 + 4, 17)))
                bank = pu[(setc % 2) * 4:(setc % 2) * 4 + 4]; setc += 1
                for k in range(16):
                    for j, tt in enumerate(tl):
                        p.op("pe", aTall + [wv[k // 8]], [bank[j]], lambda e: e.matmul(
                            bank[j][:, :nw], lhsT=aT[:, k, tt * 128:(tt + 1) * 128], rhs=wv[k // 8][:, k % 8, :nw], start=(k == 0), stop=(k == 15)))
                for j, tt in enumerate(tl):
                    ti = hf * 17 + tt
                    st = stg[scount % 4]; scount += 1
                    evac(bank[j], st, st[:, :nw], bank[j][:, :nw])
                    p.dma("pool", C.cvv[ti], C.cvv[ti][:, vg * 512:vg * 512 + nw], st, st[:, :nw])
        wv = load_w(B0c, 24)
        bst = p.sb("bstA", [64, 34, 24])
        for c4 in range(0, 34, 4):
            cl = list(range(c4, min(c4 + 4, 34)))
            bank = pu[(setc % 2) * 4:(setc % 2) * 4 + 4]; setc += 1
            for k in range(16):
                for j, cj in enumerate(cl):
                    p.op("pe", aTall + [wv[k // 8]], [bank[j]], lambda e: e.matmul(
                        bank[j][:64, :24], lhsT=aT[:, k, cj * 64:(cj + 1) * 64], rhs=wv[k // 8][:, k % 8, :24], start=(k == 0), stop=(k == 15)))
            for j, cj in enumerate(cl):
                p.op("dve", [bank[j]], [bst], lambda e: e.tensor_copy(out=bst[:, cj, :], in_=bank[j][:64, :24]))
        p.dma("sp", C.bav[hf], C.bav[hf][:], bst, bst[:])
        p.barrier()
        p.release(mk2)
    p.release(mk)


def stage_load(p, C):
    p.dma("sp", C.h, C.h.ap[0:TC, :], C.ctx, C.ctx[:])
    for i in range(4):
        p.dma("act" if i % 2 else "sp", C.h, C.h.ap[TC + i * 1024:TC + (i + 1) * 1024, :], C.x, C.x.ap[i * 1024:(i + 1) * 1024, :])
    p.barrier()


def dump(p, src_t, src_ap, name, shape, dt=F32):
    o = p.dram(name, shape, dt, kind="ExternalOutput")
    p.dma("sp", o, o[:], src_t, src_ap)
    return o


def stage_SC(p, C, l, with_ctx=True):
    mk = p.mark()
    cw = p.sb("cwSC", [128, 4, 3])
    p.dma("sp", cw, cw[:], C.sconv, C.sconv.ap[l])
    bufs = [[p.sb(f"sc{n}{i}", [128, TA]) for n in ("b", "c", "x", "y")] for i in range(2)]
    ob = [p.sb(f"scO{i}", [128, TA], BF16) for i in range(2)]
    segs = [(0, TC), (TC, TA)]
    for c in range(4):
        bg, cg, xi, y = bufs[c % 2]
        o_ = ob[c % 2]
        p.dma("sp", bg, bg[:], C.uT[24 + c], C.uT[24 + c][:])
        p.dma("act", cg, cg[:], C.uT[28 + c], C.uT[28 + c][:])
        p.dma("sp", xi, xi[:], C.uT[32 + c], C.uT[32 + c][:])
        p.op("dve", [cg, xi], [cg], lambda e: e.tensor_tensor(out=cg[:], in0=cg[:], in1=xi[:], op=ALU.mult))
        p.op("act", [cg, cw], [y], lambda e: e.mul(out=y[:], in_=cg[:], mul=cw[:, c, 1:2]))
        for (s0, s1) in segs:
            p.op("dve", [cg, cw, y], [y], lambda e: e.scalar_tensor_tensor(
                out=y[:, s0 + 1:s1], in0=cg[:, s0:s1 - 1], scalar=cw[:, c, 0:1], in1=y[:, s0 + 1:s1], op0=ALU.mult, op1=ALU.add))
            p.op("dve", [cg, cw, y], [y], lambda e: e.scalar_tensor_tensor(
                out=y[:, s0:s1 - 1], in0=cg[:, s0 + 1:s1], scalar=cw[:, c, 2:3], in1=y[:, s0:s1 - 1], op0=ALU.mult, op1=ALU.add))
        p.op("pool", [bg, y], [o_], lambda e: e.tensor_tensor(out=o_[:], in0=bg[:], in1=y[:], op=ALU.mult))
        p.dma("sp", C.mixT[6 + c], C.mixT[6 + c][:], o_, o_[:])
    p.barrier()
    p.release(mk)


def stage_ATT(p, C, l, with_ctx=True):
    lam_init = 0.8 - 0.6 * math.exp(-0.3 * l)
    mk = p.mark()
    onesb = p.sb("onesB", [128, 128], BF16)
    p.op("pool", [], [onesb], lambda e: e.memset(onesb[:], 1.0))
    onesf = p.sb("onesF", [128, 128])
    p.op("pool", [], [onesf], lambda e: e.memset(onesf[:], 1.0))
    accs = [p.sb(f"accA{i}", [128, 512]) for i in range(4)]
    rot = p.sb("rotA", [128, 128])
    p.dma("sp", rot, rot[:], C.rotm, C.rotm[:])
    cosT = p.sb("cosA", [128, TL]); sinT = p.sb("sinA", [128, TL])
    p.dma("sp", cosT, cosT[:], C.cosT, C.cosT[:])
    p.dma("act", sinT, sinT[:], C.sinT, C.sinT[:])
    dn = p.sb("dnA", [128, 1])
    p.dma("sp", dn, dn[:], C.dnorm, C.dnorm.ap[l])
    p.op("dve", [dn], [dn], lambda e: e.tensor_scalar(out=dn[:], in0=dn[:], scalar1=(1.0 - lam_init), scalar2=None, op0=ALU.mult))
    dl = p.sb("dlA", [128, 4, 64]); lw = p.sb("lwA", [128, 8])
    p.dma("sp", dl, dl[:], C.dlam, C.dlam.ap[l])
    p.op("dve", [dl], [dl], lambda e: e.tensor_tensor(out=dl[:, 0, :], in0=dl[:, 0, :], in1=dl[:, 1, :], op=ALU.mult))
    p.op("dve", [dl], [dl], lambda e: e.tensor_tensor(out=dl[:, 2, :], in0=dl[:, 2, :], in1=dl[:, 3, :], op=ALU.mult))
    p.op("dve", [dl], [lw], lambda e: e.reduce_sum(out=lw[:, 0:1], in_=dl[:, 0, :], axis=AX.X))
    p.op("dve", [dl], [lw], lambda e: e.reduce_sum(out=lw[:, 1:2], in_=dl[:, 2, :], axis=AX.X))
    p.op("act", [lw], [lw], lambda e: e.activation(out=lw[:, 2:4], in_=lw[:, 0:2], func=AF.Exp))
    p.op("dve", [lw], [lw], lambda e: e.tensor_tensor(out=lw[:, 4:5], in0=lw[:, 3:4], in1=lw[:, 2:3], op=ALU.subtract))
    p.op("dve", [lw], [lw], lambda e: e.tensor_scalar(out=lw[:, 5:6], in0=lw[:, 4:5], scalar1=-lam_init, scalar2=None, op0=ALU.add))
    neg_lam = lw
    xq = [p.sb(f"xqA{i}", [128, TA]) for i in range(2)]
    qTr = p.sb("qTrA", [128, TA], BF16); kTr = p.sb("kTrA", [128, TA], BF16)
    vsb = p.sb("vA", [128, NT, 128], BF16)
    tmp = [p.sb(f"tmpA{i}", [128, 512]) for i in range(2)]
    PT = [p.sb(f"PT{i}", [128, 512], BF16) for i in range(4)]
    ep = [p.sb(f"epA{i}", [128, 512]) for i in range(4)]
    sqb = p.sb("sqA", [128, 512], BF16)
    ost = [p.sb(f"ostA{i}", [128, 512], BF16) for i in range(2)]
    pS = [p.ps(f"pS{i}", [128, 512]) for i in range(3)]
    pO = [p.ps(f"pO{i}", [128, 512]) for i in range(2)]
    pD = [p.ps(f"pD{i}", [128, 512]) for i in range(2)]
    pR = p.ps("pR", [128, 512])
    sc_i = 0; pt_i = 0
    for h in range(GH):
        for which, (src, dst) in enumerate(((C.uT[36 + h], qTr), (C.uT[42 + h], kTr))):
            x_ = xq[which]
            p.dma("sp" if which == 0 else "act", x_, x_[:], src, src[:])
            p.op("pool", [x_], [dst], lambda e: e.tensor_copy(out=dst[:, 0:TC], in_=x_[:, 0:TC]))
            for qb in range(8):
                c0 = TC + qb * 512
                p.op("pe", [rot, x_], [pR], lambda e: e.matmul(pR[:], lhsT=rot[:], rhs=x_[:, c0:c0 + 512], start=True, stop=True))
                t_ = tmp[qb % 2]
                p.op("dve", [pR, sinT], [t_], lambda e: e.tensor_tensor(out=t_[:], in0=pR[:], in1=sinT[:, qb * 512:(qb + 1) * 512], op=ALU.mult))
                p.op("pool", [x_, cosT], [x_], lambda e: e.tensor_tensor(out=x_[:, c0:c0 + 512], in0=x_[:, c0:c0 + 512], in1=cosT[:, qb * 512:(qb + 1) * 512], op=ALU.mult))
                p.op("dve", [x_, t_], [dst], lambda e: e.tensor_tensor(out=dst[:, c0:c0 + 512], in0=x_[:, c0:c0 + 512], in1=t_[:], op=ALU.add))
        p.dma("sp", vsb, vsb[:], C.cv, C.cv.ap[:, h * 128:(h + 1) * 128].rearrange("(t p) d -> p t d", p=128))
        blocks = [(TC + qb * 512, 512, list(range(NT))) for qb in range(8)]
        if with_ctx:
            blocks.append((0, TC, [0, 1]))
        for bi, (q0, qw, ktiles) in enumerate(blocks):
            for m in range(2):
                po = pO[m]; pd = pD[m]
                ac = accs[(bi % 2) * 2 + m]
                nk = len(ktiles)

                def qk(ki):
                    nonlocal sc_i
                    kt = ktiles[ki]
                    ps_ = pS[sc_i % 3]; sc_i += 1
                    p.op("pe", [kTr, qTr], [ps_], lambda e: e.matmul(
                        ps_[:, :qw], lhsT=kTr[m * 64:(m + 1) * 64, kt * 128:(kt + 1) * 128], rhs=qTr[m * 64:(m + 1) * 64, q0:q0 + qw], start=True, stop=True))
                    return ps_
                nxt = qk(0)
                for ki, kt in enumerate(ktiles):
                    ps_ = nxt
                    if ki + 1 < nk:
                        nxt = qk(ki + 1)
                    pt = PT[pt_i % 4]; pt_i += 1
                    p.op("act", [ps_], [pt], lambda e: e.activation(out=pt[:, :qw], in_=ps_[:, :qw], func=AF.Exp, scale=0.125))
                    p.op("pe", [vsb, pt], [po], lambda e: e.matmul(po[:, :qw], lhsT=vsb[:, kt, :], rhs=pt[:, :qw], start=(ki == 0), stop=(ki == nk - 1)))
                    if ki == 0:
                        p.op("dve", [pt], [ac], lambda e: e.tensor_copy(out=ac[:, :qw], in_=pt[:, :qw]))
                    else:
                        p.op("dve", [pt, ac], [ac], lambda e: e.tensor_tensor(out=ac[:, :qw], in0=ac[:, :qw], in1=pt[:, :qw], op=ALU.add))
                p.op("pe", [onesf, ac], [pd], lambda e: e.matmul(pd[:, :qw], lhsT=onesf[:], rhs=ac[:, :qw], start=True, stop=True))
            e0, e1, e2, e3 = ep
            p.op("dve", [pD[0]], [e0], lambda e: e.reciprocal(out=e0[:, :qw], in_=pD[0][:, :qw]))
            p.op("dve", [pO[0], e0], [e0], lambda e: e.tensor_tensor(out=e0[:, :qw], in0=pO[0][:, :qw], in1=e0[:, :qw], op=ALU.mult))
            p.op("dve", [pD[1]], [e1], lambda e: e.reciprocal(out=e1[:, :qw], in_=pD[1][:, :qw]))
            p.op("dve", [pO[1], e1], [e1], lambda e: e.tensor_tensor(out=e1[:, :qw], in0=pO[1][:, :qw], in1=e1[:, :qw], op=ALU.mult))
            p.op("dve", [e0, e1, neg_lam], [e2], lambda e: e.scalar_tensor_tensor(
                out=e2[:, :qw], in0=e1[:, :qw], scalar=neg_lam[:, 5:6], in1=e0[:, :qw], op0=ALU.mult, op1=ALU.add))
            p.op("pool", [e2], [sqb], lambda e: e.tensor_tensor(out=sqb[:, :qw], in0=e2[:, :qw], in1=e2[:, :qw], op=ALU.mult))
            p.op("pe", [onesb, sqb], [pR], lambda e: e.matmul(pR[:, :qw], lhsT=onesb[:], rhs=sqb[:, :qw], start=True, stop=True))
            p.op("dve", [pR], [e3], lambda e: e.tensor_scalar(out=e3[:, :qw], in0=pR[:, :qw], scalar1=1.0 / 128, scalar2=EPS, op0=ALU.mult, op1=ALU.add))
            p.op("act", [e3], [e3], lambda e: e.activation(out=e3[:, :qw], in_=e3[:, :qw], func=AF.Ln))
            p.op("act", [e3], [e3], lambda e: e.activation(out=e3[:, :qw], in_=e3[:, :qw], func=AF.Exp, scale=-0.5))
            o_ = ost[bi % 2]
            p.op("dve", [e2, e3, dn], [o_], lambda e: e.scalar_tensor_tensor(
                out=o_[:, :qw], in0=e2[:, :qw], scalar=dn[:, 0:1], in1=e3[:, :qw], op0=ALU.mult, op1=ALU.mult))
            p.dma("sp", C.mixT[10 + h], C.mixT[10 + h][:, q0:q0 + qw], o_, o_[:, :qw])
    p.barrier()
    p.release(mk)


INV_DT = F32


def gdn_windows(d):
    lat = [(4 + 8 * i, 8) for i in range(8)]
    if d == 0:
        return [(0, 4)] + lat
    return [(0, 4)] + lat[::-1]


def stage_GDN(p, C, l, dbg=99):
    mk = p.mark()
    C.oTd = getattr(C, "oTd", None) or [[p.dram(f"oTd{d}_{h}", [128, TA], F32) for h in range(GH)] for d in range(2)]
    idf = p.sb("idG", [128, 128]); p.dma("sp", idf, idf[:], C.ident, C.ident[:])
    tri = p.sb("triG", [64, 2, 64]); p.dma("sp", tri, tri[:], C.tri, C.tri.ap.rearrange("d p i -> p d i"))
    nm = p.sb("nmG", [64, 4, 64]); p.dma("act", nm, nm[:], C.nmask, C.nmask.ap.rearrange("d p i -> p d i"))
    st01 = p.sb("st01G", [64, 2, 64])
    for d in range(2):
        p.op("dve", [tri, idf], [st01], lambda e: e.tensor_tensor(out=st01[:, d, :], in0=tri[:, d, :], in1=idf[:64, :64], op=ALU.subtract))
    ones = p.sb("onesG", [128, 128]); p.op("pool", [], [ones], lambda e: e.memset(ones[:], 1.0))
    cw = p.sb("cwG", [128, 18, 3]); p.dma("sp", cw, cw[:], C.gconv, C.gconv.ap[l])
    gn = p.sb("gnG", [128, 1]); p.dma("sp", gn, gn[:], C.gnorm, C.gnorm.ap[l])
    if dbg == -3:
        C.dbg = [(st01, st01[:].rearrange("p a i -> p (a i)"), [64, 128]), (nm, nm[:].rearrange("p a i -> p (a i)"), [64, 256])]
        return
    ba = p.sb("baG", [64, NCH, 24]); p.dma("sp", ba, ba[:], C.ba, C.ba[:])
    alog = p.sb("alogG", [64, 12]); p.dma("act", alog, alog[:], C.galog, C.galog.ap[l])
    dtb = p.sb("dtbG", [64, 12]); p.dma("act", dtb, dtb[:], C.gdtb, C.gdtb.ap[l])
    NG = NCH * GH
    bsig = p.sb("bsigG", [64, 2, NCH, GH]); gval = p.sb("gvalG", [64, 2, NCH, GH])
    gc = p.sb("gcG", [64, 2, NCH, GH]); gtot = p.sb("gtotG", [64, 2, NCH, GH])
    egc = p.sb("egcG", [64, 2, NCH, GH]); ekd = p.sb("ekdG", [64, 2, NCH, GH]); egl = p.sb("eglG", [128, 2, NCH, GH])
    pp = [p.ps(f"ppG{i}", [128, 512]) for i in range(2)]
    psA = [p.ps(f"psA{h}", [128, 512]) for h in range(GH)]
    ppi = [0]

    def nextpp():
        t = pp[ppi[0] % 2]; ppi[0] += 1
        return t
    p.op("act", [alog], [alog], lambda e: e.activation(out=alog[:], in_=alog[:], func=AF.Exp))
    for d in range(2):
        bsl = ba[:, :, d * 6:(d + 1) * 6]
        asl = ba[:, :, 12 + d * 6:12 + (d + 1) * 6]
        p.op("act", [ba], [bsig], lambda e: e.activation(out=bsig[:, d], in_=bsl, func=AF.Sigmoid))
        p.op("dve", [ba, dtb], [gval], lambda e: e.tensor_tensor(out=gval[:, d], in0=asl, in1=dtb[:, d * 6:(d + 1) * 6].unsqueeze(1).to_broadcast([64, NCH, GH]), op=ALU.add))
        p.op("act", [gval], [gval], lambda e: e.activation(out=gval[:, d], in_=gval[:, d], func=AF.Exp))
        p.op("dve", [gval], [gval], lambda e: e.tensor_scalar(out=gval[:, d], in0=gval[:, d], scalar1=1.0, scalar2=None, op0=ALU.add))
        p.op("act", [gval], [gval], lambda e: e.activation(out=gval[:, d], in_=gval[:, d], func=AF.Ln))
        p.op("dve", [gval, alog], [gval], lambda e: e.scalar_tensor_tensor(
            out=gval[:, d], in0=gval[:, d], scalar=-1.0, in1=alog[:, d * 6:(d + 1) * 6].unsqueeze(1).to_broadcast([64, NCH, GH]), op0=ALU.mult, op1=ALU.mult))
        if dbg == -2:
            C.dbg = [(gval, gval[:].rearrange("p a n h -> p (a n h)"), [64, 2 * NG]), (bsig, bsig[:].rearrange("p a n h -> p (a n h)"), [64, 2 * NG])]
            return
        t = nextpp()
        p.op("pe", [tri, gval], [t], lambda e: e.matmul(t[:64, :NG], lhsT=tri[:, d, :], rhs=gval[:, d].rearrange("p n h -> p (n h)"), start=True, stop=True))
        p.op("dve", [t], [gc], lambda e: e.tensor_copy(out=gc[:, d].rearrange("p n h -> p (n h)"), in_=t[:64, :NG]))
        if dbg == -1:
            C.dbg = [(gc, gc[:].rearrange("p a n h -> p (a n h)"), [64, 2 * NG])]
            return
        t2 = nextpp()
        p.op("pe", [ones, gval], [t2], lambda e: e.matmul(t2[:, :NG], lhsT=ones[:64, :], rhs=gval[:, d].rearrange("p n h -> p (n h)"), start=True, stop=True))
        p.op("dve", [t2], [gtot], lambda e: e.tensor_copy(out=gtot[:, d].rearrange("p n h -> p (n h)"), in_=t2[:64, :NG]))
        p.op("act", [t2], [egl], lambda e: e.activation(out=egl[:, d].rearrange("p n h -> p (n h)"), in_=t2[:, :NG], func=AF.Exp))
        p.op("act", [gc], [egc], lambda e: e.activation(out=egc[:, d], in_=gc[:, d], func=AF.Exp))
        p.op("dve", [gtot, gc], [ekd], lambda e: e.tensor_tensor(out=ekd[:, d], in0=gtot[:, d], in1=gc[:, d], op=ALU.subtract))
        p.op("act", [ekd], [ekd], lambda e: e.activation(out=ekd[:, d], in_=ekd[:, d], func=AF.Exp))
    if dbg < 1:
        C.dbg = [(gval, gval[:].rearrange("p a n h -> p (a n h)"), [64, 2 * NG]), (gc, gc[:].rearrange("p a n h -> p (a n h)"), [64, 2 * NG])]
        return
    H = []
    for h in range(GH):
        hb = Ctx()
        hb.wT = p.sb(f"wT{h}", [128, 8, 64], BF16); hb.u = p.sb(f"u{h}", [64, 8, 128])
        hb.attnT = p.sb(f"attnT{h}", [64, 8, 64], BF16); hb.kdec = p.sb(f"kdec{h}", [64, 8, 128], BF16)
        hb.qdT = p.sb(f"qdT{h}", [128, 8, 64], BF16); hb.oT = p.sb(f"oTw{h}", [128, 8, 64])
        hb.S = p.sb(f"S{h}", [128, 128]); hb.Sb = p.sb(f"Sb{h}", [128, 128], BF16)
        hb.vn = p.sb(f"vn{h}", [64, 128], BF16)
        hb.ps1 = psA[h].view((slice(0, 64), slice(0, 128)))
        hb.psO = psA[h].view((slice(None), slice(128, 192)))
        hb.psS = psA[h].view((slice(None), slice(256, 384)))
        H.append(hb)
    xrs = [p.sb(f"xrG{j}", [128, 514]) for j in range(3)]; cy = p.sb("cyG", [128, 3, 512]); sq = p.sb("sqG", [128, 512])
    rs = p.sb("rsG", [128, 512]); qn = p.sb("qnG", [128, 512]); kn = p.sb("knG", [128, 512])
    ktok = p.sb("ktokG", [64, 8, 128]); vtok = p.sb("vtokG", [64, 8, 128])
    Gm = p.sb("GmG", [64, 8, 64]); Bm = p.sb("BmG", [64, 8, 64])
    diff = p.sb("diffG", [64, 8, 64]); DT = p.sb("DTG", [64, 8, 64]); Ds2 = p.sb("Ds2G", [64, 8, 64])
    bbc = p.sb("bbcG", [64, 8, 64]); mkk = p.sb("mkkG", [64, 8, 64])
    LT = p.sb("LTG", [64, 8, 64]); Lm = p.sb("LmG", [64, 8, 64])
    Xa = [p.sb(f"XaG{i}", [64, 8, 64], INV_DT) for i in range(2)]; Xb = [p.sb(f"XbG{i}", [64, 8, 64], INV_DT) for i in range(2)]
    Rr = [p.sb(f"RG{i}", [64, 8, 64]) for i in range(2)]
    Rb = [p.sb(f"RbG{i}", [64, 8, 64], INV_DT) for i in range(2)]
    LTb = p.sb("LTbG", [64, 8, 64], INV_DT); Lmb = p.sb("LmbG", [64, 8, 64], INV_DT)
    kbs = [p.sb(f"kbG{i}", [64, 8, 128]) for i in range(2)]; vbs = [p.sb(f"vbG{i}", [64, 8, 128]) for i in range(2)]
    psC = psA[:4]; ci = [0]
    SEG = [(0, TC), (TC, TA)]

    def bcn(ap2, nc_, w):
        return ap2.unsqueeze(2).to_broadcast([64, nc_, w])

    def front_gen(h, d, c0, nc_, par):
        hb = H[h]
        W = nc_ * 64; t0 = c0 * 64
        s0, s1 = SEG[0] if c0 < 4 else SEG[1]
        lo = 1 if t0 == s0 else 0
        hi = 1 if t0 + W == s1 else 0
        for j, ch in enumerate((h, 6 + h, 12 + h)):
            xr = xrs[j]
            if lo or hi:
                p.op("pool", [], [xr], lambda e: e.memset(xr[:], 0.0))
            p.dma("sp" if j != 1 else "act", xr, xr[:, lo:W + 2 - hi], C.uT[ch], C.uT[ch][:, t0 - 1 + lo:t0 + W + 1 - hi])
        for j, ch in enumerate((h, 6 + h, 12 + h)):
            xr = xrs[j]
            p.op("act", [xr, cw], [cy], lambda e: e.mul(out=cy[:, j, :W], in_=xr[:, 1:W + 1], mul=cw[:, ch, 1:2]))
            p.op("dve", [xr, cw, cy], [cy], lambda e: e.scalar_tensor_tensor(
                out=cy[:, j, :W], in0=xr[:, 0:W], scalar=cw[:, ch, 0:1], in1=cy[:, j, :W], op0=ALU.mult, op1=ALU.add))
            p.op("dve", [xr, cw, cy], [cy], lambda e: e.scalar_tensor_tensor(
                out=cy[:, j, :W], in0=xr[:, 2:W + 2], scalar=cw[:, ch, 2:3], in1=cy[:, j, :W], op0=ALU.mult, op1=ALU.add))
            p.op("act", [cy], [cy], lambda e: e.activation(out=cy[:, j, :W], in_=cy[:, j, :W], func=AF.Silu))
        yield 0
        for j, dst, scl in ((0, qn, 128.0 ** -0.5), (1, kn, 1.0)):
            yield 0
            p.op("pool", [cy], [sq], lambda e: e.tensor_tensor(out=sq[:, :W], in0=cy[:, j, :W], in1=cy[:, j, :W], op=ALU.mult))
            t = nextpp()
            p.op("pe", [ones, sq], [t], lambda e: e.matmul(t[:, :W], lhsT=ones[:], rhs=sq[:, :W], start=True, stop=True))
            p.op("dve", [t], [rs], lambda e: e.tensor_scalar(out=rs[:, :W], in0=t[:, :W], scalar1=EPS, scalar2=None, op0=ALU.add))
            p.op("act", [rs], [rs], lambda e: e.activation(out=rs[:, :W], in_=rs[:, :W], func=AF.Ln))
            p.op("act", [rs], [rs], lambda e: e.activation(out=rs[:, :W], in_=rs[:, :W], func=AF.Exp, scale=-0.5))
            p.op("dve", [cy, rs], [dst], lambda e: e.scalar_tensor_tensor(
                out=dst[:, :W], in0=cy[:, j, :W], scalar=scl, in1=rs[:, :W], op0=ALU.mult, op1=ALU.mult))
        yield 0
        for src_t, src_ap, dst in ((kn, lambda n: kn[:, n * 64:(n + 1) * 64], ktok), (cy, lambda n: cy[:, 2, n * 64:(n + 1) * 64], vtok)):
            for g in range(0, nc_, 4):
                t = nextpp()
                tv = t[:64, :].rearrange("p (n d) -> p n d", d=128)
                for n in range(g, min(g + 4, nc_)):
                    p.op("pe", [src_t, idf], [t], lambda e: e.transpose(out=tv[:, n - g, :], in_=src_ap(n), identity=idf[:]))
                ne = min(4, nc_ - g)
                p.op("act", [t], [dst], lambda e: e.copy(out=dst[:, g:g + ne, :], in_=tv[:, :ne, :]))
        yield 0
        g_ = gval[:, d, c0:c0 + nc_, h]; b_ = bsig[:, d, c0:c0 + nc_, h]; gc_ = gc[:, d, c0:c0 + nc_, h]
        p.op("dve", [gval, tri], [Gm], lambda e: e.tensor_tensor(out=Gm[:, :nc_, :], in0=bcn(g_, nc_, 64), in1=tri[:, d, :].unsqueeze(1).to_broadcast([64, nc_, 64]), op=ALU.mult))
        p.op("pool", [bsig, idf], [Bm], lambda e: e.tensor_tensor(out=Bm[:, :nc_, :], in0=bcn(b_, nc_, 64), in1=idf[:64, :64].unsqueeze(1).to_broadcast([64, nc_, 64]), op=ALU.mult))
        t = nextpp()
        p.op("pe", [ones, Gm], [t], lambda e: e.matmul(t[:, :W], lhsT=ones[:64, :], rhs=Gm[:, :nc_, :].rearrange("p n i -> p (n i)"), start=True, stop=True))
        tv3 = t[:64, :W].rearrange("p (n i) -> p n i", i=64)
        p.op("dve", [t, gc], [diff], lambda e: e.tensor_tensor(out=diff[:, :nc_, :], in0=tv3, in1=bcn(gc_, nc_, 64), op=ALU.subtract))
        p.op("act", [t], [rs], lambda e: e.activation(out=rs[:, :W], in_=t[:, :W], func=AF.Exp))
        p.op("dve", [qn, rs], [hb.qdT], lambda e: e.tensor_tensor(out=hb.qdT[:, :nc_, :].rearrange("p n i -> p (n i)"), in0=qn[:, :W], in1=rs[:, :W], op=ALU.mult))
        t = nextpp()
        p.op("pe", [ones, Bm], [t], lambda e: e.matmul(t[:64, :W], lhsT=ones[:64, :64], rhs=Bm[:, :nc_, :].rearrange("p n i -> p (n i)"), start=True, stop=True))
        p.op("act", [t], [bbc], lambda e: e.copy(out=bbc[:, :nc_, :].rearrange("p n i -> p (n i)"), in_=t[:64, :W]))
        yield 0
        p.op("dve", [diff, nm], [DT], lambda e: e.tensor_tensor(out=DT[:, :nc_, :], in0=diff[:, :nc_, :], in1=nm[:, 2 * d, :].unsqueeze(1).to_broadcast([64, nc_, 64]), op=ALU.add))
        p.op("act", [DT], [DT], lambda e: e.activation(out=DT[:, :nc_, :], in_=DT[:, :nc_, :], func=AF.Exp))
        p.op("dve", [diff, nm], [Ds2], lambda e: e.scalar_tensor_tensor(
            out=Ds2[:, :nc_, :], in0=diff[:, :nc_, :], scalar=-1.0, in1=nm[:, 2 * (1 - d) + 1, :].unsqueeze(1).to_broadcast([64, nc_, 64]), op0=ALU.mult, op1=ALU.add))
        p.op("act", [Ds2], [Ds2], lambda e: e.activation(out=Ds2[:, :nc_, :], in_=Ds2[:, :nc_, :], func=AF.Exp))
        yield 0
        t = nextpp(); tv3 = t[:64, :W].rearrange("p (n i) -> p n i", i=64)
        for n in range(nc_):
            p.op("pe", [kn], [t], lambda e: e.matmul(tv3[:, n, :], lhsT=kn[:, n * 64:(n + 1) * 64], rhs=kn[:, n * 64:(n + 1) * 64], start=True, stop=True))
        p.op("act", [t], [mkk], lambda e: e.copy(out=mkk[:, :nc_, :], in_=tv3))
        t = nextpp(); tq3 = t[:64, :W].rearrange("p (n i) -> p n i", i=64)
        for n in range(nc_):
            p.op("pe", [kn, qn], [t], lambda e: e.matmul(tq3[:, n, :], lhsT=kn[:, n * 64:(n + 1) * 64], rhs=qn[:, n * 64:(n + 1) * 64], start=True, stop=True))
        p.op("dve", [t, DT], [hb.attnT], lambda e: e.tensor_tensor(out=hb.attnT[:, :nc_, :], in0=tq3, in1=DT[:, :nc_, :], op=ALU.mult))
        yield 0
        p.op("dve", [mkk, DT], [LT], lambda e: e.tensor_tensor(out=LT[:, :nc_, :], in0=mkk[:, :nc_, :], in1=DT[:, :nc_, :], op=ALU.mult))
        p.op("pool", [LT, st01], [LT], lambda e: e.tensor_tensor(out=LT[:, :nc_, :], in0=LT[:, :nc_, :], in1=st01[:, d, :].unsqueeze(1).to_broadcast([64, nc_, 64]), op=ALU.mult))
        p.op("dve", [LT, bbc], [LT], lambda e: e.tensor_tensor(out=LT[:, :nc_, :], in0=LT[:, :nc_, :], in1=bbc[:, :nc_, :], op=ALU.mult))
        p.op("pool", [mkk, Ds2], [Lm], lambda e: e.tensor_tensor(out=Lm[:, :nc_, :], in0=mkk[:, :nc_, :], in1=Ds2[:, :nc_, :], op=ALU.mult))
        p.op("dve", [Lm, bsig], [Lm], lambda e: e.tensor_tensor(out=Lm[:, :nc_, :], in0=Lm[:, :nc_, :], in1=bcn(b_, nc_, 64), op=ALU.mult))
        kb = kbs[par]; vb = vbs[par]
        p.op("pool", [ktok, bsig], [kb], lambda e: e.tensor_tensor(out=kb[:, :nc_, :], in0=ktok[:, :nc_, :], in1=bcn(b_, nc_, 128), op=ALU.mult))
        p.op("dve", [kb, egc], [kb], lambda e: e.tensor_tensor(out=kb[:, :nc_, :], in0=kb[:, :nc_, :], in1=bcn(egc[:, d, c0:c0 + nc_, h], nc_, 128), op=ALU.mult))
        p.op("pool", [vtok, bsig], [vb], lambda e: e.tensor_tensor(out=vb[:, :nc_, :], in0=vtok[:, :nc_, :], in1=bcn(b_, nc_, 128), op=ALU.mult))
        p.op("dve", [ktok, ekd], [hb.kdec], lambda e: e.tensor_tensor(out=hb.kdec[:, :nc_, :], in0=ktok[:, :nc_, :], in1=bcn(ekd[:, d, c0:c0 + nc_, h], nc_, 128), op=ALU.mult))
        yield 0
        yield 1
        R = Rr[0]
        p.op("dve", [LT, idf], [R], lambda e: e.scalar_tensor_tensor(
            out=R[:, :nc_, :], in0=LT[:, :nc_, :], scalar=-1.0, in1=idf[:64, :64].unsqueeze(1).to_broadcast([64, nc_, 64]), op0=ALU.mult, op1=ALU.add))
        p.op("act", [LT], [LTb], lambda e: e.copy(out=LTb[:, :nc_, :], in_=LT[:, :nc_, :]))
        p.op("act", [Lm], [Lmb], lambda e: e.copy(out=Lmb[:, :nc_, :], in_=Lm[:, :nc_, :]))
        p.op("pool", [R], [Rb[0]], lambda e: e.tensor_copy(out=Rb[0][:, :nc_, :], in_=R[:, :nc_, :]))

    def chain_gen(nc_):
        W = nc_ * 64
        R = Rr[0]; Rbc = Rb[0]
        X, XT = LTb, Lmb
        for it in range(5):
            Xn, XTn = Xa[it % 2], Xb[it % 2]
            t = psC[ci[0] % 4]; ci[0] += 1
            t3 = t[:64, :W].rearrange("p (n i) -> p n i", i=64)
            for n in range(nc_):
                p.op("pe", [X, XT], [t], lambda e: e.matmul(t3[:, n, :], lhsT=XT[:, n, :], rhs=X[:, n, :], start=True, stop=True))
            tb_ = psC[ci[0] % 4]; ci[0] += 1
            t3b = tb_[:64, :W].rearrange("p (n i) -> p n i", i=64)
            for n in range(nc_):
                p.op("pe", [X, XT], [tb_], lambda e: e.matmul(t3b[:, n, :], lhsT=X[:, n, :], rhs=XT[:, n, :], start=True, stop=True))
            yield 0
            p.op("act", [t], [Xn], lambda e: e.copy(out=Xn[:, :nc_, :], in_=t3))
            p.op("act", [tb_], [XTn], lambda e: e.copy(out=XTn[:, :nc_, :], in_=t3b))
            tc_ = psC[ci[0] % 4]; ci[0] += 1
            t3c = tc_[:64, :W].rearrange("p (n i) -> p n i", i=64)
            for n in range(nc_):
                p.op("pe", [XTn, Rbc], [tc_], lambda e: e.matmul(t3c[:, n, :], lhsT=XTn[:, n, :], rhs=Rbc[:, n, :], start=True, stop=True))
            yield 0
            Rn = Rr[(it + 1) % 2]
            p.op("dve", [tc_, R], [Rn], lambda e: e.tensor_tensor(out=Rn[:, :nc_, :], in0=t3c, in1=R[:, :nc_, :], op=ALU.add))
            if it < 4:
                Rbn = Rb[(it + 1) % 2]
                p.op("act", [Rn], [Rbn], lambda e: e.copy(out=Rbn[:, :nc_, :], in_=Rn[:, :nc_, :]))
                Rbc = Rbn
            R = Rn; X, XT = Xn, XTn

    def tail(h, nc_, par):
        hb = H[h]
        W = nc_ * 64
        kb = kbs[par]; vb = vbs[par]
        AinvT = Rr[1]
        t = psC[ci[0] % 4]; ci[0] += 1
        tw3 = t[:, :W].rearrange("p (n i) -> p n i", i=64)
        for n in range(nc_):
            p.op("pe", [kb, AinvT], [t], lambda e: e.matmul(tw3[:, n, :], lhsT=kb[:, n, :], rhs=AinvT[:, n, :], start=True, stop=True))
        p.op("act", [t], [hb.wT], lambda e: e.copy(out=hb.wT[:, :nc_, :], in_=tw3))
        for g in range(0, nc_, 4):
            t = psC[ci[0] % 4]; ci[0] += 1
            tu3 = t[:64, :].rearrange("p (n d) -> p n d", d=128)
            for n in range(g, min(g + 4, nc_)):
                p.op("pe", [AinvT, vb], [t], lambda e: e.matmul(tu3[:, n - g, :], lhsT=AinvT[:, n, :], rhs=vb[:, n, :], start=True, stop=True))
            ne = min(4, nc_ - g)
            p.op("dve", [t], [hb.u], lambda e: e.tensor_copy(out=hb.u[:, g:g + ne, :], in_=tu3[:, :ne, :]))

    def run_window(d, c0, nc_):
        def drain(g, until_final=False):
            for v in g:
                if until_final and v == 1:
                    return False
            return True
        fr = front_gen(0, d, c0, nc_, 0)
        drain(fr)
        for i in range(GH):
            ch = chain_gen(nc_)
            nf = front_gen(i + 1, d, c0, nc_, (i + 1) % 2) if i + 1 < GH else None
            nf_done = nf is None
            ch_done = False
            while not ch_done:
                try:
                    next(ch)
                except StopIteration:
                    ch_done = True
                if not nf_done:
                    try:
                        for _ in range(1):
                            v = next(nf)
                            if v == 1:
                                nf_done = True
                                break
                    except StopIteration:
                        nf_done = True; nf = None
            tail(i, nc_, i % 2)
            if nf is not None:
                drain(nf)

    def scan_step(h, d, c0, n):
        hb = H[h]
        cidx = c0 + n
        p.op("pe", [hb.wT, hb.Sb], [hb.ps1], lambda e: e.matmul(hb.ps1[:], lhsT=hb.wT[:, n, :], rhs=hb.Sb[:], start=True, stop=True))
        p.op("dve", [hb.u, hb.ps1], [hb.vn], lambda e: e.tensor_tensor(out=hb.vn[:], in0=hb.u[:, n, :], in1=hb.ps1[:], op=ALU.subtract))
        p.op("pe", [hb.Sb, hb.qdT], [hb.psO], lambda e: e.matmul(hb.psO[:], lhsT=hb.Sb[:], rhs=hb.qdT[:, n, :], start=True, stop=False))
        p.op("pe", [hb.vn, hb.attnT], [hb.psO], lambda e: e.matmul(hb.psO[:], lhsT=hb.vn[:], rhs=hb.attnT[:, n, :], start=False, stop=True))
        p.op("pe", [hb.kdec, hb.vn], [hb.psS], lambda e: e.matmul(hb.psS[:], lhsT=hb.kdec[:, n, :], rhs=hb.vn[:], start=True, stop=True))
        p.op("dve", [hb.S, egl, hb.psS], [hb.S], lambda e: e.scalar_tensor_tensor(
            out=hb.S[:], in0=hb.S[:], scalar=egl[:, d, cidx, h:h + 1], in1=hb.psS[:], op0=ALU.mult, op1=ALU.add))
        p.op("act", [hb.S], [hb.Sb], lambda e: e.copy(out=hb.Sb[:], in_=hb.S[:]))
        p.op("act", [hb.psO], [hb.oT], lambda e: e.copy(out=hb.oT[:, n, :], in_=hb.psO[:]))

    if dbg < 2:
        run_window(0, 4, 8)
        hb = H[0]
        C.dbg = [(hb.u, hb.u[:].rearrange("p n d -> p (n d)"), [64, 1024]), (Rr[1], Rr[1][:].rearrange("p n d -> p (n d)"), [64, 512]),
                 (LT, LT[:].rearrange("p n d -> p (n d)"), [64, 512]), (ktok, ktok[:].rearrange("p n d -> p (n d)"), [64, 1024])]
        return
    for d in range(2):
        for h in range(GH):
            hb = H[h]
            p.op("pool", [], [hb.S], lambda e: e.memset(hb.S[:], 0.0))
            p.op("pool", [], [hb.Sb], lambda e: e.memset(hb.Sb[:], 0.0))
        for (c0, nc_) in gdn_windows(d):
            run_window(d, c0, nc_)
            p.barrier()
            order = range(nc_) if d == 0 else range(nc_ - 1, -1, -1)
            for n in order:
                for h in range(GH):
                    scan_step(h, d, c0, n)
            for h in range(GH):
                hb = H[h]
                p.dma("sp" if h % 2 else "act", C.oTd[d][h], C.oTd[d][h][:, c0 * 64:(c0 + nc_) * 64], hb.oT, hb.oT[:, :nc_, :].rearrange("p n i -> p (n i)"))
            p.barrier()
    p.barrier()
    p.release(mk)
    mk = p.mark()
    gn2 = p.sb("gn2G", [128, 1]); p.dma("sp", gn2, gn2[:], C.gnorm, C.gnorm.ap[l])
    ones2 = p.sb("ones2G", [128, 128]); p.op("pool", [], [ones2], lambda e: e.memset(ones2[:], 1.0))
    of = [p.sb(f"ofG{i}", [128, TA]) for i in range(2)]; ob_ = [p.sb(f"obG{i}", [128, TA]) for i in range(2)]
    zz = [p.sb(f"zzG{i}", [128, TA]) for i in range(2)]
    sq2 = [p.sb(f"sq2G{i}", [128, 512]) for i in range(2)]; r2 = [p.sb(f"r2G{i}", [128, 512]) for i in range(2)]
    om = [p.sb(f"omG{i}", [128, TA], BF16) for i in range(2)]
    pq = [p.ps(f"pqG{i}", [128, 512]) for i in range(2)]
    for h in range(GH):
        a, b_, z_, o_ = of[h % 2], ob_[h % 2], zz[h % 2], om[h % 2]
        p.dma("sp", a, a[:], C.oTd[0][h], C.oTd[0][h][:])
        p.dma("act", b_, b_[:], C.oTd[1][h], C.oTd[1][h][:])
        p.dma("sp", z_, z_[:], C.uT[18 + h], C.uT[18 + h][:])
        p.op("pool", [a, b_], [a], lambda e: e.tensor_tensor(out=a[:], in0=a[:], in1=b_[:], op=ALU.add))
        p.op("act", [z_], [z_], lambda e: e.activation(out=z_[:], in_=z_[:], func=AF.Silu))
        for blk in range(9):
            c0 = blk * 512; w = min(512, TA - c0)
            s_, r_ = sq2[blk % 2], r2[blk % 2]; t = pq[blk % 2]
            p.op("pool", [a], [s_], lambda e: e.tensor_tensor(out=s_[:, :w], in0=a[:, c0:c0 + w], in1=a[:, c0:c0 + w], op=ALU.mult))
            p.op("pe", [ones2, s_], [t], lambda e: e.matmul(t[:, :w], lhsT=ones2[:], rhs=s_[:, :w], start=True, stop=True))
            p.op("dve", [t], [r_], lambda e: e.tensor_scalar(out=r_[:, :w], in0=t[:, :w], scalar1=1.0 / 128, scalar2=EPS, op0=ALU.mult, op1=ALU.add))
            p.op("act", [r_], [r_], lambda e: e.activation(out=r_[:, :w], in_=r_[:, :w], func=AF.Ln))
            p.op("act", [r_], [r_], lambda e: e.activation(out=r_[:, :w], in_=r_[:, :w], func=AF.Exp, scale=-0.5))
            p.op("dve", [a, gn2, r_], [r_], lambda e: e.scalar_tensor_tensor(
                out=r_[:, :w], in0=a[:, c0:c0 + w], scalar=gn2[:, 0:1], in1=r_[:, :w], op0=ALU.mult, op1=ALU.mult))
            p.op("dve", [r_, z_], [o_], lambda e: e.tensor_tensor(out=o_[:, c0:c0 + w], in0=r_[:, :w], in1=z_[:, c0:c0 + w], op=ALU.mult))
        p.dma("sp", C.mixT[h], C.mixT[h][:], o_, o_[:])
    p.barrier()
    p.release(mk)


def declare_scratch2(p, C):
    C.h2 = p.dram("h2_s", [TA, D], BF16)
    C.h2v = [C.h2.view((slice(i * 128, (i + 1) * 128), slice(None))) for i in range(NT)]
    C.affT = p.dram("affT_s", [NE, TA], F32)
    C.acc = p.dram("acc_s", [TA, D], F32)


def stage_D(p, C, l, with_ctx=True):
    mk = p.mark()
    wo = p.sb("woD", [128, 16, D], BF16)
    wov = [wo.view((slice(None), slice(q * 4, (q + 1) * 4), slice(None))) for q in range(4)]
    for q in range(4):
        p.dma("pool", wov[q], wov[q][:], C.w_out, C.w_out.ap[l, q * 512:(q + 1) * 512, :].rearrange("(k p) n -> p k n", p=128))
    wr = p.sb("wrD", [128, 16, NE], BF16)
    p.dma("pool", wr, wr[:], C.w_r, C.w_r.ap[l].rearrange("(k p) e -> p k e", p=128))
    idf = p.sb("idfD", [128, 128]); idb = p.sb("idbD", [128, 128], BF16)
    p.dma("sp", idf, idf[:], C.ident, C.ident[:])
    p.op("dve", [idf], [idb], lambda e: e.tensor_copy(out=idb[:], in_=idf[:]))
    ones16 = p.sb("ones16D", [NE, NE]); p.op("pool", [], [ones16], lambda e: e.memset(ones16[:], 1.0))
    expT = p.sb("expTD", [NE, TA])
    g2 = p.sb("g2D", [128, D]); gs = p.sb("gsD", [128, D]); sh = p.sb("shD", [128, D]); n2 = p.sb("n2D", [128, D])
    mx = p.sb("mxD", [128, 16, 1024], BF16)
    mxv = [mx.view((slice(None), c, slice(None))) for c in range(16)]
    hb = [p.sb(f"hbD{i}", [128, D]) for i in range(2)]
    yb = [p.sb(f"ybD{i}", [128, D]) for i in range(2)]
    ab = [p.sb(f"abD{i}", [128, D], BF16) for i in range(2)]
    h2T = [p.sb(f"h2TD{i}", [128, 16, 128], BF16) for i in range(2)]
    ss = [p.sb(f"ssD{i}", [128, 2]) for i in range(2)]
    tmp = [p.sb(f"tmpD{i}", [128, 512]) for i in range(2)]
    pu = [p.ps(f"puD{i}", [128, 512]) for i in range(4)]
    ptr = [p.ps(f"ptrD{i}", [128, 4, 128], BF16) for i in range(2)]
    pr = [p.ps(f"prD{i}", [NE, 128]) for i in range(2)]
    bc_load(p, "sp", n2, n2[:], C.norm2, C.norm2.ap[l:l + 1, :])
    groups = [(0, [0, 1])] + [(1, list(range(2 + 8 * g, 10 + 8 * g))) for g in range(4)]
    pc = 0; tc_ = 0; tcount = 0
    pending = [None]
    last_kind = None
    for kind, tiles in groups:
        if kind == 0 and not with_ctx:
            continue
        if kind != last_kind:
            last_kind = kind
            bc_load(p, "sp", g2, g2[:], C.mod, modrow(C, l, kind, 2))
            bc_load(p, "act", gs, gs[:], C.mod, modrow(C, l, kind, 4))
            bc_load(p, "sp", sh, sh[:], C.mod, modrow(C, l, kind, 3))
            p.op("dve", [gs, n2], [gs], lambda e: e.scalar_tensor_tensor(out=gs[:], in0=gs[:], scalar=1.0, in1=n2[:], op0=ALU.add, op1=ALU.mult))
        g0 = tiles[0] * 128; gw = len(tiles) * 128
        for c in range(16):
            p.dma("sp" if c % 2 else "act", mxv[c], mxv[c][:, :gw], C.mixT[c], C.mixT[c][:, g0:g0 + gw])
        for tj, ti in enumerate(tiles):
            ht = hb[tcount % 2]; y_ = yb[tcount % 2]; a_ = ab[tcount % 2]; s_ = ss[tcount % 2]; hT = h2T[tcount % 2]
            tcount += 1
            p.dma("sp", ht, ht[:], C.hv[ti], C.hv[ti][:])
            for k in range(16):
                for nb in range(4):
                    p.op("pe", [mxv[k], wov[k // 4]], [pu[nb]], lambda e: e.matmul(
                        pu[nb][:], lhsT=mx[:, k, tj * 128:(tj + 1) * 128], rhs=wo[:, k, nb * 512:(nb + 1) * 512], start=(k == 0), stop=(k == 15)))
            if pending[0] is not None:
                pending[0](); pending[0] = None
            for nb in range(4):
                t_ = tmp[nb % 2]
                p.op("dve", [pu[nb], g2], [t_], lambda e: e.tensor_tensor(out=t_[:], in0=pu[nb][:], in1=g2[:, nb * 512:(nb + 1) * 512], op=ALU.mult))
                p.op("pool", [t_, ht], [ht], lambda e: e.tensor_tensor(out=ht[:, nb * 512:(nb + 1) * 512], in0=ht[:, nb * 512:(nb + 1) * 512], in1=t_[:], op=ALU.add))
            p.dma("sp", C.hv[ti], C.hv[ti][:], ht, ht[:])
            p.op("pool", [], [s_], lambda e: e.memset(s_[:], 0.0))
            p.op("act", [ht], [y_, s_], lambda e: e.activation(out=y_[:], in_=ht[:], func=AF.Square, accum_out=s_[:, 0:1]))
            p.op("dve", [s_], [s_], lambda e: e.tensor_scalar(out=s_[:, 1:2], in0=s_[:, 0:1], scalar1=1.0 / D, scalar2=EPS, op0=ALU.mult, op1=ALU.add))
            p.op("act", [s_], [s_], lambda e: e.sqrt(out=s_[:, 1:2], in_=s_[:, 1:2]))
            p.op("dve", [s_], [s_], lambda e: e.reciprocal(out=s_[:, 1:2], in_=s_[:, 1:2]))
            p.op("dve", [ht, s_, gs], [y_], lambda e: e.scalar_tensor_tensor(out=y_[:], in0=ht[:], scalar=s_[:, 1:2], in1=gs[:], op0=ALU.mult, op1=ALU.mult))
            p.op("pool", [y_, sh], [a_], lambda e: e.tensor_tensor(out=a_[:], in0=y_[:], in1=sh[:], op=ALU.add))
            p.dma("act", C.h2v[ti], C.h2v[ti][:], a_, a_[:])

            def tail(a_=a_, hT=hT, ti=ti, pq=pr[tcount % 2]):
                nonlocal tc_
                for k4 in range(4):
                    pt = ptr[tc_ % 2]; tc_ += 1
                    for kk in range(4):
                        k = k4 * 4 + kk
                        p.op("pe", [a_, idb], [pt], lambda e: e.transpose(out=pt[:, kk, :], in_=a_[:, k * 128:(k + 1) * 128], identity=idb[:]))
                    p.op("act", [pt], [hT], lambda e: e.copy(out=hT[:, k4 * 4:(k4 + 1) * 4, :], in_=pt[:]))
                for k in range(16):
                    p.op("pe", [wr, hT], [pq], lambda e: e.matmul(pq[:], lhsT=wr[:, k, :], rhs=hT[:, k, :], start=(k == 0), stop=(k == 15)))
                p.op("act", [pq], [expT], lambda e: e.activation(out=expT[:, ti * 128:(ti + 1) * 128], in_=pq[:], func=AF.Exp))
            pending[0] = tail
    if pending[0] is not None:
        pending[0](); pending[0] = None
    t0_ = 0 if with_ctx else TC
    blk = t0_
    while blk < TA:
        w = min(512, TA - blk)
        pp = pu[pc % 4]; pc += 1
        t_ = tmp[pc % 2]
        p.op("pe", [ones16, expT], [pp], lambda e: e.matmul(pp[:NE, :w], lhsT=ones16[:], rhs=expT[:, blk:blk + w], start=True, stop=True))
        p.op("dve", [pp], [t_], lambda e: e.reciprocal(out=t_[:NE, :w], in_=pp[:NE, :w]))
        p.op("dve", [t_, expT], [expT], lambda e: e.tensor_tensor(out=expT[:, blk:blk + w], in0=expT[:, blk:blk + w], in1=t_[:NE, :w], op=ALU.mult))
        blk += w
    p.dma("sp", C.affT, C.affT[:, t0_:], expT, expT[:, t0_:])
    p.barrier()
    p.release(mk)


def stage_E(p, C, l, with_ctx=True):
    segs = [(TC, TL, 512)] + ([(0, TC, 32)] if with_ctx else [])
    nslots = sum(s[2] for s in segs)
    mk = p.mark()
    idf = p.sb("idfE", [128, 128]); idb = p.sb("idbE", [128, 128], BF16)
    p.dma("sp", idf, idf[:], C.ident, C.ident[:])
    p.op("dve", [idf], [idb], lambda e: e.tensor_copy(out=idb[:], in_=idf[:]))
    idxT = p.sb("idxTE", [128, 5, NE], I32); gateT = p.sb("gateTE", [128, 5, NE])
    mk0 = p.mark()
    zt = p.sb("ztE", [128, D]); p.op("pool", [], [zt], lambda e: e.memset(zt[:], 0.0))
    for i in range(NT):
        p.dma("sp" if i % 2 else "act", C.acc, C.acc.ap[i * 128:(i + 1) * 128, :], zt, zt[:])
    aff = p.sb("affE", [NE, TA]); work = p.sb("workE", [NE, TL])
    vals = p.sb("valsE", [NE, 544]); idxu = p.sb("idxuE", [NE, 544], U32); idxf = p.sb("idxfE", [NE, 544])
    ptk = p.ps("ptkE", [128, 2, NE])
    p.dma("sp", aff, aff[:], C.affT, C.affT[:])
    so = 0
    slot_tiles = []
    for (s0, n, cap) in segs:
        p.op("dve", [aff], [work], lambda e: e.tensor_copy(out=work[:, :n], in_=aff[:, s0:s0 + n]))
        for r in range(cap // 8):
            c0 = so + r * 8
            p.op("dve", [work], [vals], lambda e: e.max(out=vals[:, c0:c0 + 8], in_=work[:, :n]))
            p.op("dve", [vals, work], [idxu], lambda e: e.max_index(out=idxu[:, c0:c0 + 8], in_max=vals[:, c0:c0 + 8], in_values=work[:, :n]))
            p.op("dve", [vals, work], [work], lambda e: e.match_replace(out=work[:, :n], in_to_replace=vals[:, c0:c0 + 8], in_values=work[:, :n], imm_value=-1.0))
        p.op("dve", [idxu], [idxf], lambda e: e.tensor_copy(out=idxf[:, so:so + cap], in_=idxu[:, so:so + cap]))
        p.op("dve", [idxf], [idxf], lambda e: e.tensor_scalar(out=idxf[:, so:so + cap], in0=idxf[:, so:so + cap], scalar1=float(s0), scalar2=None, op0=ALU.add))
        for st in range((cap + 127) // 128):
            rows = min(128, cap - st * 128)
            ti = len(slot_tiles)
            c0 = so + st * 128
            p.op("pe", [idxf, idf], [ptk], lambda e: e.transpose(out=ptk[:rows, 0, :], in_=idxf[:, c0:c0 + rows], identity=idf[:NE, :NE]))
            p.op("pe", [vals, idf], [ptk], lambda e: e.transpose(out=ptk[:rows, 1, :], in_=vals[:, c0:c0 + rows], identity=idf[:NE, :NE]))
            p.op("dve", [ptk], [idxT], lambda e: e.tensor_copy(out=idxT[:rows, ti, :], in_=ptk[:rows, 0, :]))
            p.op("dve", [ptk], [gateT], lambda e: e.tensor_copy(out=gateT[:rows, ti, :], in_=ptk[:rows, 1, :]))
            slot_tiles.append((c0, rows, ti))
        so += cap
    p.barrier()
    p.release(mk0)
    gu = [[p.sb(f"guE{i}{j}", [128, 16, 512], BF16) for j in range(2)] for i in range(2)]
    wd = p.sb("wdE", [128, 8, D], BF16)
    wdv = [wd.view((slice(None), slice(q * 4, (q + 1) * 4), slice(None))) for q in range(2)]
    xsT = p.sb("xsTE", [128, 16, 544], BF16); hidT = p.sb("hidTE", [128, 8, 544], BF16)
    xs = [p.sb(f"xsE{i}", [128, D], BF16) for i in range(2)]
    yt = [p.sb(f"ytE{i}", [128, D]) for i in range(2)]
    sg = [p.sb(f"sgE{i}", [128, 512]) for i in range(2)]
    ptr = [p.ps(f"ptrE{i}", [128, 4, 128], BF16) for i in range(2)]
    X6 = [p.ps(f"pxE{i}", [128, 512]) for i in range(6)]
    blocks = [(0, 512)] + ([(512, 32)] if with_ctx else [])
    gi = 0; xi = 0; tci = 0; yi = 0; si = 0
    for e_ in range(NE):
        for (c0, rows, ti) in slot_tiles:
            x_ = xs[xi % 2]; xi += 1
            p.dma_indirect_gather(x_, x_[:rows, :], C.h2, C.h2.ap[:, :], idxT, idxT[:rows, ti, e_:e_ + 1])
            for k4 in range(4):
                pt = ptr[tci % 2]; tci += 1
                for kk in range(4):
                    k = k4 * 4 + kk
                    p.op("pe", [x_, idb], [pt], lambda e: e.transpose(out=pt[:, kk, :rows], in_=x_[:rows, k * 128:(k + 1) * 128], identity=idb[:rows, :rows]))
                p.op("act", [pt], [xsT], lambda e: e.copy(out=xsT[:, k4 * 4:(k4 + 1) * 4, c0:c0 + rows], in_=pt[:, :, :rows]))
        for half in range(2):
            g_, u_ = gu[gi % 2]; gi += 1
            for hh in range(2):
                p.dma("pool", g_, g_[:, hh * 8:(hh + 1) * 8, :], C.w_eg, C.w_eg.ap[l, e_, hh * 1024:(hh + 1) * 1024, half * 512:(half + 1) * 512].rearrange("(k p) n -> p k n", p=128))
                p.dma("pool", u_, u_[:, hh * 8:(hh + 1) * 8, :], C.w_eu, C.w_eu.ap[l, e_, hh * 1024:(hh + 1) * 1024, half * 512:(half + 1) * 512].rearrange("(k p) n -> p k n", p=128))
            for fcc in range(4):
                fc = half * 4 + fcc
                for (b0, bw) in blocks:
                    pG, pU = X6[2 * (si % 3)], X6[2 * (si % 3) + 1]; s_ = sg[si % 2]; si += 1
                    for k in range(16):
                        p.op("pe", [g_, xsT], [pG], lambda e: e.matmul(pG[:, :bw], lhsT=g_[:, k, fcc * 128:(fcc + 1) * 128], rhs=xsT[:, k, b0:b0 + bw], start=(k == 0), stop=(k == 15)))
                        p.op("pe", [u_, xsT], [pU], lambda e: e.matmul(pU[:, :bw], lhsT=u_[:, k, fcc * 128:(fcc + 1) * 128], rhs=xsT[:, k, b0:b0 + bw], start=(k == 0), stop=(k == 15)))
                    p.op("act", [pG], [s_], lambda e: e.activation(out=s_[:, :bw], in_=pG[:, :bw], func=AF.Silu))
                    p.op("dve", [s_, pU], [hidT], lambda e: e.tensor_tensor(out=hidT[:, fc, b0:b0 + bw], in0=s_[:, :bw], in1=pU[:, :bw], op=ALU.mult))
        for q in range(2):
            p.dma("pool", wdv[q], wdv[q][:], C.w_ed, C.w_ed.ap[l, e_, q * 512:(q + 1) * 512, :].rearrange("(k p) n -> p k n", p=128))
        for (c0, rows, ti) in slot_tiles:
            y_ = yt[yi % 2]; yi += 1
            for n2 in range(2):
                pA, pB = X6[2 * (si % 3)], X6[2 * (si % 3) + 1]; si += 1
                for fc in range(8):
                    for j, pb in enumerate((pA, pB)):
                        nb = n2 * 2 + j
                        p.op("pe", [hidT, wdv[fc // 4]], [pb], lambda e: e.matmul(
                            pb[:rows, :], lhsT=hidT[:, fc, c0:c0 + rows], rhs=wd[:, fc, nb * 512:(nb + 1) * 512], start=(fc == 0), stop=(fc == 7)))
                nb = n2 * 2
                p.op("act", [pA, gateT], [y_], lambda e: e.mul(out=y_[:rows, nb * 512:(nb + 1) * 512], in_=pA[:rows, :], mul=gateT[:rows, ti, e_:e_ + 1]))
                p.op("dve", [pB, gateT], [y_], lambda e: e.tensor_scalar(
                    out=y_[:rows, (nb + 1) * 512:(nb + 2) * 512], in0=pB[:rows, :], scalar1=gateT[:rows, ti, e_:e_ + 1], scalar2=None, op0=ALU.mult))
            p.dma_indirect_scatter_add(C.acc, C.acc.ap[:, :], y_, y_[:rows, :], idxT, idxT[:rows, ti, e_:e_ + 1])
    p.barrier()
    p.release(mk)


def stage_F(p, C, l, with_ctx=True, final=False):
    mk = p.mark()
    g5 = p.sb("g5F", [128, 2, D])
    for kind in range(2):
        bc_load(p, "sp", g5, g5[:, kind, :], C.mod, modrow(C, l, kind, 5))
    if final:
        fg = p.sb("fgF", [128, D]); bc_load(p, "act", fg, fg[:], C.fnorm, C.fnorm.ap[0:1, :])
    hb = [p.sb(f"hbF{i}", [128, D]) for i in range(2)]; ac = [p.sb(f"acF{i}", [128, D]) for i in range(2)]
    ss = [p.sb(f"ssF{i}", [128, 2]) for i in range(2)]
    for ti in range(NT):
        kind = 0 if ti < 2 else 1
        if kind == 0 and (final or not with_ctx):
            continue
        ht = hb[ti % 2]; a_ = ac[ti % 2]; s_ = ss[ti % 2]
        p.dma("sp", ht, ht[:], C.hv[ti], C.hv[ti][:])
        p.dma("act", a_, a_[:], C.acc, C.acc.ap[ti * 128:(ti + 1) * 128, :])
        p.op("pool", [a_, g5], [a_], lambda e: e.tensor_tensor(out=a_[:], in0=a_[:], in1=g5[:, kind, :], op=ALU.mult))
        p.op("dve", [ht, a_], [ht], lambda e: e.tensor_tensor(out=ht[:], in0=ht[:], in1=a_[:], op=ALU.add))
        if not final:
            p.dma("sp", C.hv[ti], C.hv[ti][:], ht, ht[:])
        else:
            p.op("pool", [], [s_], lambda e: e.memset(s_[:], 0.0))
            p.op("act", [ht], [a_, s_], lambda e: e.activation(out=a_[:], in_=ht[:], func=AF.Square, accum_out=s_[:, 0:1]))
            p.op("dve", [s_], [s_], lambda e: e.tensor_scalar(out=s_[:, 1:2], in0=s_[:, 0:1], scalar1=1.0 / D, scalar2=EPS, op0=ALU.mult, op1=ALU.add))
            p.op("act", [s_], [s_], lambda e: e.sqrt(out=s_[:, 1:2], in_=s_[:, 1:2]))
            p.op("dve", [s_], [s_], lambda e: e.reciprocal(out=s_[:, 1:2], in_=s_[:, 1:2]))
            p.op("dve", [ht, s_, fg], [a_], lambda e: e.scalar_tensor_tensor(out=a_[:], in0=ht[:], scalar=s_[:, 1:2], in1=fg[:], op0=ALU.mult, op1=ALU.mult))
            r0 = ti * 128 - TC
            p.dma("sp", C.out, C.out.ap[r0:r0 + 128, :], a_, a_[:])
    p.barrier()
    p.release(mk)


def host_consts():
    ident = np.eye(128, dtype=np.float32)
    rows = TL // 64
    row = np.repeat(np.arange(rows, dtype=np.float32), 64)
    col = np.tile(np.arange(64, dtype=np.float32), rows)
    inv = (10000.0 ** (-np.arange(16, dtype=np.float32) / 16)).astype(np.float32)
    ang = np.stack([row[:, None] * inv, col[:, None] * inv], 1).astype(np.float32)
    cosA = np.cos(ang); sinA = np.sin(ang)
    cosT = np.zeros((128, TL), np.float32); sinT = np.zeros((128, TL), np.float32)
    rot = np.zeros((128, 128), np.float32)
    for m in range(128):
        base = (m // 64) * 64; q = m % 64
        axis = q // 32; sel = (q % 32) // 16; pair = q % 16
        cosT[m] = cosA[:, axis, pair]
        sinT[m] = sinA[:, axis, pair] * (-1.0 if sel == 0 else 1.0)
        src = base + (q + 16 if sel == 0 else q - 16)
        rot[src, m] = 1.0
    idx = np.arange(64)
    tri = np.stack([(idx[:, None] <= idx[None, :]), (idx[:, None] >= idx[None, :])], 0).astype(np.float32)
    j = idx[:, None]; i = idx[None, :]
    nm = np.stack([np.where(i >= j, 0.0, NEG), np.where(i > j, 0.0, NEG), np.where(i <= j, 0.0, NEG), np.where(i < j, 0.0, NEG)], 0).astype(np.float32)
    return dict(ident=ident, cosT=np.ascontiguousarray(cosT), sinT=np.ascontiguousarray(sinT), rotm=rot, tri=tri, nmask=nm)


def make_inputs(inp, b, depth=DEPTH, l0=0):
    ls = slice(l0, l0 + depth)
    m = dict(host_consts())
    m["x"] = inp["x"][b]; m["ctx"] = inp["ctx"][b]
    sc = np.stack([inp["c"][b], inp["c_ctx"]], 0)
    m["cT"] = np.ascontiguousarray(sc.reshape(2, 16, 128).transpose(2, 1, 0))
    m["w_mod"] = inp["w_mod"][ls]
    m["b_mod"] = np.ascontiguousarray(np.broadcast_to(inp["b_mod"][ls][:, None, :], (depth, 2, 6 * D)))
    m["norm1"] = inp["norm1_g"][ls]; m["norm2"] = inp["norm2_g"][ls]
    m["w_in"] = inp["w_in"][ls]
    gc = inp["gdn_conv_w"][ls]
    m["gconv"] = np.ascontiguousarray(gc.reshape(depth, 3, 18, 128).transpose(0, 3, 2, 1))
    m["galog"] = np.ascontiguousarray(np.broadcast_to(inp["gdn_a_log"][ls].reshape(depth, 1, 12), (depth, 64, 12)))
    m["gdtb"] = np.ascontiguousarray(np.broadcast_to(inp["gdn_dt_bias"][ls].reshape(depth, 1, 12), (depth, 64, 12)))
    m["gnorm"] = np.ascontiguousarray(inp["gdn_norm_g"][ls].reshape(depth, 128, 1))
    sc_w = inp["sc_conv_w"][ls]
    m["sconv"] = np.ascontiguousarray(sc_w.reshape(depth, 3, 4, 128).transpose(0, 3, 2, 1))
    m["dlam"] = np.ascontiguousarray(np.broadcast_to(inp["diff_lambda"][ls][:, None], (depth, 128, 4, 64)))
    m["dnorm"] = np.ascontiguousarray(inp["diff_norm_g"][ls].reshape(depth, 128, 1))
    m["w_out"] = inp["w_out"][ls]; m["w_r"] = inp["w_router"][ls]
    m["w_eg"] = inp["w_e_gate"][ls]; m["w_eu"] = inp["w_e_up"][ls]; m["w_ed"] = inp["w_e_down"][ls]
    m["fnorm"] = inp["final_norm_g"].reshape(1, D)
    return m


def build_program(depth=DEPTH, debug_dump=False):
    nc = bass.Bass("TRN2", target_bir_lowering=False)
    p = P(nc)
    C = declare_io(p, depth)
    declare_scratch(p, C)
    declare_scratch2(p, C)
    stage_load(p, C)
    stage_M(p, C)
    outs = [C.out]
    for l in range(depth):
        last = (l == depth - 1) and not debug_dump
        with_ctx = not last
        stage_A(p, C, l)
        stage_SC(p, C, l, with_ctx)
        stage_ATT(p, C, l, with_ctx)
        stage_GDN(p, C, l)
        stage_D(p, C, l, with_ctx)
        stage_E(p, C, l, with_ctx)
        stage_F(p, C, l, with_ctx, final=False)
    if debug_dump:
        outs.append(dump(p, C.h, C.h[:], "d_h", [TA, D]))
        outs.append(dump(p, C.acc, C.acc[:], "d_acc", [TA, D]))
        outs.append(dump(p, C.affT, C.affT[:], "d_affT", [NE, TA]))
    stage_final(p, C)
    p.finish(outs)
    p.close()
    return nc, p


def stage_final(p, C):
    mk = p.mark()
    fg = p.sb("fgZ", [128, D]); bc_load(p, "act", fg, fg[:], C.fnorm, C.fnorm.ap[0:1, :])
    hb = [p.sb(f"hbZ{i}", [128, D]) for i in range(2)]; ac = [p.sb(f"acZ{i}", [128, D]) for i in range(2)]
    ss = [p.sb(f"ssZ{i}", [128, 2]) for i in range(2)]
    for ti in range(2, NT):
        ht = hb[ti % 2]; a_ = ac[ti % 2]; s_ = ss[ti % 2]
        p.dma("sp", ht, ht[:], C.hv[ti], C.hv[ti][:])
        p.op("pool", [], [s_], lambda e: e.memset(s_[:], 0.0))
        p.op("act", [ht], [a_, s_], lambda e: e.activation(out=a_[:], in_=ht[:], func=AF.Square, accum_out=s_[:, 0:1]))
        p.op("dve", [s_], [s_], lambda e: e.tensor_scalar(out=s_[:, 1:2], in0=s_[:, 0:1], scalar1=1.0 / D, scalar2=EPS, op0=ALU.mult, op1=ALU.add))
        p.op("act", [s_], [s_], lambda e: e.sqrt(out=s_[:, 1:2], in_=s_[:, 1:2]))
        p.op("dve", [s_], [s_], lambda e: e.reciprocal(out=s_[:, 1:2], in_=s_[:, 1:2]))
        p.op("dve", [ht, s_, fg], [a_], lambda e: e.scalar_tensor_tensor(out=a_[:], in0=ht[:], scalar=s_[:, 1:2], in1=fg[:], op0=ALU.mult, op1=ALU.mult))
        r0 = ti * 128 - TC
        p.dma("act", C.out, C.out.ap[r0:r0 + 128, :], a_, a_[:])
    p.barrier()
    p.release(mk)


def kernel(**inputs):
    inp = {k: np.asarray(v) for k, v in inputs.items()}
    nc, _ = get_prog("main", lambda: build_program(DEPTH))
    maps = [make_inputs(inp, b) for b in range(2)]
    res = launch(nc, maps)
    out = np.stack([np.asarray(res[b]["out"]) for b in range(2)], 0).astype(np.float32)
    return out
```

```python
import math
import numpy as np
import concourse.bass as bass
import concourse.mybir as mybir
from concourse.bass_utils import run_bass_kernel_spmd

F32 = mybir.dt.float32
BF16 = mybir.dt.bfloat16
I32 = mybir.dt.int32
U32 = mybir.dt.uint32
AF = mybir.ActivationFunctionType
ALU = mybir.AluOpType
AX = mybir.AxisListType


class T:
    __slots__ = ("ap", "w", "r", "name", "psum")

    def __init__(self, ap, name="", psum=False):
        self.ap = ap
        self.psum = psum
        self.w = None
        self.r = {}
        self.name = name

    def __getitem__(self, idx):
        return self.ap[idx]

    def view(self, idx, name=""):
        return T(self.ap[idx], name or self.name, self.psum)


class P:
    NDS = 8

    def __init__(self, nc):
        self.nc = nc
        self.eng = {"pe": nc.tensor, "act": nc.scalar, "dve": nc.vector, "pool": nc.gpsimd, "sp": nc.sync}
        self.sems = {}
        self.cnt = {}
        self._ctx = []
        for e in ("pe", "act", "dve", "pool"):
            self._mksem(e)
        for q in ("sp", "act", "pool"):
            for i in range(self.NDS):
                self._mksem(("d", q, i))
        self.dma_i = {"sp": 0, "act": 0, "pool": 0}
        self.seen = {e: {} for e in self.eng}
        self.n_instr = 0
        self.n_wait = 0

    def _mksem(self, key):
        name = "s_" + ("_".join(str(k) for k in key) if isinstance(key, tuple) else key)
        cm = self.nc.semaphore(name)
        s = cm.__enter__()
        self._ctx.append(cm)
        self.sems[key] = s
        self.cnt[key] = 0

    def _uniq(self, name):
        self._uid = getattr(self, "_uid", 0) + 1
        return f"{name}_{self._uid}"

    def sb(self, name, shape, dt=F32):
        name = self._uniq(name)
        cm = self.nc.sbuf_tensor(name, list(shape), dt)
        t = cm.__enter__()
        self._ctx.append(cm)
        return T(t, name)

    def ps(self, name, shape, dt=F32):
        name = self._uniq(name)
        cm = self.nc.psum_tensor(name, list(shape), dt)
        t = cm.__enter__()
        self._ctx.append(cm)
        return T(t, name, True)

    def dram(self, name, shape, dt, kind="Internal"):
        t = self.nc.dram_tensor(name, list(shape), dt, kind=kind)
        return T(t.ap(), name)

    def _wait(self, e, key, val):
        if val <= 0:
            return
        if self.seen[e].get(key, 0) >= val:
            return
        self.eng[e].wait_ge(self.sems[key], val)
        self.seen[e][key] = val
        self.n_wait += 1

    def _deps(self, e, mykey, reads, writes):
        for t in reads:
            if t.w is not None:
                self._wait(e, *t.w)
        for t in writes:
            if t.w is not None:
                self._wait(e, *t.w)
            for k, v in t.r.items():
                self._wait(e, k, v)

    def _mark(self, key, val, reads, writes):
        for t in reads:
            if t.r.get(key, 0) < val:
                t.r[key] = val
        for t in writes:
            t.w = (key, val)
            t.r = {}

    def op(self, e, reads, writes, fn):
        rp = [t for t in reads if t.psum and t not in writes]
        if rp:
            writes = list(writes) + rp
        self._deps(e, e, reads, writes)
        ins = fn(self.eng[e])
        self.cnt[e] += 1
        ins.then_inc(self.sems[e], 1)
        self._mark(e, self.cnt[e], reads, writes)
        self.n_instr += 1
        return ins

    def dma(self, q, out_t, out_ap, in_t, in_ap, **kw):
        i = self.dma_i[q] % self.NDS
        self.dma_i[q] += 1
        key = ("d", q, i)
        self._wait(q, key, self.cnt[key])
        self._deps(q, key, [in_t], [out_t])
        ins = self.eng[q].dma_start(out=out_ap, in_=in_ap, **kw)
        self.cnt[key] += 16
        ins.then_inc(self.sems[key], 16)
        self._mark(key, self.cnt[key], [in_t], [out_t])
        self.n_instr += 1
        return ins

    def _dma_generic(self, q, out_t, in_ts, issue):
        i = self.dma_i[q] % self.NDS
        self.dma_i[q] += 1
        key = ("d", q, i)
        self._wait(q, key, self.cnt[key])
        self._deps(q, key, in_ts, [out_t])
        ins = issue(self.eng[q])
        self.cnt[key] += 16
        ins.then_inc(self.sems[key], 16)
        self._mark(key, self.cnt[key], in_ts, [out_t])
        self.n_instr += 1
        return ins

    def dma_indirect_gather(self, out_t, out_ap, src_t, src_ap, idx_t, idx_ap):
        return self._dma_generic("pool", out_t, [src_t, idx_t], lambda g: g.indirect_dma_start(
            out=out_ap, out_offset=None, in_=src_ap, in_offset=bass.IndirectOffsetOnAxis(ap=idx_ap, axis=0)))

    def dma_indirect_scatter_add(self, dst_t, dst_ap, src_t, src_ap, idx_t, idx_ap):
        return self._dma_generic("pool", dst_t, [src_t, idx_t], lambda g: g.indirect_dma_start(
            out=dst_ap, out_offset=bass.IndirectOffsetOnAxis(ap=idx_ap, axis=0), in_=src_ap, in_offset=None,
            compute_op=ALU.add))

    def finish(self, out_ts, e="sp"):
        for t in out_ts:
            if t.w is not None:
                self._wait(e, *t.w)
        for key, v in self.cnt.items():
            self._wait(e, key, v)

    def mark(self):
        return len(self._ctx)

    def release(self, mark):
        while len(self._ctx) > mark:
            self._ctx.pop().__exit__(None, None, None)

    def barrier(self):
        for e in ("pe", "act", "dve", "pool", "sp"):
            for key, v in self.cnt.items():
                self._wait(e, key, v)

    def close(self):
        for cm in reversed(self._ctx):
            cm.__exit__(None, None, None)
        self._ctx = []


D = 2048
DEPTH = 4
EPS = 1e-6
N_IN = 6936
_PROGS = {}
N_LAUNCH = [0]


def get_prog(name, builder):
    if name not in _PROGS:
        _PROGS[name] = builder()
    return _PROGS[name]


def launch(nc, maps):
    N_LAUNCH[0] += 1
    res = run_bass_kernel_spmd(nc, maps, core_ids=list(range(len(maps))))
    return res.results


def rep128(v):
    return np.ascontiguousarray(np.broadcast_to(np.asarray(v, np.float32).reshape(1, -1), (128, v.size)))
TC = 256
TL = 4096
TA = TC + TL
NT = TA // 128
GH = 6
NCH = TA // 64
QKV0, Z0, B0c, A0c = 0, 2304, 3072, 3084
SC0 = 3096
CQ0, CK0, CV0 = 4632, 5400, 6168
NE = 16
FF = 1024
NEG = -1.0e30


class Ctx:
    pass


def declare_io(p, depth):
    C = Ctx()
    C.depth = depth
    ein = lambda n, s: p.dram(n, s, F32, kind="ExternalInput")
    C.x = ein("x", [TL, D]); C.ctx = ein("ctx", [TC, D])
    C.cT = ein("cT", [128, 16, 2])
    C.w_mod = ein("w_mod", [depth, D, 6 * D]); C.b_mod = ein("b_mod", [depth, 2, 6 * D])
    C.norm1 = ein("norm1", [depth, D]); C.norm2 = ein("norm2", [depth, D])
    C.w_in = ein("w_in", [depth, D, N_IN])
    C.gconv = ein("gconv", [depth, 128, 18, 3])
    C.galog = ein("galog", [depth, 64, 12]); C.gdtb = ein("gdtb", [depth, 64, 12])
    C.gnorm = ein("gnorm", [depth, 128, 1])
    C.sconv = ein("sconv", [depth, 128, 4, 3])
    C.dlam = ein("dlam", [depth, 128, 4, 64]); C.dnorm = ein("dnorm", [depth, 128, 1])
    C.w_out = ein("w_out", [depth, D, D]); C.w_r = ein("w_r", [depth, D, NE])
    C.w_eg = ein("w_eg", [depth, NE, D, FF]); C.w_eu = ein("w_eu", [depth, NE, D, FF]); C.w_ed = ein("w_ed", [depth, NE, FF, D])
    C.fnorm = ein("fnorm", [1, D])
    C.ident = ein("ident", [128, 128])
    C.cosT = ein("cosT", [128, TL]); C.sinT = ein("sinT", [128, TL])
    C.rotm = ein("rotm", [128, 128])
    C.tri = ein("tri", [2, 64, 64])
    C.nmask = ein("nmask", [4, 64, 64])
    C.out = p.dram("out", [TL, D], F32, kind="ExternalOutput")
    return C


def declare_scratch(p, C):
    C.h = p.dram("h_s", [TA, D], F32)
    C.hv = [C.h.view((slice(i * 128, (i + 1) * 128), slice(None))) for i in range(NT)]
    C.mod = p.dram("mod_s", [C.depth, 2, 6 * D], F32)
    C.uT = [p.dram(f"uT{c}", [128, TA], F32) for c in range(48)]
    C.uTv = [[t.view((slice(None), slice(hf * 2176, (hf + 1) * 2176))) for hf in range(2)] for t in C.uT]
    C.ba = p.dram("ba_s", [64, NCH, 24], F32)
    C.bav = [C.ba.view((slice(None), slice(hf * 34, (hf + 1) * 34), slice(None))) for hf in range(2)]
    C.cv = p.dram("cv_s", [TA, 768], BF16)
    C.cvv = [C.cv.view((slice(i * 128, (i + 1) * 128), slice(None))) for i in range(NT)]
    C.mixT = [p.dram(f"mixT{c}", [128, TA], BF16) for c in range(16)]


def bc_load(p, q, dst_t, dst_ap, src_t, src_row_ap, nparts=128):
    p.dma(q, dst_t, dst_ap, src_t, src_row_ap.partition_broadcast(nparts))


def stage_M(p, C):
    mk = p.mark()
    cT = p.sb("cT_s", [128, 16, 2]); sg = p.sb("sg", [128, 16, 2])
    bm = p.sb("bm_s", [2, 6 * D]); res = p.sb("resM", [2, 6 * D])
    wts = [p.sb(f"wtM{i}", [128, 4, 1024]) for i in range(3)]
    pss = [p.ps(f"psM{i}", [2, 512]) for i in range(4)]
    p.dma("sp", cT, cT[:], C.cT, C.cT[:])
    p.op("act", [cT], [sg], lambda e: e.activation(out=sg[:], in_=cT[:], func=AF.Sigmoid))
    p.op("dve", [cT, sg], [sg], lambda e: e.tensor_tensor(out=sg[:], in0=cT[:], in1=sg[:], op=ALU.mult))
    wi = 0
    for l in range(C.depth):
        p.dma("sp", bm, bm[:], C.b_mod, C.b_mod.ap[l])
        for ng in range(12):
            ps4 = pss[(ng % 2) * 2:(ng % 2) * 2 + 2]
            for kq in range(4):
                wt = wts[wi % 3]; wi += 1
                p.dma("sp" if wi % 2 else "act", wt, wt[:], C.w_mod,
                      C.w_mod.ap[l, kq * 512:(kq + 1) * 512, ng * 1024:(ng + 1) * 1024].rearrange("(k p) n -> p k n", p=128))
                for n in range(2):
                    for k in range(4):
                        p.op("pe", [sg, wt], [ps4[n]], lambda e: e.matmul(
                            ps4[n][:], lhsT=sg[:, kq * 4 + k, :], rhs=wt[:, k, n * 512:(n + 1) * 512],
                            start=(kq == 0 and k == 0), stop=(kq == 3 and k == 3)))
            for n in range(2):
                c0 = ng * 1024 + n * 512
                p.op("dve", [ps4[n], bm], [res], lambda e: e.tensor_tensor(
                    out=res[:, c0:c0 + 512], in0=ps4[n][:], in1=bm[:, c0:c0 + 512], op=ALU.add))
        p.dma("sp", C.mod, C.mod.ap[l], res, res[:])
    p.barrier()
    p.release(mk)


def modrow(C, l, kind, i):
    r = 1 if kind == 0 else 0
    return C.mod.ap[l, r:r + 1, i * D:(i + 1) * D]


def stage_A(p, C, l):
    mk = p.mark()
    gs = p.sb("gsA", [128, 2, D]); sh = p.sb("shA", [128, 2, D]); g1 = p.sb("g1A", [128, D])
    idf = p.sb("idfA", [128, 128]); idb = p.sb("idbA", [128, 128], BF16)
    aT = p.sb("aT", [128, 16, 2176], BF16)
    aTv = [aT.view((slice(None), slice(None), slice(i * 128, (i + 1) * 128))) for i in range(17)]
    p.dma("sp", idf, idf[:], C.ident, C.ident[:])
    p.op("dve", [idf], [idb], lambda e: e.tensor_copy(out=idb[:], in_=idf[:]))
    bc_load(p, "sp", g1, g1[:], C.norm1, C.norm1.ap[l:l + 1, :])
    for kind in range(2):
        bc_load(p, "act", gs, gs[:, kind, :], C.mod, modrow(C, l, kind, 1))
        bc_load(p, "sp", sh, sh[:, kind, :], C.mod, modrow(C, l, kind, 0))
    for kind in range(2):
        p.op("dve", [gs, g1], [gs], lambda e: e.scalar_tensor_tensor(
            out=gs[:, kind, :], in0=gs[:, kind, :], scalar=1.0, in1=g1[:], op0=ALU.add, op1=ALU.mult))
    for hf in range(2):
        mk1 = p.mark()
        hb = [p.sb(f"hbA{i}", [128, D]) for i in range(2)]
        yb = [p.sb(f"ybA{i}", [128, D]) for i in range(2)]
        ab = [p.sb(f"abA{i}", [128, D], BF16) for i in range(2)]
        ss = [p.sb(f"ssA{i}", [128, 2]) for i in range(2)]
        ptr = [p.ps(f"ptrA{i}", [128, 4, 128], BF16) for i in range(4)]
        tcount = 0
        for tt in range(17):
            ti = hf * 17 + tt
            kind = 0 if ti < 2 else 1
            ht = hb[tt % 2]; y_ = yb[tt % 2]; a_ = ab[tt % 2]; s_ = ss[tt % 2]
            p.dma("sp", ht, ht[:], C.hv[ti], C.hv[ti][:])
            p.op("pool", [], [s_], lambda e: e.memset(s_[:], 0.0))
            p.op("act", [ht], [y_, s_], lambda e: e.activation(out=y_[:], in_=ht[:], func=AF.Square, accum_out=s_[:, 0:1]))
            p.op("dve", [s_], [s_], lambda e: e.tensor_scalar(out=s_[:, 1:2], in0=s_[:, 0:1], scalar1=1.0 / D, scalar2=EPS, op0=ALU.mult, op1=ALU.add))
            p.op("act", [s_], [s_], lambda e: e.sqrt(out=s_[:, 1:2], in_=s_[:, 1:2]))
            p.op("dve", [s_], [s_], lambda e: e.reciprocal(out=s_[:, 1:2], in_=s_[:, 1:2]))
            p.op("dve", [ht, s_, gs], [y_], lambda e: e.scalar_tensor_tensor(
                out=y_[:], in0=ht[:], scalar=s_[:, 1:2], in1=gs[:, kind, :], op0=ALU.mult, op1=ALU.mult))
            p.op("pool", [y_, sh], [a_], lambda e: e.tensor_tensor(out=a_[:], in0=y_[:], in1=sh[:, kind, :], op=ALU.add))
            for k4 in range(4):
                pt = ptr[tcount % 4]; tcount += 1
                for kk in range(4):
                    k = k4 * 4 + kk
                    p.op("pe", [a_, idb], [pt], lambda e: e.transpose(out=pt[:, kk, :], in_=a_[:, k * 128:(k + 1) * 128], identity=idb[:]))
                p.op("act", [pt], [aTv[tt]], lambda e: e.copy(out=aT[:, k4 * 4:(k4 + 1) * 4, tt * 128:(tt + 1) * 128], in_=pt[:]))
        p.barrier()
        p.release(mk1)
        aTall = [aT] + aTv
        mk2 = p.mark()
        wb = [p.sb(f"wbA{i}", [128, 16, 512], BF16) for i in range(2)]
        wbv = [(w.view((slice(None), slice(0, 8), slice(None))), w.view((slice(None), slice(8, 16), slice(None)))) for w in wb]
        stg = [p.sb(f"stgA{i}", [128, 2176]) for i in range(4)]
        pu = [p.ps(f"puA{i}", [128, 512]) for i in range(8)]
        wcount = 0; setc = 0; scount = 0; ec = 0

        def load_w(c0, nw):
            nonlocal wcount
            wv = wbv[wcount % 2]; wcount += 1
            for half in range(2):
                p.dma("pool", wv[half], wv[half][:, :, :nw], C.w_in,
                      C.w_in.ap[l, half * 1024:(half + 1) * 1024, c0:c0 + nw].rearrange("(k p) n -> p k n", p=128))
            return wv

        def evac(pp, dst_t, dst_ap, src_ap):
            nonlocal ec
            ec += 1
            if ec % 2:
                p.op("act", [pp], [dst_t], lambda e: e.copy(out=dst_ap, in_=src_ap))
            else:
                p.op("dve", [pp], [dst_t], lambda e: e.tensor_copy(out=dst_ap, in_=src_ap))
        fm_cols = [QKV0 + 128 * i for i in range(24)] + [SC0 + 128 * i for i in range(12)] + [CQ0 + 128 * i for i in range(12)]
        for g4 in range(12):
            c0 = fm_cols[g4 * 4]
            wv = load_w(c0, 512)
            sts = [stg[cc] for cc in range(4)]
            for cc in range(4):
                bank = pu[(setc % 2) * 4:(setc % 2) * 4 + 4]; setc += 1
                for k in range(16):
                    for tb in range(4):
                        p.op("pe", aTall + [wv[k // 8]], [bank[tb]], lambda e: e.matmul(
                            bank[tb][:, :], lhsT=wv[k // 8][:, k % 8, cc * 128:(cc + 1) * 128], rhs=aT[:, k, tb * 512:(tb + 1) * 512], start=(k == 0), stop=(k == 15)))
                for tb in range(4):
                    evac(bank[tb], sts[cc], sts[cc][:, tb * 512:(tb + 1) * 512], bank[tb][:, :])
            bank = pu[(setc % 2) * 4:(setc % 2) * 4 + 4]; setc += 1
            for k in range(16):
                for cc in range(4):
                    p.op("pe", aTall + [wv[k // 8]], [bank[cc]], lambda e: e.matmul(
                        bank[cc][:, :128], lhsT=wv[k // 8][:, k % 8, cc * 128:(cc + 1) * 128], rhs=aT[:, k, 2048:2176], start=(k == 0), stop=(k == 15)))
            for cc in range(4):
                evac(bank[cc], sts[cc], sts[cc][:, 2048:2176], bank[cc][:, :128])
                ch = g4 * 4 + cc
                p.dma("sp" if cc % 2 else "act", C.uTv[ch][hf], C.uTv[ch][hf][:], sts[cc], sts[cc][:])
        for vg in range(2):
            nw = 512 if vg == 0 else 256
            wv = load_w(CV0 + vg * 512, nw)
            for t4 in range(0, 17, 4):
                tl = list(range(t4, min(t4 + 4, 17)))
                bank = pu[(setc % 2) * 4:(setc % 2) * 4 + 4]; setc += 1
                for k in range(16):
                    for j, tt in enumerate(tl):
                        p.op("pe", aTall + [wv[k // 8]], [bank[j]], lambda e: e.matmul(
                            bank[j][:, :nw], lhsT=aT[:, k, tt * 128:(tt + 1) * 128], rhs=wv[k // 8][:, k % 8, :nw], start=(k == 0), stop=(k == 15)))
                for j, tt in enumerate(tl):
                    ti = hf * 17 + tt
                    st = stg[scount % 4]; scount += 1
                    evac(bank[j], st, st[:, :nw], bank[j][:, :nw])
                    p.dma("pool", C.cvv[ti], C.cvv[ti][:, vg * 512:vg * 512 + nw], st, st[:, :nw])
        wv = load_w(B0c, 24)
        bst = p.sb("bstA", [64, 34, 24])
        for c4 in range(0, 34, 4):
            cl = list(range(c4, min(c4 + 4, 34)))
            bank = pu[(setc % 2) * 4:(setc % 2) * 4 + 4]; setc += 1
            for k in range(16):
                for j, cj in enumerate(cl):
                    p.op("pe", aTall + [wv[k // 8]], [bank[j]], lambda e: e.matmul(
                        bank[j][:64, :24], lhsT=aT[:, k, cj * 64:(cj + 1) * 64], rhs=wv[k // 8][:, k % 8, :24], start=(k == 0), stop=(k == 15)))
            for j, cj in enumerate(cl):
                p.op("dve", [bank[j]], [bst], lambda e: e.tensor_copy(out=bst[:, cj, :], in_=bank[j][:64, :24]))
        p.dma("sp", C.bav[hf], C.bav[hf][:], bst, bst[:])
        p.barrier()
        p.release(mk2)
    p.release(mk)


def stage_load(p, C):
    p.dma("sp", C.h, C.h.ap[0:TC, :], C.ctx, C.ctx[:])
    for i in range(4):
        p.dma("act" if i % 2 else "sp", C.h, C.h.ap[TC + i * 1024:TC + (i + 1) * 1024, :], C.x, C.x.ap[i * 1024:(i + 1) * 1024, :])
    p.barrier()


def dump(p, src_t, src_ap, name, shape, dt=F32):
    o = p.dram(name, shape, dt, kind="ExternalOutput")
    p.dma("sp", o, o[:], src_t, src_ap)
    return o


def stage_SC(p, C, l, with_ctx=True):
    mk = p.mark()
    cw = p.sb("cwSC", [128, 4, 3])
    p.dma("sp", cw, cw[:], C.sconv, C.sconv.ap[l])
    bufs = [[p.sb(f"sc{n}{i}", [128, TA]) for n in ("b", "c", "x", "y")] for i in range(2)]
    ob = [p.sb(f"scO{i}", [128, TA], BF16) for i in range(2)]
    segs = [(0, TC), (TC, TA)]
    for c in range(4):
        bg, cg, xi, y = bufs[c % 2]
        o_ = ob[c % 2]
        p.dma("sp", bg, bg[:], C.uT[24 + c], C.uT[24 + c][:])
        p.dma("act", cg, cg[:], C.uT[28 + c], C.uT[28 + c][:])
        p.dma("sp", xi, xi[:], C.uT[32 + c], C.uT[32 + c][:])
        p.op("dve", [cg, xi], [cg], lambda e: e.tensor_tensor(out=cg[:], in0=cg[:], in1=xi[:], op=ALU.mult))
        p.op("act", [cg, cw], [y], lambda e: e.mul(out=y[:], in_=cg[:], mul=cw[:, c, 1:2]))
        for (s0, s1) in segs:
            p.op("dve", [cg, cw, y], [y], lambda e: e.scalar_tensor_tensor(
                out=y[:, s0 + 1:s1], in0=cg[:, s0:s1 - 1], scalar=cw[:, c, 0:1], in1=y[:, s0 + 1:s1], op0=ALU.mult, op1=ALU.add))
            p.op("dve", [cg, cw, y], [y], lambda e: e.scalar_tensor_tensor(
                out=y[:, s0:s1 - 1], in0=cg[:, s0 + 1:s1], scalar=cw[:, c, 2:3], in1=y[:, s0:s1 - 1], op0=ALU.mult, op1=ALU.add))
        p.op("pool", [bg, y], [o_], lambda e: e.tensor_tensor(out=o_[:], in0=bg[:], in1=y[:], op=ALU.mult))
        p.dma("sp", C.mixT[6 + c], C.mixT[6 + c][:], o_, o_[:])
    p.barrier()
    p.release(mk)


def stage_ATT(p, C, l, with_ctx=True):
    lam_init = 0.8 - 0.6 * math.exp(-0.3 * l)
    mk = p.mark()
    onesb = p.sb("onesB", [128, 128], BF16)
    p.op("pool", [], [onesb], lambda e: e.memset(onesb[:], 1.0))
    onesf = p.sb("onesF", [128, 128])
    p.op("pool", [], [onesf], lambda e: e.memset(onesf[:], 1.0))
    accs = [p.sb(f"accA{i}", [128, 512]) for i in range(4)]
    rot = p.sb("rotA", [128, 128])
    p.dma("sp", rot, rot[:], C.rotm, C.rotm[:])
    cosT = p.sb("cosA", [128, TL]); sinT = p.sb("sinA", [128, TL])
    p.dma("sp", cosT, cosT[:], C.cosT, C.cosT[:])
    p.dma("act", sinT, sinT[:], C.sinT, C.sinT[:])
    dn = p.sb("dnA", [128, 1])
    p.dma("sp", dn, dn[:], C.dnorm, C.dnorm.ap[l])
    p.op("dve", [dn], [dn], lambda e: e.tensor_scalar(out=dn[:], in0=dn[:], scalar1=(1.0 - lam_init), scalar2=None, op0=ALU.mult))
    dl = p.sb("dlA", [128, 4, 64]); lw = p.sb("lwA", [128, 8])
    p.dma("sp", dl, dl[:], C.dlam, C.dlam.ap[l])
    p.op("dve", [dl], [dl], lambda e: e.tensor_tensor(out=dl[:, 0, :], in0=dl[:, 0, :], in1=dl[:, 1, :], op=ALU.mult))
    p.op("dve", [dl], [dl], lambda e: e.tensor_tensor(out=dl[:, 2, :], in0=dl[:, 2, :], in1=dl[:, 3, :], op=ALU.mult))
    p.op("dve", [dl], [lw], lambda e: e.reduce_sum(out=lw[:, 0:1], in_=dl[:, 0, :], axis=AX.X))
    p.op("dve", [dl], [lw], lambda e: e.reduce_sum(out=lw[:, 1:2], in_=dl[:, 2, :], axis=AX.X))
    p.op("act", [lw], [lw], lambda e: e.activation(out=lw[:, 2:4], in_=lw[:, 0:2], func=AF.Exp))
    p.op("dve", [lw], [lw], lambda e: e.tensor_tensor(out=lw[:, 4:5], in0=lw[:, 3:4], in1=lw[:, 2:3], op=ALU.subtract))
    p.op("dve", [lw], [lw], lambda e: e.tensor_scalar(out=lw[:, 5:6], in0=lw[:, 4:5], scalar1=-lam_init, scalar2=None, op0=ALU.add))
    neg_lam = lw
    xq = [p.sb(f"xqA{i}", [128, TA]) for i in range(2)]
    qTr = p.sb("qTrA", [128, TA], BF16); kTr = p.sb("kTrA", [128, TA], BF16)
    vsb = p.sb("vA", [128, NT, 128], BF16)
    tmp = [p.sb(f"tmpA{i}", [128, 512]) for i in range(2)]
    PT = [p.sb(f"PT{i}", [128, 512], BF16) for i in range(4)]
    ep = [p.sb(f"epA{i}", [128, 512]) for i in range(4)]
    sqb = p.sb("sqA", [128, 512], BF16)
    ost = [p.sb(f"ostA{i}", [128, 512], BF16) for i in range(2)]
    pS = [p.ps(f"pS{i}", [128, 512]) for i in range(3)]
    pO = [p.ps(f"pO{i}", [128, 512]) for i in range(2)]
    pD = [p.ps(f"pD{i}", [128, 512]) for i in range(2)]
    pR = p.ps("pR", [128, 512])
    sc_i = 0; pt_i = 0
    for h in range(GH):
        for which, (src, dst) in enumerate(((C.uT[36 + h], qTr), (C.uT[42 + h], kTr))):
            x_ = xq[which]
            p.dma("sp" if which == 0 else "act", x_, x_[:], src, src[:])
            p.op("pool", [x_], [dst], lambda e: e.tensor_copy(out=dst[:, 0:TC], in_=x_[:, 0:TC]))
            for qb in range(8):
                c0 = TC + qb * 512
                p.op("pe", [rot, x_], [pR], lambda e: e.matmul(pR[:], lhsT=rot[:], rhs=x_[:, c0:c0 + 512], start=True, stop=True))
                t_ = tmp[qb % 2]
                p.op("dve", [pR, sinT], [t_], lambda e: e.tensor_tensor(out=t_[:], in0=pR[:], in1=sinT[:, qb * 512:(qb + 1) * 512], op=ALU.mult))
                p.op("pool", [x_, cosT], [x_], lambda e: e.tensor_tensor(out=x_[:, c0:c0 + 512], in0=x_[:, c0:c0 + 512], in1=cosT[:, qb * 512:(qb + 1) * 512], op=ALU.mult))
                p.op("dve", [x_, t_], [dst], lambda e: e.tensor_tensor(out=dst[:, c0:c0 + 512], in0=x_[:, c0:c0 + 512], in1=t_[:], op=ALU.add))
        p.dma("sp", vsb, vsb[:], C.cv, C.cv.ap[:, h * 128:(h + 1) * 128].rearrange("(t p) d -> p t d", p=128))
        blocks = [(TC + qb * 512, 512, list(range(NT))) for qb in range(8)]
        if with_ctx:
            blocks.append((0, TC, [0, 1]))
        for bi, (q0, qw, ktiles) in enumerate(blocks):
            for m in range(2):
                po = pO[m]; pd = pD[m]
                ac = accs[(bi % 2) * 2 + m]
                nk = len(ktiles)

                def qk(ki):
                    nonlocal sc_i
                    kt = ktiles[ki]
                    ps_ = pS[sc_i % 3]; sc_i += 1
                    p.op("pe", [kTr, qTr], [ps_], lambda e: e.matmul(
                        ps_[:, :qw], lhsT=kTr[m * 64:(m + 1) * 64, kt * 128:(kt + 1) * 128], rhs=qTr[m * 64:(m + 1) * 64, q0:q0 + qw], start=True, stop=True))
                    return ps_
                pend = [qk(0)]
                if nk > 1:
                    pend.append(qk(1))
                for ki, kt in enumerate(ktiles):
                    ps_ = pend.pop(0)
                    if ki + 2 < nk:
                        pend.append(qk(ki + 2))
                    pt = PT[pt_i % 4]; pt_i += 1
                    p.op("act", [ps_], [pt], lambda e: e.activation(out=pt[:, :qw], in_=ps_[:, :qw], func=AF.Exp, scale=0.125))
                    p.op("pe", [vsb, pt], [po], lambda e: e.matmul(po[:, :qw], lhsT=vsb[:, kt, :], rhs=pt[:, :qw], start=(ki == 0), stop=(ki == nk - 1)))
                    if ki == 0:
                        p.op("dve", [pt], [ac], lambda e: e.tensor_copy(out=ac[:, :qw], in_=pt[:, :qw]))
                    else:
                        p.op("dve", [pt, ac], [ac], lambda e: e.tensor_tensor(out=ac[:, :qw], in0=ac[:, :qw], in1=pt[:, :qw], op=ALU.add))
                p.op("pe", [onesf, ac], [pd], lambda e: e.matmul(pd[:, :qw], lhsT=onesf[:], rhs=ac[:, :qw], start=True, stop=True))
            e0, e1, e2, e3 = ep
            p.op("dve", [pD[0]], [e0], lambda e: e.reciprocal(out=e0[:, :qw], in_=pD[0][:, :qw]))
            p.op("dve", [pO[0], e0], [e0], lambda e: e.tensor_tensor(out=e0[:, :qw], in0=pO[0][:, :qw], in1=e0[:, :qw], op=ALU.mult))
            p.op("dve", [pD[1]], [e1], lambda e: e.reciprocal(out=e1[:, :qw], in_=pD[1][:, :qw]))
            p.op("dve", [pO[1], e1], [e1], lambda e: e.tensor_tensor(out=e1[:, :qw], in0=pO[1][:, :qw], in1=e1[:, :qw], op=ALU.mult))
            p.op("dve", [e0, e1, neg_lam], [e2], lambda e: e.scalar_tensor_tensor(
                out=e2[:, :qw], in0=e1[:, :qw], scalar=neg_lam[:, 5:6], in1=e0[:, :qw], op0=ALU.mult, op1=ALU.add))
            p.op("pool", [e2], [sqb], lambda e: e.tensor_tensor(out=sqb[:, :qw], in0=e2[:, :qw], in1=e2[:, :qw], op=ALU.mult))
            p.op("pe", [onesb, sqb], [pR], lambda e: e.matmul(pR[:, :qw], lhsT=onesb[:], rhs=sqb[:, :qw], start=True, stop=True))
            p.op("dve", [pR], [e3], lambda e: e.tensor_scalar(out=e3[:, :qw], in0=pR[:, :qw], scalar1=1.0 / 128, scalar2=EPS, op0=ALU.mult, op1=ALU.add))
            p.op("act", [e3], [e3], lambda e: e.activation(out=e3[:, :qw], in_=e3[:, :qw], func=AF.Ln))
            p.op("act", [e3], [e3], lambda e: e.activation(out=e3[:, :qw], in_=e3[:, :qw], func=AF.Exp, scale=-0.5))
            o_ = ost[bi % 2]
            p.op("dve", [e2, e3, dn], [o_], lambda e: e.scalar_tensor_tensor(
                out=o_[:, :qw], in0=e2[:, :qw], scalar=dn[:, 0:1], in1=e3[:, :qw], op0=ALU.mult, op1=ALU.mult))
            p.dma("sp", C.mixT[10 + h], C.mixT[10 + h][:, q0:q0 + qw], o_, o_[:, :qw])
    p.barrier()
    p.release(mk)


INV_DT = F32


def gdn_windows(d):
    lat = [(4 + 8 * i, 8) for i in range(8)]
    if d == 0:
        return [(0, 4)] + lat
    return [(0, 4)] + lat[::-1]


def stage_GDN(p, C, l, dbg=99):
    mk = p.mark()
    C.oTd = getattr(C, "oTd", None) or [[p.dram(f"oTd{d}_{h}", [128, TA], F32) for h in range(GH)] for d in range(2)]
    idf = p.sb("idG", [128, 128]); p.dma("sp", idf, idf[:], C.ident, C.ident[:])
    tri = p.sb("triG", [64, 2, 64]); p.dma("sp", tri, tri[:], C.tri, C.tri.ap.rearrange("d p i -> p d i"))
    nm = p.sb("nmG", [64, 4, 64]); p.dma("act", nm, nm[:], C.nmask, C.nmask.ap.rearrange("d p i -> p d i"))
    st01 = p.sb("st01G", [64, 2, 64])
    for d in range(2):
        p.op("dve", [tri, idf], [st01], lambda e: e.tensor_tensor(out=st01[:, d, :], in0=tri[:, d, :], in1=idf[:64, :64], op=ALU.subtract))
    ones = p.sb("onesG", [128, 128]); p.op("pool", [], [ones], lambda e: e.memset(ones[:], 1.0))
    cw = p.sb("cwG", [128, 18, 3]); p.dma("sp", cw, cw[:], C.gconv, C.gconv.ap[l])
    gn = p.sb("gnG", [128, 1]); p.dma("sp", gn, gn[:], C.gnorm, C.gnorm.ap[l])
    if dbg == -3:
        C.dbg = [(st01, st01[:].rearrange("p a i -> p (a i)"), [64, 128]), (nm, nm[:].rearrange("p a i -> p (a i)"), [64, 256])]
        return
    ba = p.sb("baG", [64, NCH, 24]); p.dma("sp", ba, ba[:], C.ba, C.ba[:])
    alog = p.sb("alogG", [64, 12]); p.dma("act", alog, alog[:], C.galog, C.galog.ap[l])
    dtb = p.sb("dtbG", [64, 12]); p.dma("act", dtb, dtb[:], C.gdtb, C.gdtb.ap[l])
    NG = NCH * GH
    bsig = p.sb("bsigG", [64, 2, NCH, GH]); gval = p.sb("gvalG", [64, 2, NCH, GH])
    gc = p.sb("gcG", [64, 2, NCH, GH]); gtot = p.sb("gtotG", [64, 2, NCH, GH])
    egc = p.sb("egcG", [64, 2, NCH, GH]); ekd = p.sb("ekdG", [64, 2, NCH, GH]); egl = p.sb("eglG", [128, 2, NCH, GH])
    pp = [p.ps(f"ppG{i}", [128, 512]) for i in range(2)]
    psA = [p.ps(f"psA{h}", [128, 512]) for h in range(GH)]
    ppi = [0]

    def nextpp():
        t = pp[ppi[0] % 2]; ppi[0] += 1
        return t
    p.op("act", [alog], [alog], lambda e: e.activation(out=alog[:], in_=alog[:], func=AF.Exp))
    for d in range(2):
        bsl = ba[:, :, d * 6:(d + 1) * 6]
        asl = ba[:, :, 12 + d * 6:12 + (d + 1) * 6]
        p.op("act", [ba], [bsig], lambda e: e.activation(out=bsig[:, d], in_=bsl, func=AF.Sigmoid))
        p.op("dve", [ba, dtb], [gval], lambda e: e.tensor_tensor(out=gval[:, d], in0=asl, in1=dtb[:, d * 6:(d + 1) * 6].unsqueeze(1).to_broadcast([64, NCH, GH]), op=ALU.add))
        p.op("act", [gval], [gval], lambda e: e.activation(out=gval[:, d], in_=gval[:, d], func=AF.Exp))
        p.op("dve", [gval], [gval], lambda e: e.tensor_scalar(out=gval[:, d], in0=gval[:, d], scalar1=1.0, scalar2=None, op0=ALU.add))
        p.op("act", [gval], [gval], lambda e: e.activation(out=gval[:, d], in_=gval[:, d], func=AF.Ln))
        p.op("dve", [gval, alog], [gval], lambda e: e.scalar_tensor_tensor(
            out=gval[:, d], in0=gval[:, d], scalar=-1.0, in1=alog[:, d * 6:(d + 1) * 6].unsqueeze(1).to_broadcast([64, NCH, GH]), op0=ALU.mult, op1=ALU.mult))
        if dbg == -2:
            C.dbg = [(gval, gval[:].rearrange("p a n h -> p (a n h)"), [64, 2 * NG]), (bsig, bsig[:].rearrange("p a n h -> p (a n h)"), [64, 2 * NG])]
            return
        t = nextpp()
        p.op("pe", [tri, gval], [t], lambda e: e.matmul(t[:64, :NG], lhsT=tri[:, d, :], rhs=gval[:, d].rearrange("p n h -> p (n h)"), start=True, stop=True))
        p.op("dve", [t], [gc], lambda e: e.tensor_copy(out=gc[:, d].rearrange("p n h -> p (n h)"), in_=t[:64, :NG]))
        if dbg == -1:
            C.dbg = [(gc, gc[:].rearrange("p a n h -> p (a n h)"), [64, 2 * NG])]
            return
        t2 = nextpp()
        p.op("pe", [ones, gval], [t2], lambda e: e.matmul(t2[:, :NG], lhsT=ones[:64, :], rhs=gval[:, d].rearrange("p n h -> p (n h)"), start=True, stop=True))
        p.op("dve", [t2], [gtot], lambda e: e.tensor_copy(out=gtot[:, d].rearrange("p n h -> p (n h)"), in_=t2[:64, :NG]))
        p.op("act", [t2], [egl], lambda e: e.activation(out=egl[:, d].rearrange("p n h -> p (n h)"), in_=t2[:, :NG], func=AF.Exp))
        p.op("act", [gc], [egc], lambda e: e.activation(out=egc[:, d], in_=gc[:, d], func=AF.Exp))
        p.op("dve", [gtot, gc], [ekd], lambda e: e.tensor_tensor(out=ekd[:, d], in0=gtot[:, d], in1=gc[:, d], op=ALU.subtract))
        p.op("act", [ekd], [ekd], lambda e: e.activation(out=ekd[:, d], in_=ekd[:, d], func=AF.Exp))
    if dbg < 1:
        C.dbg = [(gval, gval[:].rearrange("p a n h -> p (a n h)"), [64, 2 * NG]), (gc, gc[:].rearrange("p a n h -> p (a n h)"), [64, 2 * NG])]
        return
    H = []
    for h in range(GH):
        hb = Ctx()
        hb.wT = p.sb(f"wT{h}", [128, 8, 64], BF16); hb.u = p.sb(f"u{h}", [64, 8, 128])
        hb.attnT = p.sb(f"attnT{h}", [64, 8, 64], BF16); hb.kdec = p.sb(f"kdec{h}", [64, 8, 128], BF16)
        hb.qdT = p.sb(f"qdT{h}", [128, 8, 64], BF16); hb.oT = p.sb(f"oTw{h}", [128, 8, 64])
        hb.S = p.sb(f"S{h}", [128, 128]); hb.Sb = p.sb(f"Sb{h}", [128, 128], BF16)
        hb.vn = p.sb(f"vn{h}", [64, 128], BF16)
        hb.ps1 = psA[h].view((slice(0, 64), slice(0, 128)))
        hb.psO = psA[h].view((slice(None), slice(128, 192)))
        hb.psS = psA[h].view((slice(None), slice(256, 384)))
        H.append(hb)
    xrs = [p.sb(f"xrG{j}", [128, 514]) for j in range(3)]; cy = p.sb("cyG", [128, 3, 512]); sq = p.sb("sqG", [128, 512])
    rs = p.sb("rsG", [128, 512]); qn = p.sb("qnG", [128, 512]); kn = p.sb("knG", [128, 512])
    ktok = p.sb("ktokG", [64, 8, 128]); vtok = p.sb("vtokG", [64, 8, 128])
    Gm = p.sb("GmG", [64, 8, 64]); Bm = p.sb("BmG", [64, 8, 64])
    diff = p.sb("diffG", [64, 8, 64]); DT = p.sb("DTG", [64, 8, 64]); Ds2 = p.sb("Ds2G", [64, 8, 64])
    bbc = p.sb("bbcG", [64, 8, 64]); mkk = p.sb("mkkG", [64, 8, 64])
    LT = p.sb("LTG", [64, 8, 64]); Lm = p.sb("LmG", [64, 8, 64])
    Xa = [p.sb(f"XaG{i}", [64, 8, 64], INV_DT) for i in range(2)]; Xb = [p.sb(f"XbG{i}", [64, 8, 64], INV_DT) for i in range(2)]
    Rr = [p.sb(f"RG{i}", [64, 8, 64]) for i in range(2)]
    Rb = [p.sb(f"RbG{i}", [64, 8, 64], INV_DT) for i in range(2)]
    LTb = p.sb("LTbG", [64, 8, 64], INV_DT); Lmb = p.sb("LmbG", [64, 8, 64], INV_DT)
    kbs = [p.sb(f"kbG{i}", [64, 8, 128]) for i in range(2)]; vbs = [p.sb(f"vbG{i}", [64, 8, 128]) for i in range(2)]
    psC = psA[:4]; ci = [0]
    SEG = [(0, TC), (TC, TA)]

    def bcn(ap2, nc_, w):
        return ap2.unsqueeze(2).to_broadcast([64, nc_, w])

    def front_gen(h, d, c0, nc_, par):
        hb = H[h]
        W = nc_ * 64; t0 = c0 * 64
        s0, s1 = SEG[0] if c0 < 4 else SEG[1]
        lo = 1 if t0 == s0 else 0
        hi = 1 if t0 + W == s1 else 0
        for j, ch in enumerate((h, 6 + h, 12 + h)):
            xr = xrs[j]
            if lo or hi:
                p.op("pool", [], [xr], lambda e: e.memset(xr[:], 0.0))
            p.dma("sp" if j != 1 else "act", xr, xr[:, lo:W + 2 - hi], C.uT[ch], C.uT[ch][:, t0 - 1 + lo:t0 + W + 1 - hi])
        for j, ch in enumerate((h, 6 + h, 12 + h)):
            xr = xrs[j]
            p.op("act", [xr, cw], [cy], lambda e: e.mul(out=cy[:, j, :W], in_=xr[:, 1:W + 1], mul=cw[:, ch, 1:2]))
            p.op("dve", [xr, cw, cy], [cy], lambda e: e.scalar_tensor_tensor(
                out=cy[:, j, :W], in0=xr[:, 0:W], scalar=cw[:, ch, 0:1], in1=cy[:, j, :W], op0=ALU.mult, op1=ALU.add))
            p.op("dve", [xr, cw, cy], [cy], lambda e: e.scalar_tensor_tensor(
                out=cy[:, j, :W], in0=xr[:, 2:W + 2], scalar=cw[:, ch, 2:3], in1=cy[:, j, :W], op0=ALU.mult, op1=ALU.add))
            p.op("act", [cy], [cy], lambda e: e.activation(out=cy[:, j, :W], in_=cy[:, j, :W], func=AF.Silu))
        yield 0
        for j, dst, scl in ((0, qn, 128.0 ** -0.5), (1, kn, 1.0)):
            yield 0
            p.op("pool", [cy], [sq], lambda e: e.tensor_tensor(out=sq[:, :W], in0=cy[:, j, :W], in1=cy[:, j, :W], op=ALU.mult))
            t = nextpp()
            p.op("pe", [ones, sq], [t], lambda e: e.matmul(t[:, :W], lhsT=ones[:], rhs=sq[:, :W], start=True, stop=True))
            p.op("dve", [t], [rs], lambda e: e.tensor_scalar(out=rs[:, :W], in0=t[:, :W], scalar1=EPS, scalar2=None, op0=ALU.add))
            p.op("act", [rs], [rs], lambda e: e.activation(out=rs[:, :W], in_=rs[:, :W], func=AF.Ln))
            p.op("act", [rs], [rs], lambda e: e.activation(out=rs[:, :W], in_=rs[:, :W], func=AF.Exp, scale=-0.5))
            p.op("dve", [cy, rs], [dst], lambda e: e.scalar_tensor_tensor(
                out=dst[:, :W], in0=cy[:, j, :W], scalar=scl, in1=rs[:, :W], op0=ALU.mult, op1=ALU.mult))
        yield 0
        for src_t, src_ap, dst in ((kn, lambda n: kn[:, n * 64:(n + 1) * 64], ktok), (cy, lambda n: cy[:, 2, n * 64:(n + 1) * 64], vtok)):
            for g in range(0, nc_, 4):
                t = nextpp()
                tv = t[:64, :].rearrange("p (n d) -> p n d", d=128)
                for n in range(g, min(g + 4, nc_)):
                    p.op("pe", [src_t, idf], [t], lambda e: e.transpose(out=tv[:, n - g, :], in_=src_ap(n), identity=idf[:]))
                ne = min(4, nc_ - g)
                p.op("act", [t], [dst], lambda e: e.copy(out=dst[:, g:g + ne, :], in_=tv[:, :ne, :]))
        yield 0
        g_ = gval[:, d, c0:c0 + nc_, h]; b_ = bsig[:, d, c0:c0 + nc_, h]; gc_ = gc[:, d, c0:c0 + nc_, h]
        p.op("dve", [gval, tri], [Gm], lambda e: e.tensor_tensor(out=Gm[:, :nc_, :], in0=bcn(g_, nc_, 64), in1=tri[:, d, :].unsqueeze(1).to_broadcast([64, nc_, 64]), op=ALU.mult))
        p.op("pool", [bsig, idf], [Bm], lambda e: e.tensor_tensor(out=Bm[:, :nc_, :], in0=bcn(b_, nc_, 64), in1=idf[:64, :64].unsqueeze(1).to_broadcast([64, nc_, 64]), op=ALU.mult))
        t = nextpp()
        p.op("pe", [ones, Gm], [t], lambda e: e.matmul(t[:, :W], lhsT=ones[:64, :], rhs=Gm[:, :nc_, :].rearrange("p n i -> p (n i)"), start=True, stop=True))
        tv3 = t[:64, :W].rearrange("p (n i) -> p n i", i=64)
        p.op("dve", [t, gc], [diff], lambda e: e.tensor_tensor(out=diff[:, :nc_, :], in0=tv3, in1=bcn(gc_, nc_, 64), op=ALU.subtract))
        p.op("act", [t], [rs], lambda e: e.activation(out=rs[:, :W], in_=t[:, :W], func=AF.Exp))
        p.op("dve", [qn, rs], [hb.qdT], lambda e: e.tensor_tensor(out=hb.qdT[:, :nc_, :].rearrange("p n i -> p (n i)"), in0=qn[:, :W], in1=rs[:, :W], op=ALU.mult))
        t = nextpp()
        p.op("pe", [ones, Bm], [t], lambda e: e.matmul(t[:64, :W], lhsT=ones[:64, :64], rhs=Bm[:, :nc_, :].rearrange("p n i -> p (n i)"), start=True, stop=True))
        p.op("act", [t], [bbc], lambda e: e.copy(out=bbc[:, :nc_, :].rearrange("p n i -> p (n i)"), in_=t[:64, :W]))
        yield 0
        p.op("dve", [diff, nm], [DT], lambda e: e.tensor_tensor(out=DT[:, :nc_, :], in0=diff[:, :nc_, :], in1=nm[:, 2 * d, :].unsqueeze(1).to_broadcast([64, nc_, 64]), op=ALU.add))
        p.op("act", [DT], [DT], lambda e: e.activation(out=DT[:, :nc_, :], in_=DT[:, :nc_, :], func=AF.Exp))
        p.op("dve", [diff, nm], [Ds2], lambda e: e.scalar_tensor_tensor(
            out=Ds2[:, :nc_, :], in0=diff[:, :nc_, :], scalar=-1.0, in1=nm[:, 2 * (1 - d) + 1, :].unsqueeze(1).to_broadcast([64, nc_, 64]), op0=ALU.mult, op1=ALU.add))
        p.op("act", [Ds2], [Ds2], lambda e: e.activation(out=Ds2[:, :nc_, :], in_=Ds2[:, :nc_, :], func=AF.Exp))
        yield 0
        t = nextpp(); tv3 = t[:64, :W].rearrange("p (n i) -> p n i", i=64)
        for n in range(nc_):
            p.op("pe", [kn], [t], lambda e: e.matmul(tv3[:, n, :], lhsT=kn[:, n * 64:(n + 1) * 64], rhs=kn[:, n * 64:(n + 1) * 64], start=True, stop=True))
        p.op("act", [t], [mkk], lambda e: e.copy(out=mkk[:, :nc_, :], in_=tv3))
        t = nextpp(); tq3 = t[:64, :W].rearrange("p (n i) -> p n i", i=64)
        for n in range(nc_):
            p.op("pe", [kn, qn], [t], lambda e: e.matmul(tq3[:, n, :], lhsT=kn[:, n * 64:(n + 1) * 64], rhs=qn[:, n * 64:(n + 1) * 64], start=True, stop=True))
        p.op("dve", [t, DT], [hb.attnT], lambda e: e.tensor_tensor(out=hb.attnT[:, :nc_, :], in0=tq3, in1=DT[:, :nc_, :], op=ALU.mult))
        yield 0
        p.op("dve", [mkk, DT], [LT], lambda e: e.tensor_tensor(out=LT[:, :nc_, :], in0=mkk[:, :nc_, :], in1=DT[:, :nc_, :], op=ALU.mult))
        p.op("pool", [LT, st01], [LT], lambda e: e.tensor_tensor(out=LT[:, :nc_, :], in0=LT[:, :nc_, :], in1=st01[:, d, :].unsqueeze(1).to_broadcast([64, nc_, 64]), op=ALU.mult))
        p.op("dve", [LT, bbc], [LT], lambda e: e.tensor_tensor(out=LT[:, :nc_, :], in0=LT[:, :nc_, :], in1=bbc[:, :nc_, :], op=ALU.mult))
        p.op("pool", [mkk, Ds2], [Lm], lambda e: e.tensor_tensor(out=Lm[:, :nc_, :], in0=mkk[:, :nc_, :], in1=Ds2[:, :nc_, :], op=ALU.mult))
        p.op("dve", [Lm, bsig], [Lm], lambda e: e.tensor_tensor(out=Lm[:, :nc_, :], in0=Lm[:, :nc_, :], in1=bcn(b_, nc_, 64), op=ALU.mult))
        kb = kbs[par]; vb = vbs[par]
        p.op("pool", [ktok, bsig], [kb], lambda e: e.tensor_tensor(out=kb[:, :nc_, :], in0=ktok[:, :nc_, :], in1=bcn(b_, nc_, 128), op=ALU.mult))
        p.op("dve", [kb, egc], [kb], lambda e: e.tensor_tensor(out=kb[:, :nc_, :], in0=kb[:, :nc_, :], in1=bcn(egc[:, d, c0:c0 + nc_, h], nc_, 128), op=ALU.mult))
        p.op("pool", [vtok, bsig], [vb], lambda e: e.tensor_tensor(out=vb[:, :nc_, :], in0=vtok[:, :nc_, :], in1=bcn(b_, nc_, 128), op=ALU.mult))
        p.op("dve", [ktok, ekd], [hb.kdec], lambda e: e.tensor_tensor(out=hb.kdec[:, :nc_, :], in0=ktok[:, :nc_, :], in1=bcn(ekd[:, d, c0:c0 + nc_, h], nc_, 128), op=ALU.mult))
        yield 0
        yield 1
        R = Rr[0]
        p.op("dve", [LT, idf], [R], lambda e: e.scalar_tensor_tensor(
            out=R[:, :nc_, :], in0=LT[:, :nc_, :], scalar=-1.0, in1=idf[:64, :64].unsqueeze(1).to_broadcast([64, nc_, 64]), op0=ALU.mult, op1=ALU.add))
        p.op("act", [LT], [LTb], lambda e: e.copy(out=LTb[:, :nc_, :], in_=LT[:, :nc_, :]))
        p.op("act", [Lm], [Lmb], lambda e: e.copy(out=Lmb[:, :nc_, :], in_=Lm[:, :nc_, :]))
        p.op("pool", [R], [Rb[0]], lambda e: e.tensor_copy(out=Rb[0][:, :nc_, :], in_=R[:, :nc_, :]))

    def chain_gen(nc_):
        W = nc_ * 64
        R = Rr[0]; Rbc = Rb[0]
        X, XT = LTb, Lmb
        for it in range(5):
            Xn, XTn = Xa[it % 2], Xb[it % 2]
            t = psC[ci[0] % 4]; ci[0] += 1
            t3 = t[:64, :W].rearrange("p (n i) -> p n i", i=64)
            for n in range(nc_):
                p.op("pe", [X, XT], [t], lambda e: e.matmul(t3[:, n, :], lhsT=XT[:, n, :], rhs=X[:, n, :], start=True, stop=True))
            tb_ = psC[ci[0] % 4]; ci[0] += 1
            t3b = tb_[:64, :W].rearrange("p (n i) -> p n i", i=64)
            for n in range(nc_):
                p.op("pe", [X, XT], [tb_], lambda e: e.matmul(t3b[:, n, :], lhsT=X[:, n, :], rhs=XT[:, n, :], start=True, stop=True))
            yield 0
            p.op("act", [t], [Xn], lambda e: e.copy(out=Xn[:, :nc_, :], in_=t3))
            p.op("act", [tb_], [XTn], lambda e: e.copy(out=XTn[:, :nc_, :], in_=t3b))
            tc_ = psC[ci[0] % 4]; ci[0] += 1
            t3c = tc_[:64, :W].rearrange("p (n i) -> p n i", i=64)
            for n in range(nc_):
                p.op("pe", [XTn, Rbc], [tc_], lambda e: e.matmul(t3c[:, n, :], lhsT=XTn[:, n, :], rhs=Rbc[:, n, :], start=True, stop=True))
            yield 0
            Rn = Rr[(it + 1) % 2]
            p.op("dve", [tc_, R], [Rn], lambda e: e.tensor_tensor(out=Rn[:, :nc_, :], in0=t3c, in1=R[:, :nc_, :], op=ALU.add))
            if it < 4:
                Rbn = Rb[(it + 1) % 2]
                p.op("act", [Rn], [Rbn], lambda e: e.copy(out=Rbn[:, :nc_, :], in_=Rn[:, :nc_, :]))
                Rbc = Rbn
            R = Rn; X, XT = Xn, XTn

    def tail(h, nc_, par):
        hb = H[h]
        W = nc_ * 64
        kb = kbs[par]; vb = vbs[par]
        AinvT = Rr[1]
        t = psC[ci[0] % 4]; ci[0] += 1
        tw3 = t[:, :W].rearrange("p (n i) -> p n i", i=64)
        for n in range(nc_):
            p.op("pe", [kb, AinvT], [t], lambda e: e.matmul(tw3[:, n, :], lhsT=kb[:, n, :], rhs=AinvT[:, n, :], start=True, stop=True))
        p.op("act", [t], [hb.wT], lambda e: e.copy(out=hb.wT[:, :nc_, :], in_=tw3))
        for g in range(0, nc_, 4):
            t = psC[ci[0] % 4]; ci[0] += 1
            tu3 = t[:64, :].rearrange("p (n d) -> p n d", d=128)
            for n in range(g, min(g + 4, nc_)):
                p.op("pe", [AinvT, vb], [t], lambda e: e.matmul(tu3[:, n - g, :], lhsT=AinvT[:, n, :], rhs=vb[:, n, :], start=True, stop=True))
            ne = min(4, nc_ - g)
            p.op("dve", [t], [hb.u], lambda e: e.tensor_copy(out=hb.u[:, g:g + ne, :], in_=tu3[:, :ne, :]))

    def run_window(d, c0, nc_):
        def drain(g, until_final=False):
            for v in g:
                if until_final and v == 1:
                    return False
            return True
        fr = front_gen(0, d, c0, nc_, 0)
        drain(fr)
        for i in range(GH):
            ch = chain_gen(nc_)
            nf = front_gen(i + 1, d, c0, nc_, (i + 1) % 2) if i + 1 < GH else None
            nf_done = nf is None
            ch_done = False
            while not ch_done:
                try:
                    next(ch)
                except StopIteration:
                    ch_done = True
                if not nf_done:
                    try:
                        for _ in range(1):
                            v = next(nf)
                            if v == 1:
                                nf_done = True
                                break
                    except StopIteration:
                        nf_done = True; nf = None
            tail(i, nc_, i % 2)
            if nf is not None:
                drain(nf)

    def scan_step(h, d, c0, n):
        hb = H[h]
        cidx = c0 + n
        p.op("pe", [hb.wT, hb.Sb], [hb.ps1], lambda e: e.matmul(hb.ps1[:], lhsT=hb.wT[:, n, :], rhs=hb.Sb[:], start=True, stop=True))
        p.op("dve", [hb.u, hb.ps1], [hb.vn], lambda e: e.tensor_tensor(out=hb.vn[:], in0=hb.u[:, n, :], in1=hb.ps1[:], op=ALU.subtract))
        p.op("pe", [hb.Sb, hb.qdT], [hb.psO], lambda e: e.matmul(hb.psO[:], lhsT=hb.Sb[:], rhs=hb.qdT[:, n, :], start=True, stop=False))
        p.op("pe", [hb.vn, hb.attnT], [hb.psO], lambda e: e.matmul(hb.psO[:], lhsT=hb.vn[:], rhs=hb.attnT[:, n, :], start=False, stop=True))
        p.op("pe", [hb.kdec, hb.vn], [hb.psS], lambda e: e.matmul(hb.psS[:], lhsT=hb.kdec[:, n, :], rhs=hb.vn[:], start=True, stop=True))
        p.op("dve", [hb.S, egl, hb.psS], [hb.S], lambda e: e.scalar_tensor_tensor(
            out=hb.S[:], in0=hb.S[:], scalar=egl[:, d, cidx, h:h + 1], in1=hb.psS[:], op0=ALU.mult, op1=ALU.add))
        p.op("act", [hb.S], [hb.Sb], lambda e: e.copy(out=hb.Sb[:], in_=hb.S[:]))
        p.op("act", [hb.psO], [hb.oT], lambda e: e.copy(out=hb.oT[:, n, :], in_=hb.psO[:]))

    if dbg < 2:
        run_window(0, 4, 8)
        hb = H[0]
        C.dbg = [(hb.u, hb.u[:].rearrange("p n d -> p (n d)"), [64, 1024]), (Rr[1], Rr[1][:].rearrange("p n d -> p (n d)"), [64, 512]),
                 (LT, LT[:].rearrange("p n d -> p (n d)"), [64, 512]), (ktok, ktok[:].rearrange("p n d -> p (n d)"), [64, 1024])]
        return
    for d in range(2):
        for h in range(GH):
            hb = H[h]
            p.op("pool", [], [hb.S], lambda e: e.memset(hb.S[:], 0.0))
            p.op("pool", [], [hb.Sb], lambda e: e.memset(hb.Sb[:], 0.0))
        for (c0, nc_) in gdn_windows(d):
            run_window(d, c0, nc_)
            p.barrier()
            order = range(nc_) if d == 0 else range(nc_ - 1, -1, -1)
            for n in order:
                for h in range(GH):
                    scan_step(h, d, c0, n)
            for h in range(GH):
                hb = H[h]
                p.dma("sp" if h % 2 else "act", C.oTd[d][h], C.oTd[d][h][:, c0 * 64:(c0 + nc_) * 64], hb.oT, hb.oT[:, :nc_, :].rearrange("p n i -> p (n i)"))
            p.barrier()
    p.barrier()
    p.release(mk)
    mk = p.mark()
    gn2 = p.sb("gn2G", [128, 1]); p.dma("sp", gn2, gn2[:], C.gnorm, C.gnorm.ap[l])
    ones2 = p.sb("ones2G", [128, 128]); p.op("pool", [], [ones2], lambda e: e.memset(ones2[:], 1.0))
    of = [p.sb(f"ofG{i}", [128, TA]) for i in range(2)]; ob_ = [p.sb(f"obG{i}", [128, TA]) for i in range(2)]
    zz = [p.sb(f"zzG{i}", [128, TA]) for i in range(2)]
    sq2 = [p.sb(f"sq2G{i}", [128, 512]) for i in range(2)]; r2 = [p.sb(f"r2G{i}", [128, 512]) for i in range(2)]
    om = [p.sb(f"omG{i}", [128, TA], BF16) for i in range(2)]
    pq = [p.ps(f"pqG{i}", [128, 512]) for i in range(2)]
    for h in range(GH):
        a, b_, z_, o_ = of[h % 2], ob_[h % 2], zz[h % 2], om[h % 2]
        p.dma("sp", a, a[:], C.oTd[0][h], C.oTd[0][h][:])
        p.dma("act", b_, b_[:], C.oTd[1][h], C.oTd[1][h][:])
        p.dma("sp", z_, z_[:], C.uT[18 + h], C.uT[18 + h][:])
        p.op("pool", [a, b_], [a], lambda e: e.tensor_tensor(out=a[:], in0=a[:], in1=b_[:], op=ALU.add))
        p.op("act", [z_], [z_], lambda e: e.activation(out=z_[:], in_=z_[:], func=AF.Silu))
        for blk in range(9):
            c0 = blk * 512; w = min(512, TA - c0)
            s_, r_ = sq2[blk % 2], r2[blk % 2]; t = pq[blk % 2]
            p.op("pool", [a], [s_], lambda e: e.tensor_tensor(out=s_[:, :w], in0=a[:, c0:c0 + w], in1=a[:, c0:c0 + w], op=ALU.mult))
            p.op("pe", [ones2, s_], [t], lambda e: e.matmul(t[:, :w], lhsT=ones2[:], rhs=s_[:, :w], start=True, stop=True))
            p.op("dve", [t], [r_], lambda e: e.tensor_scalar(out=r_[:, :w], in0=t[:, :w], scalar1=1.0 / 128, scalar2=EPS, op0=ALU.mult, op1=ALU.add))
            p.op("act", [r_], [r_], lambda e: e.activation(out=r_[:, :w], in_=r_[:, :w], func=AF.Ln))
            p.op("act", [r_], [r_], lambda e: e.activation(out=r_[:, :w], in_=r_[:, :w], func=AF.Exp, scale=-0.5))
            p.op("dve", [a, gn2, r_], [r_], lambda e: e.scalar_tensor_tensor(
                out=r_[:, :w], in0=a[:, c0:c0 + w], scalar=gn2[:, 0:1], in1=r_[:, :w], op0=ALU.mult, op1=ALU.mult))
            p.op("dve", [r_, z_], [o_], lambda e: e.tensor_tensor(out=o_[:, c0:c0 + w], in0=r_[:, :w], in1=z_[:, c0:c0 + w], op=ALU.mult))
        p.dma("sp", C.mixT[h], C.mixT[h][:], o_, o_[:])
    p.barrier()
    p.release(mk)


def declare_scratch2(p, C):
    C.h2 = p.dram("h2_s", [TA, D], BF16)
    C.h2v = [C.h2.view((slice(i * 128, (i + 1) * 128), slice(None))) for i in range(NT)]
    C.affT = p.dram("affT_s", [NE, TA], F32)
    C.acc = p.dram("acc_s", [TA, D], F32)


def stage_D(p, C, l, with_ctx=True):
    mk = p.mark()
    wo = p.sb("woD", [128, 16, D], BF16)
    wov = [wo.view((slice(None), slice(q * 4, (q + 1) * 4), slice(None))) for q in range(4)]
    for q in range(4):
        p.dma("pool", wov[q], wov[q][:], C.w_out, C.w_out.ap[l, q * 512:(q + 1) * 512, :].rearrange("(k p) n -> p k n", p=128))
    wr = p.sb("wrD", [128, 16, NE], BF16)
    p.dma("pool", wr, wr[:], C.w_r, C.w_r.ap[l].rearrange("(k p) e -> p k e", p=128))
    idf = p.sb("idfD", [128, 128]); idb = p.sb("idbD", [128, 128], BF16)
    p.dma("sp", idf, idf[:], C.ident, C.ident[:])
    p.op("dve", [idf], [idb], lambda e: e.tensor_copy(out=idb[:], in_=idf[:]))
    ones16 = p.sb("ones16D", [NE, NE]); p.op("pool", [], [ones16], lambda e: e.memset(ones16[:], 1.0))
    expT = p.sb("expTD", [NE, TA])
    g2 = p.sb("g2D", [128, D]); gs = p.sb("gsD", [128, D]); sh = p.sb("shD", [128, D]); n2 = p.sb("n2D", [128, D])
    mx = p.sb("mxD", [128, 16, 1024], BF16)
    mxv = [mx.view((slice(None), c, slice(None))) for c in range(16)]
    hb = [p.sb(f"hbD{i}", [128, D]) for i in range(2)]
    yb = [p.sb(f"ybD{i}", [128, D]) for i in range(2)]
    ab = [p.sb(f"abD{i}", [128, D], BF16) for i in range(2)]
    h2T = [p.sb(f"h2TD{i}", [128, 16, 128], BF16) for i in range(2)]
    ss = [p.sb(f"ssD{i}", [128, 2]) for i in range(2)]
    tmp = [p.sb(f"tmpD{i}", [128, 512]) for i in range(2)]
    pu = [p.ps(f"puD{i}", [128, 512]) for i in range(4)]
    ptr = [p.ps(f"ptrD{i}", [128, 4, 128], BF16) for i in range(2)]
    pr = [p.ps(f"prD{i}", [NE, 128]) for i in range(2)]
    bc_load(p, "sp", n2, n2[:], C.norm2, C.norm2.ap[l:l + 1, :])
    groups = [(0, [0, 1])] + [(1, list(range(2 + 8 * g, 10 + 8 * g))) for g in range(4)]
    pc = 0; tc_ = 0; tcount = 0
    pending = [None]
    last_kind = None
    for kind, tiles in groups:
        if kind == 0 and not with_ctx:
            continue
        if kind != last_kind:
            last_kind = kind
            bc_load(p, "sp", g2, g2[:], C.mod, modrow(C, l, kind, 2))
            bc_load(p, "act", gs, gs[:], C.mod, modrow(C, l, kind, 4))
            bc_load(p, "sp", sh, sh[:], C.mod, modrow(C, l, kind, 3))
            p.op("dve", [gs, n2], [gs], lambda e: e.scalar_tensor_tensor(out=gs[:], in0=gs[:], scalar=1.0, in1=n2[:], op0=ALU.add, op1=ALU.mult))
        g0 = tiles[0] * 128; gw = len(tiles) * 128
        for c in range(16):
            p.dma("sp" if c % 2 else "act", mxv[c], mxv[c][:, :gw], C.mixT[c], C.mixT[c][:, g0:g0 + gw])
        for tj, ti in enumerate(tiles):
            ht = hb[tcount % 2]; y_ = yb[tcount % 2]; a_ = ab[tcount % 2]; s_ = ss[tcount % 2]; hT = h2T[tcount % 2]
            tcount += 1
            p.dma("sp", ht, ht[:], C.hv[ti], C.hv[ti][:])
            for k in range(16):
                for nb in range(4):
                    p.op("pe", [mxv[k], wov[k // 4]], [pu[nb]], lambda e: e.matmul(
                        pu[nb][:], lhsT=mx[:, k, tj * 128:(tj + 1) * 128], rhs=wo[:, k, nb * 512:(nb + 1) * 512], start=(k == 0), stop=(k == 15)))
            if pending[0] is not None:
                pending[0](); pending[0] = None
            for nb in range(4):
                t_ = tmp[nb % 2]
                p.op("dve", [pu[nb], g2], [t_], lambda e: e.tensor_tensor(out=t_[:], in0=pu[nb][:], in1=g2[:, nb * 512:(nb + 1) * 512], op=ALU.mult))
                p.op("pool", [t_, ht], [ht], lambda e: e.tensor_tensor(out=ht[:, nb * 512:(nb + 1) * 512], in0=ht[:, nb * 512:(nb + 1) * 512], in1=t_[:], op=ALU.add))
            p.dma("sp", C.hv[ti], C.hv[ti][:], ht, ht[:])
            p.op("pool", [], [s_], lambda e: e.memset(s_[:], 0.0))
            p.op("act", [ht], [y_, s_], lambda e: e.activation(out=y_[:], in_=ht[:], func=AF.Square, accum_out=s_[:, 0:1]))
            p.op("dve", [s_], [s_], lambda e: e.tensor_scalar(out=s_[:, 1:2], in0=s_[:, 0:1], scalar1=1.0 / D, scalar2=EPS, op0=ALU.mult, op1=ALU.add))
            p.op("act", [s_], [s_], lambda e: e.sqrt(out=s_[:, 1:2], in_=s_[:, 1:2]))
            p.op("dve", [s_], [s_], lambda e: e.reciprocal(out=s_[:, 1:2], in_=s_[:, 1:2]))
            p.op("dve", [ht, s_, gs], [y_], lambda e: e.scalar_tensor_tensor(out=y_[:], in0=ht[:], scalar=s_[:, 1:2], in1=gs[:], op0=ALU.mult, op1=ALU.mult))
            p.op("pool", [y_, sh], [a_], lambda e: e.tensor_tensor(out=a_[:], in0=y_[:], in1=sh[:], op=ALU.add))
            p.dma("act", C.h2v[ti], C.h2v[ti][:], a_, a_[:])

            def tail(a_=a_, hT=hT, ti=ti, pq=pr[tcount % 2]):
                nonlocal tc_
                for k4 in range(4):
                    pt = ptr[tc_ % 2]; tc_ += 1
                    for kk in range(4):
                        k = k4 * 4 + kk
                        p.op("pe", [a_, idb], [pt], lambda e: e.transpose(out=pt[:, kk, :], in_=a_[:, k * 128:(k + 1) * 128], identity=idb[:]))
                    p.op("act", [pt], [hT], lambda e: e.copy(out=hT[:, k4 * 4:(k4 + 1) * 4, :], in_=pt[:]))
                for k in range(16):
                    p.op("pe", [wr, hT], [pq], lambda e: e.matmul(pq[:], lhsT=wr[:, k, :], rhs=hT[:, k, :], start=(k == 0), stop=(k == 15)))
                p.op("act", [pq], [expT], lambda e: e.activation(out=expT[:, ti * 128:(ti + 1) * 128], in_=pq[:], func=AF.Exp))
            pending[0] = tail
    if pending[0] is not None:
        pending[0](); pending[0] = None
    t0_ = 0 if with_ctx else TC
    blk = t0_
    while blk < TA:
        w = min(512, TA - blk)
        pp = pu[pc % 4]; pc += 1
        t_ = tmp[pc % 2]
        p.op("pe", [ones16, expT], [pp], lambda e: e.matmul(pp[:NE, :w], lhsT=ones16[:], rhs=expT[:, blk:blk + w], start=True, stop=True))
        p.op("dve", [pp], [t_], lambda e: e.reciprocal(out=t_[:NE, :w], in_=pp[:NE, :w]))
        p.op("dve", [t_, expT], [expT], lambda e: e.tensor_tensor(out=expT[:, blk:blk + w], in0=expT[:, blk:blk + w], in1=t_[:NE, :w], op=ALU.mult))
        blk += w
    p.dma("sp", C.affT, C.affT[:, t0_:], expT, expT[:, t0_:])
    p.barrier()
    p.release(mk)


def stage_E(p, C, l, with_ctx=True):
    segs = [(TC, TL, 512)] + ([(0, TC, 32)] if with_ctx else [])
    nslots = sum(s[2] for s in segs)
    mk = p.mark()
    idf = p.sb("idfE", [128, 128]); idb = p.sb("idbE", [128, 128], BF16)
    p.dma("sp", idf, idf[:], C.ident, C.ident[:])
    p.op("dve", [idf], [idb], lambda e: e.tensor_copy(out=idb[:], in_=idf[:]))
    idxT = p.sb("idxTE", [128, 5, NE], I32); gateT = p.sb("gateTE", [128, 5, NE])
    mk0 = p.mark()
    zt = p.sb("ztE", [128, D]); p.op("pool", [], [zt], lambda e: e.memset(zt[:], 0.0))
    for i in range(NT):
        p.dma("sp" if i % 2 else "act", C.acc, C.acc.ap[i * 128:(i + 1) * 128, :], zt, zt[:])
    aff = p.sb("affE", [NE, TA]); work = p.sb("workE", [NE, TL])
    vals = p.sb("valsE", [NE, 544]); idxu = p.sb("idxuE", [NE, 544], U32); idxf = p.sb("idxfE", [NE, 544])
    ptk = p.ps("ptkE", [128, 2, NE])
    p.dma("sp", aff, aff[:], C.affT, C.affT[:])
    so = 0
    slot_tiles = []
    for (s0, n, cap) in segs:
        p.op("dve", [aff], [work], lambda e: e.tensor_copy(out=work[:, :n], in_=aff[:, s0:s0 + n]))
        for r in range(cap // 8):
            c0 = so + r * 8
            p.op("dve", [work], [vals], lambda e: e.max(out=vals[:, c0:c0 + 8], in_=work[:, :n]))
            p.op("dve", [vals, work], [idxu], lambda e: e.max_index(out=idxu[:, c0:c0 + 8], in_max=vals[:, c0:c0 + 8], in_values=work[:, :n]))
            p.op("dve", [vals, work], [work], lambda e: e.match_replace(out=work[:, :n], in_to_replace=vals[:, c0:c0 + 8], in_values=work[:, :n], imm_value=-1.0))
        p.op("dve", [idxu], [idxf], lambda e: e.tensor_copy(out=idxf[:, so:so + cap], in_=idxu[:, so:so + cap]))
        p.op("dve", [idxf], [idxf], lambda e: e.tensor_scalar(out=idxf[:, so:so + cap], in0=idxf[:, so:so + cap], scalar1=float(s0), scalar2=None, op0=ALU.add))
        for st in range((cap + 127) // 128):
            rows = min(128, cap - st * 128)
            ti = len(slot_tiles)
            c0 = so + st * 128
            p.op("pe", [idxf, idf], [ptk], lambda e: e.transpose(out=ptk[:rows, 0, :], in_=idxf[:, c0:c0 + rows], identity=idf[:NE, :NE]))
            p.op("pe", [vals, idf], [ptk], lambda e: e.transpose(out=ptk[:rows, 1, :], in_=vals[:, c0:c0 + rows], identity=idf[:NE, :NE]))
            p.op("dve", [ptk], [idxT], lambda e: e.tensor_copy(out=idxT[:rows, ti, :], in_=ptk[:rows, 0, :]))
            p.op("dve", [ptk], [gateT], lambda e: e.tensor_copy(out=gateT[:rows, ti, :], in_=ptk[:rows, 1, :]))
            slot_tiles.append((c0, rows, ti))
        so += cap
    p.barrier()
    p.release(mk0)
    gu = [[p.sb(f"guE{i}{j}", [128, 16, 512], BF16) for j in range(2)] for i in range(2)]
    wd = p.sb("wdE", [128, 8, D], BF16)
    wdv = [wd.view((slice(None), slice(q * 4, (q + 1) * 4), slice(None))) for q in range(2)]
    xsT = p.sb("xsTE", [128, 16, 544], BF16); hidT = p.sb("hidTE", [128, 8, 544], BF16)
    xs = [p.sb(f"xsE{i}", [128, D], BF16) for i in range(2)]
    yt = [p.sb(f"ytE{i}", [128, D]) for i in range(2)]
    sg = [p.sb(f"sgE{i}", [128, 512]) for i in range(2)]
    ptr = [p.ps(f"ptrE{i}", [128, 4, 128], BF16) for i in range(2)]
    X6 = [p.ps(f"pxE{i}", [128, 512]) for i in range(6)]
    blocks = [(0, 512)] + ([(512, 32)] if with_ctx else [])
    gi = 0; xi = 0; tci = 0; yi = 0; si = 0
    for e_ in range(NE):
        for (c0, rows, ti) in slot_tiles:
            x_ = xs[xi % 2]; xi += 1
            p.dma_indirect_gather(x_, x_[:rows, :], C.h2, C.h2.ap[:, :], idxT, idxT[:rows, ti, e_:e_ + 1])
            for k4 in range(4):
                pt = ptr[tci % 2]; tci += 1
                for kk in range(4):
                    k = k4 * 4 + kk
                    p.op("pe", [x_, idb], [pt], lambda e: e.transpose(out=pt[:, kk, :rows], in_=x_[:rows, k * 128:(k + 1) * 128], identity=idb[:rows, :rows]))
                p.op("act", [pt], [xsT], lambda e: e.copy(out=xsT[:, k4 * 4:(k4 + 1) * 4, c0:c0 + rows], in_=pt[:, :, :rows]))
        for half in range(2):
            g_, u_ = gu[gi % 2]; gi += 1
            for hh in range(2):
                p.dma("pool", g_, g_[:, hh * 8:(hh + 1) * 8, :], C.w_eg, C.w_eg.ap[l, e_, hh * 1024:(hh + 1) * 1024, half * 512:(half + 1) * 512].rearrange("(k p) n -> p k n", p=128))
                p.dma("pool", u_, u_[:, hh * 8:(hh + 1) * 8, :], C.w_eu, C.w_eu.ap[l, e_, hh * 1024:(hh + 1) * 1024, half * 512:(half + 1) * 512].rearrange("(k p) n -> p k n", p=128))
            for fcc in range(4):
                fc = half * 4 + fcc
                for (b0, bw) in blocks:
                    pG, pU = X6[2 * (si % 3)], X6[2 * (si % 3) + 1]; s_ = sg[si % 2]; si += 1
                    for k in range(16):
                        p.op("pe", [g_, xsT], [pG], lambda e: e.matmul(pG[:, :bw], lhsT=g_[:, k, fcc * 128:(fcc + 1) * 128], rhs=xsT[:, k, b0:b0 + bw], start=(k == 0), stop=(k == 15)))
                        p.op("pe", [u_, xsT], [pU], lambda e: e.matmul(pU[:, :bw], lhsT=u_[:, k, fcc * 128:(fcc + 1) * 128], rhs=xsT[:, k, b0:b0 + bw], start=(k == 0), stop=(k == 15)))
                    p.op("act", [pG], [s_], lambda e: e.activation(out=s_[:, :bw], in_=pG[:, :bw], func=AF.Silu))
                    p.op("dve", [s_, pU], [hidT], lambda e: e.tensor_tensor(out=hidT[:, fc, b0:b0 + bw], in0=s_[:, :bw], in1=pU[:, :bw], op=ALU.mult))
        for q in range(2):
            p.dma("pool", wdv[q], wdv[q][:], C.w_ed, C.w_ed.ap[l, e_, q * 512:(q + 1) * 512, :].rearrange("(k p) n -> p k n", p=128))
        for (c0, rows, ti) in slot_tiles:
            y_ = yt[yi % 2]; yi += 1
            for n2 in range(2):
                pA, pB = X6[2 * (si % 3)], X6[2 * (si % 3) + 1]; si += 1
                for fc in range(8):
                    for j, pb in enumerate((pA, pB)):
                        nb = n2 * 2 + j
                        p.op("pe", [hidT, wdv[fc // 4]], [pb], lambda e: e.matmul(
                            pb[:rows, :], lhsT=hidT[:, fc, c0:c0 + rows], rhs=wd[:, fc, nb * 512:(nb + 1) * 512], start=(fc == 0), stop=(fc == 7)))
                nb = n2 * 2
                p.op("act", [pA, gateT], [y_], lambda e: e.mul(out=y_[:rows, nb * 512:(nb + 1) * 512], in_=pA[:rows, :], mul=gateT[:rows, ti, e_:e_ + 1]))
                p.op("dve", [pB, gateT], [y_], lambda e: e.tensor_scalar(
                    out=y_[:rows, (nb + 1) * 512:(nb + 2) * 512], in0=pB[:rows, :], scalar1=gateT[:rows, ti, e_:e_ + 1], scalar2=None, op0=ALU.mult))
            p.dma_indirect_scatter_add(C.acc, C.acc.ap[:, :], y_, y_[:rows, :], idxT, idxT[:rows, ti, e_:e_ + 1])
    p.barrier()
    p.release(mk)


def stage_F(p, C, l, with_ctx=True, final=False):
    mk = p.mark()
    g5 = p.sb("g5F", [128, 2, D])
    for kind in range(2):
        bc_load(p, "sp", g5, g5[:, kind, :], C.mod, modrow(C, l, kind, 5))
    if final:
        fg = p.sb("fgF", [128, D]); bc_load(p, "act", fg, fg[:], C.fnorm, C.fnorm.ap[0:1, :])
    hb = [p.sb(f"hbF{i}", [128, D]) for i in range(2)]; ac = [p.sb(f"acF{i}", [128, D]) for i in range(2)]
    ss = [p.sb(f"ssF{i}", [128, 2]) for i in range(2)]
    for ti in range(NT):
        kind = 0 if ti < 2 else 1
        if kind == 0 and (final or not with_ctx):
            continue
        ht = hb[ti % 2]; a_ = ac[ti % 2]; s_ = ss[ti % 2]
        p.dma("sp", ht, ht[:], C.hv[ti], C.hv[ti][:])
        p.dma("act", a_, a_[:], C.acc, C.acc.ap[ti * 128:(ti + 1) * 128, :])
        p.op("pool", [a_, g5], [a_], lambda e: e.tensor_tensor(out=a_[:], in0=a_[:], in1=g5[:, kind, :], op=ALU.mult))
        p.op("dve", [ht, a_], [ht], lambda e: e.tensor_tensor(out=ht[:], in0=ht[:], in1=a_[:], op=ALU.add))
        if not final:
            p.dma("sp", C.hv[ti], C.hv[ti][:], ht, ht[:])
        else:
            p.op("pool", [], [s_], lambda e: e.memset(s_[:], 0.0))
            p.op("act", [ht], [a_, s_], lambda e: e.activation(out=a_[:], in_=ht[:], func=AF.Square, accum_out=s_[:, 0:1]))
            p.op("dve", [s_], [s_], lambda e: e.tensor_scalar(out=s_[:, 1:2], in0=s_[:, 0:1], scalar1=1.0 / D, scalar2=EPS, op0=ALU.mult, op1=ALU.add))
            p.op("act", [s_], [s_], lambda e: e.sqrt(out=s_[:, 1:2], in_=s_[:, 1:2]))
            p.op("dve", [s_], [s_], lambda e: e.reciprocal(out=s_[:, 1:2], in_=s_[:, 1:2]))
            p.op("dve", [ht, s_, fg], [a_], lambda e: e.scalar_tensor_tensor(out=a_[:], in0=ht[:], scalar=s_[:, 1:2], in1=fg[:], op0=ALU.mult, op1=ALU.mult))
            r0 = ti * 128 - TC
            p.dma("sp", C.out, C.out.ap[r0:r0 + 128, :], a_, a_[:])
    p.barrier()
    p.release(mk)


def host_consts():
    ident = np.eye(128, dtype=np.float32)
    rows = TL // 64
    row = np.repeat(np.arange(rows, dtype=np.float32), 64)
    col = np.tile(np.arange(64, dtype=np.float32), rows)
    inv = (10000.0 ** (-np.arange(16, dtype=np.float32) / 16)).astype(np.float32)
    ang = np.stack([row[:, None] * inv, col[:, None] * inv], 1).astype(np.float32)
    cosA = np.cos(ang); sinA = np.sin(ang)
    cosT = np.zeros((128, TL), np.float32); sinT = np.zeros((128, TL), np.float32)
    rot = np.zeros((128, 128), np.float32)
    for m in range(128):
        base = (m // 64) * 64; q = m % 64
        axis = q // 32; sel = (q % 32) // 16; pair = q % 16
        cosT[m] = cosA[:, axis, pair]
        sinT[m] = sinA[:, axis, pair] * (-1.0 if sel == 0 else 1.0)
        src = base + (q + 16 if sel == 0 else q - 16)
        rot[src, m] = 1.0
    idx = np.arange(64)
    tri = np.stack([(idx[:, None] <= idx[None, :]), (idx[:, None] >= idx[None, :])], 0).astype(np.float32)
    j = idx[:, None]; i = idx[None, :]
    nm = np.stack([np.where(i >= j, 0.0, NEG), np.where(i > j, 0.0, NEG), np.where(i <= j, 0.0, NEG), np.where(i < j, 0.0, NEG)], 0).astype(np.float32)
    return dict(ident=ident, cosT=np.ascontiguousarray(cosT), sinT=np.ascontiguousarray(sinT), rotm=rot, tri=tri, nmask=nm)


def make_inputs(inp, b, depth=DEPTH, l0=0):
    ls = slice(l0, l0 + depth)
    m = dict(host_consts())
    m["x"] = inp["x"][b]; m["ctx"] = inp["ctx"][b]
    sc = np.stack([inp["c"][b], inp["c_ctx"]], 0)
    m["cT"] = np.ascontiguousarray(sc.reshape(2, 16, 128).transpose(2, 1, 0))
    m["w_mod"] = inp["w_mod"][ls]
    m["b_mod"] = np.ascontiguousarray(np.broadcast_to(inp["b_mod"][ls][:, None, :], (depth, 2, 6 * D)))
    m["norm1"] = inp["norm1_g"][ls]; m["norm2"] = inp["norm2_g"][ls]
    m["w_in"] = inp["w_in"][ls]
    gc = inp["gdn_conv_w"][ls]
    m["gconv"] = np.ascontiguousarray(gc.reshape(depth, 3, 18, 128).transpose(0, 3, 2, 1))
    m["galog"] = np.ascontiguousarray(np.broadcast_to(inp["gdn_a_log"][ls].reshape(depth, 1, 12), (depth, 64, 12)))
    m["gdtb"] = np.ascontiguousarray(np.broadcast_to(inp["gdn_dt_bias"][ls].reshape(depth, 1, 12), (depth, 64, 12)))
    m["gnorm"] = np.ascontiguousarray(inp["gdn_norm_g"][ls].reshape(depth, 128, 1))
    sc_w = inp["sc_conv_w"][ls]
    m["sconv"] = np.ascontiguousarray(sc_w.reshape(depth, 3, 4, 128).transpose(0, 3, 2, 1))
    m["dlam"] = np.ascontiguousarray(np.broadcast_to(inp["diff_lambda"][ls][:, None], (depth, 128, 4, 64)))
    m["dnorm"] = np.ascontiguousarray(inp["diff_norm_g"][ls].reshape(depth, 128, 1))
    m["w_out"] = inp["w_out"][ls]; m["w_r"] = inp["w_router"][ls]
    m["w_eg"] = inp["w_e_gate"][ls]; m["w_eu"] = inp["w_e_up"][ls]; m["w_ed"] = inp["w_e_down"][ls]
    m["fnorm"] = inp["final_norm_g"].reshape(1, D)
    return m


def build_program(depth=DEPTH, debug_dump=False):
    nc = bass.Bass("TRN2", target_bir_lowering=False)
    p = P(nc)
    C = declare_io(p, depth)
    declare_scratch(p, C)
    declare_scratch2(p, C)
    stage_load(p, C)
    stage_M(p, C)
    outs = [C.out]
    for l in range(depth):
        last = (l == depth - 1) and not debug_dump
        with_ctx = not last
        stage_A(p, C, l)
        stage_SC(p, C, l, with_ctx)
        stage_ATT(p, C, l, with_ctx)
        stage_GDN(p, C, l)
        stage_D(p, C, l, with_ctx)
        stage_E(p, C, l, with_ctx)
        stage_F(p, C, l, with_ctx, final=False)
    if debug_dump:
        outs.append(dump(p, C.h, C.h[:], "d_h", [TA, D]))
        outs.append(dump(p, C.acc, C.acc[:], "d_acc", [TA, D]))
        outs.append(dump(p, C.affT, C.affT[:], "d_affT", [NE, TA]))
    stage_final(p, C)
    p.finish(outs)
    p.close()
    return nc, p


def stage_final(p, C):
    mk = p.mark()
    fg = p.sb("fgZ", [128, D]); bc_load(p, "act", fg, fg[:], C.fnorm, C.fnorm.ap[0:1, :])
    hb = [p.sb(f"hbZ{i}", [128, D]) for i in range(2)]; ac = [p.sb(f"acZ{i}", [128, D]) for i in range(2)]
    ss = [p.sb(f"ssZ{i}", [128, 2]) for i in range(2)]
    for ti in range(2, NT):
        ht = hb[ti % 2]; a_ = ac[ti % 2]; s_ = ss[ti % 2]
        p.dma("sp", ht, ht[:], C.hv[ti], C.hv[ti][:])
        p.op("pool", [], [s_], lambda e: e.memset(s_[:], 0.0))
        p.op("act", [ht], [a_, s_], lambda e: e.activation(out=a_[:], in_=ht[:], func=AF.Square, accum_out=s_[:, 0:1]))
        p.op("dve", [s_], [s_], lambda e: e.tensor_scalar(out=s_[:, 1:2], in0=s_[:, 0:1], scalar1=1.0 / D, scalar2=EPS, op0=ALU.mult, op1=ALU.add))
        p.op("act", [s_], [s_], lambda e: e.sqrt(out=s_[:, 1:2], in_=s_[:, 1:2]))
        p.op("dve", [s_], [s_], lambda e: e.reciprocal(out=s_[:, 1:2], in_=s_[:, 1:2]))
        p.op("dve", [ht, s_, fg], [a_], lambda e: e.scalar_tensor_tensor(out=a_[:], in0=ht[:], scalar=s_[:, 1:2], in1=fg[:], op0=ALU.mult, op1=ALU.mult))
        r0 = ti * 128 - TC
        p.dma("act", C.out, C.out.ap[r0:r0 + 128, :], a_, a_[:])
    p.barrier()
    p.release(mk)


def kernel(**inputs):
    inp = {k: np.asarray(v) for k, v in inputs.items()}
    nc, _ = get_prog("main", lambda: build_program(DEPTH))
    maps = [make_inputs(inp, b) for b in range(2)]
    res = launch(nc, maps)
    out = np.stack([np.asarray(res[b]["out"]) for b in range(2)], 0).astype(np.float32)
    return out
```

```python
import math
import numpy as np
import concourse.bass as bass
import concourse.mybir as mybir
from concourse.bass_utils import run_bass_kernel_spmd

F32 = mybir.dt.float32
BF16 = mybir.dt.bfloat16
I32 = mybir.dt.int32
U32 = mybir.dt.uint32
AF = mybir.ActivationFunctionType
ALU = mybir.AluOpType
AX = mybir.AxisListType


class T:
    __slots__ = ("ap", "w", "r", "name", "psum")

    def __init__(self, ap, name="", psum=False):
        self.ap = ap
        self.psum = psum
        self.w = None
        self.r = {}
        self.name = name

    def __getitem__(self, idx):
        return self.ap[idx]

    def view(self, idx, name=""):
        return T(self.ap[idx], name or self.name, self.psum)


class P:
    NDS = 8

    def __init__(self, nc):
        self.nc = nc
        self.eng = {"pe": nc.tensor, "act": nc.scalar, "dve": nc.vector, "pool": nc.gpsimd, "sp": nc.sync}
        self.sems = {}
        self.cnt = {}
        self._ctx = []
        for e in ("pe", "act", "dve", "pool"):
            self._mksem(e)
        for q in ("sp", "act", "pool"):
            for i in range(self.NDS):
                self._mksem(("d", q, i))
        self.dma_i = {"sp": 0, "act": 0, "pool": 0}
        self.seen = {e: {} for e in self.eng}
        self.n_instr = 0
        self.n_wait = 0

    def _mksem(self, key):
        name = "s_" + ("_".join(str(k) for k in key) if isinstance(key, tuple) else key)
        cm = self.nc.semaphore(name)
        s = cm.__enter__()
        self._ctx.append(cm)
        self.sems[key] = s
        self.cnt[key] = 0

    def _uniq(self, name):
        self._uid = getattr(self, "_uid", 0) + 1
        return f"{name}_{self._uid}"

    def sb(self, name, shape, dt=F32):
        name = self._uniq(name)
        cm = self.nc.sbuf_tensor(name, list(shape), dt)
        t = cm.__enter__()
        self._ctx.append(cm)
        return T(t, name)

    def ps(self, name, shape, dt=F32):
        name = self._uniq(name)
        cm = self.nc.psum_tensor(name, list(shape), dt)
        t = cm.__enter__()
        self._ctx.append(cm)
        return T(t, name, True)

    def dram(self, name, shape, dt, kind="Internal"):
        t = self.nc.dram_tensor(name, list(shape), dt, kind=kind)
        return T(t.ap(), name)

    def _wait(self, e, key, val):
        if val <= 0:
            return
        if self.seen[e].get(key, 0) >= val:
            return
        self.eng[e].wait_ge(self.sems[key], val)
        self.seen[e][key] = val
        self.n_wait += 1

    def _deps(self, e, mykey, reads, writes):
        for t in reads:
            if t.w is not None:
                self._wait(e, *t.w)
        for t in writes:
            if t.w is not None:
                self._wait(e, *t.w)
            for k, v in t.r.items():
                self._wait(e, k, v)

    def _mark(self, key, val, reads, writes):
        for t in reads:
            if t.r.get(key, 0) < val:
                t.r[key] = val
        for t in writes:
            t.w = (key, val)
            t.r = {}

    def op(self, e, reads, writes, fn):
        rp = [t for t in reads if t.psum and t not in writes]
        if rp:
            writes = list(writes) + rp
        self._deps(e, e, reads, writes)
        ins = fn(self.eng[e])
        self.cnt[e] += 1
        ins.then_inc(self.sems[e], 1)
        self._mark(e, self.cnt[e], reads, writes)
        self.n_instr += 1
        return ins

    def dma(self, q, out_t, out_ap, in_t, in_ap, **kw):
        i = self.dma_i[q] % self.NDS
        self.dma_i[q] += 1
        key = ("d", q, i)
        self._wait(q, key, self.cnt[key])
        self._deps(q, key, [in_t], [out_t])
        ins = self.eng[q].dma_start(out=out_ap, in_=in_ap, **kw)
        self.cnt[key] += 16
        ins.then_inc(self.sems[key], 16)
        self._mark(key, self.cnt[key], [in_t], [out_t])
        self.n_instr += 1
        return ins

    def _dma_generic(self, q, out_t, in_ts, issue):
        i = self.dma_i[q] % self.NDS
        self.dma_i[q] += 1
        key = ("d", q, i)
        self._wait(q, key, self.cnt[key])
        self._deps(q, key, in_ts, [out_t])
        ins = issue(self.eng[q])
        self.cnt[key] += 16
        ins.then_inc(self.sems[key], 16)
        self._mark(key, self.cnt[key], in_ts, [out_t])
        self.n_instr += 1
        return ins

    def dma_indirect_gather(self, out_t, out_ap, src_t, src_ap, idx_t, idx_ap):
        return self._dma_generic("pool", out_t, [src_t, idx_t], lambda g: g.indirect_dma_start(
            out=out_ap, out_offset=None, in_=src_ap, in_offset=bass.IndirectOffsetOnAxis(ap=idx_ap, axis=0)))

    def dma_indirect_scatter_add(self, dst_t, dst_ap, src_t, src_ap, idx_t, idx_ap):
        return self._dma_generic("pool", dst_t, [src_t, idx_t], lambda g: g.indirect_dma_start(
            out=dst_ap, out_offset=bass.IndirectOffsetOnAxis(ap=idx_ap, axis=0), in_=src_ap, in_offset=None,
            compute_op=ALU.add))

    def finish(self, out_ts, e="sp"):
        for t in out_ts:
            if t.w is not None:
                self._wait(e, *t.w)
        for key, v in self.cnt.items():
            self._wait(e, key, v)

    def mark(self):
        return len(self._ctx)

    def release(self, mark):
        while len(self._ctx) > mark:
            self._ctx.pop().__exit__(None, None, None)

    def barrier(self):
        for e in ("pe", "act", "dve", "pool", "sp"):
            for key, v in self.cnt.items():
                self._wait(e, key, v)

    def close(self):
        for cm in reversed(self._ctx):
            cm.__exit__(None, None, None)
        self._ctx = []


D = 2048
DEPTH = 4
EPS = 1e-6
N_IN = 6936
_PROGS = {}
N_LAUNCH = [0]


def get_prog(name, builder):
    if name not in _PROGS:
        _PROGS[name] = builder()
    return _PROGS[name]


def launch(nc, maps):
    N_LAUNCH[0] += 1
    res = run_bass_kernel_spmd(nc, maps, core_ids=list(range(len(maps))))
    return res.results


def rep128(v):
    return np.ascontiguousarray(np.broadcast_to(np.asarray(v, np.float32).reshape(1, -1), (128, v.size)))
TC = 256
TL = 4096
TA = TC + TL
NT = TA // 128
GH = 6
NCH = TA // 64
QKV0, Z0, B0c, A0c = 0, 2304, 3072, 3084
SC0 = 3096
CQ0, CK0, CV0 = 4632, 5400, 6168
NE = 16
FF = 1024
NEG = -1.0e30


class Ctx:
    pass


def declare_io(p, depth):
    C = Ctx()
    C.depth = depth
    ein = lambda n, s: p.dram(n, s, F32, kind="ExternalInput")
    C.x = ein("x", [TL, D]); C.ctx = ein("ctx", [TC, D])
    C.cT = ein("cT", [128, 16, 2])
    C.w_mod = ein("w_mod", [depth, D, 6 * D]); C.b_mod = ein("b_mod", [depth, 2, 6 * D])
    C.norm1 = ein("norm1", [depth, D]); C.norm2 = ein("norm2", [depth, D])
    C.w_in = ein("w_in", [depth, D, N_IN])
    C.gconv = ein("gconv", [depth, 128, 18, 3])
    C.galog = ein("galog", [depth, 64, 12]); C.gdtb = ein("gdtb", [depth, 64, 12])
    C.gnorm = ein("gnorm", [depth, 128, 1])
    C.sconv = ein("sconv", [depth, 128, 4, 3])
    C.dlam = ein("dlam", [depth, 128, 4, 64]); C.dnorm = ein("dnorm", [depth, 128, 1])
    C.w_out = ein("w_out", [depth, D, D]); C.w_r = ein("w_r", [depth, D, NE])
    C.w_eg = ein("w_eg", [depth, NE, D, FF]); C.w_eu = ein("w_eu", [depth, NE, D, FF]); C.w_ed = ein("w_ed", [depth, NE, FF, D])
    C.fnorm = ein("fnorm", [1, D])
    C.ident = ein("ident", [128, 128])
    C.cosT = ein("cosT", [128, TL]); C.sinT = ein("sinT", [128, TL])
    C.rotm = ein("rotm", [128, 128])
    C.tri = ein("tri", [2, 64, 64])
    C.nmask = ein("nmask", [4, 64, 64])
    C.out = p.dram("out", [TL, D], F32, kind="ExternalOutput")
    return C


def declare_scratch(p, C):
    C.h = p.dram("h_s", [TA, D], F32)
    C.hv = [C.h.view((slice(i * 128, (i + 1) * 128), slice(None))) for i in range(NT)]
    C.mod = p.dram("mod_s", [C.depth, 2, 6 * D], F32)
    C.uT = [p.dram(f"uT{c}", [128, TA], F32) for c in range(48)]
    C.uTv = [[t.view((slice(None), slice(hf * 2176, (hf + 1) * 2176))) for hf in range(2)] for t in C.uT]
    C.ba = p.dram("ba_s", [64, NCH, 24], F32)
    C.bav = [C.ba.view((slice(None), slice(hf * 34, (hf + 1) * 34), slice(None))) for hf in range(2)]
    C.cv = p.dram("cv_s", [TA, 768], BF16)
    C.cvv = [C.cv.view((slice(i * 128, (i + 1) * 128), slice(None))) for i in range(NT)]
    C.mixT = [p.dram(f"mixT{c}", [128, TA], BF16) for c in range(16)]


def bc_load(p, q, dst_t, dst_ap, src_t, src_row_ap, nparts=128):
    p.dma(q, dst_t, dst_ap, src_t, src_row_ap.partition_broadcast(nparts))


def stage_M(p, C):
    mk = p.mark()
    cT = p.sb("cT_s", [128, 16, 2]); sg = p.sb("sg", [128, 16, 2])
    bm = p.sb("bm_s", [2, 6 * D]); res = p.sb("resM", [2, 6 * D])
    wts = [p.sb(f"wtM{i}", [128, 4, 1024]) for i in range(3)]
    pss = [p.ps(f"psM{i}", [2, 512]) for i in range(4)]
    p.dma("sp", cT, cT[:], C.cT, C.cT[:])
    p.op("act", [cT], [sg], lambda e: e.activation(out=sg[:], in_=cT[:], func=AF.Sigmoid))
    p.op("dve", [cT, sg], [sg], lambda e: e.tensor_tensor(out=sg[:], in0=cT[:], in1=sg[:], op=ALU.mult))
    wi = 0
    for l in range(C.depth):
        p.dma("sp", bm, bm[:], C.b_mod, C.b_mod.ap[l])
        for ng in range(12):
            ps4 = pss[(ng % 2) * 2:(ng % 2) * 2 + 2]
            for kq in range(4):
                wt = wts[wi % 3]; wi += 1
                p.dma("sp" if wi % 2 else "act", wt, wt[:], C.w_mod,
                      C.w_mod.ap[l, kq * 512:(kq + 1) * 512, ng * 1024:(ng + 1) * 1024].rearrange("(k p) n -> p k n", p=128))
                for n in range(2):
                    for k in range(4):
                        p.op("pe", [sg, wt], [ps4[n]], lambda e: e.matmul(
                            ps4[n][:], lhsT=sg[:, kq * 4 + k, :], rhs=wt[:, k, n * 512:(n + 1) * 512],
                            start=(kq == 0 and k == 0), stop=(kq == 3 and k == 3)))
            for n in range(2):
                c0 = ng * 1024 + n * 512
                p.op("dve", [ps4[n], bm], [res], lambda e: e.tensor_tensor(
                    out=res[:, c0:c0 + 512], in0=ps4[n][:], in1=bm[:, c0:c0 + 512], op=ALU.add))
        p.dma("sp", C.mod, C.mod.ap[l], res, res[:])
    p.barrier()
    p.release(mk)


def modrow(C, l, kind, i):
    r = 1 if kind == 0 else 0
    return C.mod.ap[l, r:r + 1, i * D:(i + 1) * D]


def stage_A(p, C, l):
    mk = p.mark()
    gs = p.sb("gsA", [128, 2, D]); sh = p.sb("shA", [128, 2, D]); g1 = p.sb("g1A", [128, D])
    idf = p.sb("idfA", [128, 128]); idb = p.sb("idbA", [128, 128], BF16)
    aT = p.sb("aT", [128, 16, 2176], BF16)
    aTv = [aT.view((slice(None), slice(None), slice(i * 128, (i + 1) * 128))) for i in range(17)]
    p.dma("sp", idf, idf[:], C.ident, C.ident[:])
    p.op("dve", [idf], [idb], lambda e: e.tensor_copy(out=idb[:], in_=idf[:]))
    bc_load(p, "sp", g1, g1[:], C.norm1, C.norm1.ap[l:l + 1, :])
    for kind in range(2):
        bc_load(p, "act", gs, gs[:, kind, :], C.mod, modrow(C, l, kind, 1))
        bc_load(p, "sp", sh, sh[:, kind, :], C.mod, modrow(C, l, kind, 0))
    for kind in range(2):
        p.op("dve", [gs, g1], [gs], lambda e: e.scalar_tensor_tensor(
            out=gs[:, kind, :], in0=gs[:, kind, :], scalar=1.0, in1=g1[:], op0=ALU.add, op1=ALU.mult))
    for hf in range(2):
        mk1 = p.mark()
        hb = [p.sb(f"hbA{i}", [128, D]) for i in range(2)]
        yb = [p.sb(f"ybA{i}", [128, D]) for i in range(2)]
        ab = [p.sb(f"abA{i}", [128, D], BF16) for i in range(2)]
        ss = [p.sb(f"ssA{i}", [128, 2]) for i in range(2)]
        ptr = [p.ps(f"ptrA{i}", [128, 4, 128], BF16) for i in range(4)]
        tcount = 0
        for tt in range(17):
            ti = hf * 17 + tt
            kind = 0 if ti < 2 else 1
            ht = hb[tt % 2]; y_ = yb[tt % 2]; a_ = ab[tt % 2]; s_ = ss[tt % 2]
            p.dma("sp", ht, ht[:], C.hv[ti], C.hv[ti][:])
            p.op("pool", [], [s_], lambda e: e.memset(s_[:], 0.0))
            p.op("act", [ht], [y_, s_], lambda e: e.activation(out=y_[:], in_=ht[:], func=AF.Square, accum_out=s_[:, 0:1]))
            p.op("dve", [s_], [s_], lambda e: e.tensor_scalar(out=s_[:, 1:2], in0=s_[:, 0:1], scalar1=1.0 / D, scalar2=EPS, op0=ALU.mult, op1=ALU.add))
            p.op("act", [s_], [s_], lambda e: e.sqrt(out=s_[:, 1:2], in_=s_[:, 1:2]))
            p.op("dve", [s_], [s_], lambda e: e.reciprocal(out=s_[:, 1:2], in_=s_[:, 1:2]))
            p.op("dve", [ht, s_, gs], [y_], lambda e: e.scalar_tensor_tensor(
                out=y_[:], in0=ht[:], scalar=s_[:, 1:2], in1=gs[:, kind, :], op0=ALU.mult, op1=ALU.mult))
            p.op("pool", [y_, sh], [a_], lambda e: e.tensor_tensor(out=a_[:], in0=y_[:], in1=sh[:, kind, :], op=ALU.add))
            for k4 in range(4):
                pt = ptr[tcount % 4]; tcount += 1
                for kk in range(4):
                    k = k4 * 4 + kk
                    p.op("pe", [a_, idb], [pt], lambda e: e.transpose(out=pt[:, kk, :], in_=a_[:, k * 128:(k + 1) * 128], identity=idb[:]))
                p.op("act", [pt], [aTv[tt]], lambda e: e.copy(out=aT[:, k4 * 4:(k4 + 1) * 4, tt * 128:(tt + 1) * 128], in_=pt[:]))
        p.barrier()
        p.release(mk1)
        aTall = [aT] + aTv
        mk2 = p.mark()
        wb = [p.sb(f"wbA{i}", [128, 16, 512], BF16) for i in range(2)]
        wbv = [(w.view((slice(None), slice(0, 8), slice(None))), w.view((slice(None), slice(8, 16), slice(None)))) for w in wb]
        stg = [p.sb(f"stgA{i}", [128, 2176]) for i in range(4)]
        pu = [p.ps(f"puA{i}", [128, 512]) for i in range(8)]
        wcount = 0; setc = 0; scount = 0; ec = 0

        def load_w(c0, nw):
            nonlocal wcount
            wv = wbv[wcount % 2]; wcount += 1
            for half in range(2):
                p.dma("pool", wv[half], wv[half][:, :, :nw], C.w_in,
                      C.w_in.ap[l, half * 1024:(half + 1) * 1024, c0:c0 + nw].rearrange("(k p) n -> p k n", p=128))
            return wv

        def evac(pp, dst_t, dst_ap, src_ap):
            nonlocal ec
            ec += 1
            if ec % 2:
                p.op("act", [pp], [dst_t], lambda e: e.copy(out=dst_ap, in_=src_ap))
            else:
                p.op("dve", [pp], [dst_t], lambda e: e.tensor_copy(out=dst_ap, in_=src_ap))
        fm_cols = [QKV0 + 128 * i for i in range(24)] + [SC0 + 128 * i for i in range(12)] + [CQ0 + 128 * i for i in range(12)]
        for g4 in range(12):
            c0 = fm_cols[g4 * 4]
            wv = load_w(c0, 512)
            sts = [stg[cc] for cc in range(4)]
            for cc in range(4):
                bank = pu[(setc % 2) * 4:(setc % 2) * 4 + 4]; setc += 1
                for k in range(16):
                    for tb in range(4):
                        p.op("pe", aTall + [wv[k // 8]], [bank[tb]], lambda e: e.matmul(
                            bank[tb][:, :], lhsT=wv[k // 8][:, k % 8, cc * 128:(cc + 1) * 128], rhs=aT[:, k, tb * 512:(tb + 1) * 512], start=(k == 0), stop=(k == 15)))
                for tb in range(4):
                    evac(bank[tb], sts[cc], sts[cc][:, tb * 512:(tb + 1) * 512], bank[tb][:, :])
            bank = pu[(setc % 2) * 4:(setc % 2) * 4 + 4]; setc += 1
            for k in range(16):
                for cc in range(4):
                    p.op("pe", aTall + [wv[k // 8]], [bank[cc]], lambda e: e.matmul(
                        bank[cc][:, :128], lhsT=wv[k // 8][:, k % 8, cc * 128:(cc + 1) * 128], rhs=aT[:, k, 2048:2176], start=(k == 0), stop=(k == 15)))
            for cc in range(4):
                evac(bank[cc], sts[cc], sts[cc][:, 2048:2176], bank[cc][:, :128])
                ch = g4 * 4 + cc
                p.dma("sp" if cc % 2 else "act", C.uTv[ch][hf], C.uTv[ch][hf][:], sts[cc], sts[cc][:])
        for vg in range(2):
            nw = 512 if vg == 0 else 256
            wv = load_w(CV0 + vg * 512, nw)
            for t4 in range(0, 17, 4):
                tl = list(range(t4, min(t4 + 4, 17)))
                bank = pu[(setc % 2) * 4:(setc % 2) * 4 + 4]; setc += 1
                for k in range(16):
                    for j, tt in enumerate(tl):
                        p.op("pe", aTall + [wv[k // 8]], [bank[j]], lambda e: e.matmul(
                            bank[j][:, :nw], lhsT=aT[:, k, tt * 128:(tt + 1) * 128], rhs=wv[k // 8][:, k % 8, :nw], start=(k == 0), stop=(k == 15)))
                for j, tt in enumerate(tl):
                    ti = hf * 17 + tt
                    st = stg[scount % 4]; scount += 1
                    evac(bank[j], st, st[:, :nw], bank[j][:, :nw])
                    p.dma("pool", C.cvv[ti], C.cvv[ti][:, vg * 512:vg * 512 + nw], st, st[:, :nw])
        wv = load_w(B0c, 24)
        bst = p.sb("bstA", [64, 34, 24])
        for c4 in range(0, 34, 4):
            cl = list(range(c4, min(c4 + 4, 34)))
            bank = pu[(setc % 2) * 4:(setc % 2) * 4 + 4]; setc += 1
            for k in range(16):
                for j, cj in enumerate(cl):
                    p.op("pe", aTall + [wv[k // 8]], [bank[j]], lambda e: e.matmul(
                        bank[j][:64, :24], lhsT=aT[:, k, cj * 64:(cj + 1) * 64], rhs=wv[k // 8][:, k % 8, :24], start=(k == 0), stop=(k == 15)))
            for j, cj in enumerate(cl):
                p.op("dve", [bank[j]], [bst], lambda e: e.tensor_copy(out=bst[:, cj, :], in_=bank[j][:64, :24]))
        p.dma("sp", C.bav[hf], C.bav[hf][:], bst, bst[:])
        p.barrier()
        p.release(mk2)
    p.release(mk)


def stage_load(p, C):
    p.dma("sp", C.h, C.h.ap[0:TC, :], C.ctx, C.ctx[:])
    for i in range(4):
        p.dma("act" if i % 2 else "sp", C.h, C.h.ap[TC + i * 1024:TC + (i + 1) * 1024, :], C.x, C.x.ap[i * 1024:(i + 1) * 1024, :])
    p.barrier()


def dump(p, src_t, src_ap, name, shape, dt=F32):
    o = p.dram(name, shape, dt, kind="ExternalOutput")
    p.dma("sp", o, o[:], src_t, src_ap)
    return o


def stage_SC(p, C, l, with_ctx=True):
    mk = p.mark()
    cw = p.sb("cwSC", [128, 4, 3])
    p.dma("sp", cw, cw[:], C.sconv, C.sconv.ap[l])
    bufs = [[p.sb(f"sc{n}{i}", [128, TA]) for n in ("b", "c", "x", "y")] for i in range(2)]
    ob = [p.sb(f"scO{i}", [128, TA], BF16) for i in range(2)]
    segs = [(0, TC), (TC, TA)]
    for c in range(4):
        bg, cg, xi, y = bufs[c % 2]
        o_ = ob[c % 2]
        p.dma("sp", bg, bg[:], C.uT[24 + c], C.uT[24 + c][:])
        p.dma("act", cg, cg[:], C.uT[28 + c], C.uT[28 + c][:])
        p.dma("sp", xi, xi[:], C.uT[32 + c], C.uT[32 + c][:])
        p.op("dve", [cg, xi], [cg], lambda e: e.tensor_tensor(out=cg[:], in0=cg[:], in1=xi[:], op=ALU.mult))
        p.op("act", [cg, cw], [y], lambda e: e.mul(out=y[:], in_=cg[:], mul=cw[:, c, 1:2]))
        for (s0, s1) in segs:
            p.op("dve", [cg, cw, y], [y], lambda e: e.scalar_tensor_tensor(
                out=y[:, s0 + 1:s1], in0=cg[:, s0:s1 - 1], scalar=cw[:, c, 0:1], in1=y[:, s0 + 1:s1], op0=ALU.mult, op1=ALU.add))
            p.op("dve", [cg, cw, y], [y], lambda e: e.scalar_tensor_tensor(
                out=y[:, s0:s1 - 1], in0=cg[:, s0 + 1:s1], scalar=cw[:, c, 2:3], in1=y[:, s0:s1 - 1], op0=ALU.mult, op1=ALU.add))
        p.op("pool", [bg, y], [o_], lambda e: e.tensor_tensor(out=o_[:], in0=bg[:], in1=y[:], op=ALU.mult))
        p.dma("sp", C.mixT[6 + c], C.mixT[6 + c][:], o_, o_[:])
    p.barrier()
    p.release(mk)


def stage_ATT(p, C, l, with_ctx=True):
    lam_init = 0.8 - 0.6 * math.exp(-0.3 * l)
    mk = p.mark()
    onesb = p.sb("onesB", [128, 128], BF16)
    p.op("pool", [], [onesb], lambda e: e.memset(onesb[:], 1.0))
    onesf = p.sb("onesF", [128, 128])
    p.op("pool", [], [onesf], lambda e: e.memset(onesf[:], 1.0))
    accs = [p.sb(f"accA{i}", [128, 512]) for i in range(4)]
    rot = p.sb("rotA", [128, 128])
    p.dma("sp", rot, rot[:], C.rotm, C.rotm[:])
    cosT = p.sb("cosA", [128, TL]); sinT = p.sb("sinA", [128, TL])
    p.dma("sp", cosT, cosT[:], C.cosT, C.cosT[:])
    p.dma("act", sinT, sinT[:], C.sinT, C.sinT[:])
    dn = p.sb("dnA", [128, 1])
    p.dma("sp", dn, dn[:], C.dnorm, C.dnorm.ap[l])
    p.op("dve", [dn], [dn], lambda e: e.tensor_scalar(out=dn[:], in0=dn[:], scalar1=(1.0 - lam_init), scalar2=None, op0=ALU.mult))
    dl = p.sb("dlA", [128, 4, 64]); lw = p.sb("lwA", [128, 8])
    p.dma("sp", dl, dl[:], C.dlam, C.dlam.ap[l])
    p.op("dve", [dl], [dl], lambda e: e.tensor_tensor(out=dl[:, 0, :], in0=dl[:, 0, :], in1=dl[:, 1, :], op=ALU.mult))
    p.op("dve", [dl], [dl], lambda e: e.tensor_tensor(out=dl[:, 2, :], in0=dl[:, 2, :], in1=dl[:, 3, :], op=ALU.mult))
    p.op("dve", [dl], [lw], lambda e: e.reduce_sum(out=lw[:, 0:1], in_=dl[:, 0, :], axis=AX.X))
    p.op("dve", [dl], [lw], lambda e: e.reduce_sum(out=lw[:, 1:2], in_=dl[:, 2, :], axis=AX.X))
    p.op("act", [lw], [lw], lambda e: e.activation(out=lw[:, 2:4], in_=lw[:, 0:2], func=AF.Exp))
    p.op("dve", [lw], [lw], lambda e: e.tensor_tensor(out=lw[:, 4:5], in0=lw[:, 3:4], in1=lw[:, 2:3], op=ALU.subtract))
    p.op("dve", [lw], [lw], lambda e: e.tensor_scalar(out=lw[:, 5:6], in0=lw[:, 4:5], scalar1=-lam_init, scalar2=None, op0=ALU.add))
    neg_lam = lw
    xq = [p.sb(f"xqA{i}", [128, TA]) for i in range(2)]
    qTr = p.sb("qTrA", [128, TA], BF16); kTr = p.sb("kTrA", [128, TA], BF16)
    vsb = p.sb("vA", [128, NT, 128], BF16)
    tmp = [p.sb(f"tmpA{i}", [128, 512]) for i in range(2)]
    PT = [p.sb(f"PT{i}", [128, 512], BF16) for i in range(4)]
    ep = [p.sb(f"epA{i}", [128, 512]) for i in range(4)]
    sqb = p.sb("sqA", [128, 512], BF16)
    ost = [p.sb(f"ostA{i}", [128, 512], BF16) for i in range(2)]
    pS = [p.ps(f"pS{i}", [128, 512]) for i in range(3)]
    pO = [p.ps(f"pO{i}", [128, 512]) for i in range(2)]
    pD = [p.ps(f"pD{i}", [128, 512]) for i in range(2)]
    pR = p.ps("pR", [128, 512])
    sc_i = 0; pt_i = 0
    for h in range(GH):
        for which, (src, dst) in enumerate(((C.uT[36 + h], qTr), (C.uT[42 + h], kTr))):
            x_ = xq[which]
            p.dma("sp" if which == 0 else "act", x_, x_[:], src, src[:])
            p.op("pool", [x_], [dst], lambda e: e.tensor_copy(out=dst[:, 0:TC], in_=x_[:, 0:TC]))
            for qb in range(8):
                c0 = TC + qb * 512
                p.op("pe", [rot, x_], [pR], lambda e: e.matmul(pR[:], lhsT=rot[:], rhs=x_[:, c0:c0 + 512], start=True, stop=True))
                t_ = tmp[qb % 2]
                p.op("dve", [pR, sinT], [t_], lambda e: e.tensor_tensor(out=t_[:], in0=pR[:], in1=sinT[:, qb * 512:(qb + 1) * 512], op=ALU.mult))
                p.op("pool", [x_, cosT], [x_], lambda e: e.tensor_tensor(out=x_[:, c0:c0 + 512], in0=x_[:, c0:c0 + 512], in1=cosT[:, qb * 512:(qb + 1) * 512], op=ALU.mult))
                p.op("dve", [x_, t_], [dst], lambda e: e.tensor_tensor(out=dst[:, c0:c0 + 512], in0=x_[:, c0:c0 + 512], in1=t_[:], op=ALU.add))
        p.dma("sp", vsb, vsb[:], C.cv, C.cv.ap[:, h * 128:(h + 1) * 128].rearrange("(t p) d -> p t d", p=128))
        blocks = [(TC + qb * 512, 512, list(range(NT))) for qb in range(8)]
        if with_ctx:
            blocks.append((0, TC, [0, 1]))
        for bi, (q0, qw, ktiles) in enumerate(blocks):
            for m in range(2):
                po = pO[m]; pd = pD[m]
                ac = accs[(bi % 2) * 2 + m]
                nk = len(ktiles)

                def qk(ki):
                    nonlocal sc_i
                    kt = ktiles[ki]
                    ps_ = pS[sc_i % 3]; sc_i += 1
                    p.op("pe", [kTr, qTr], [ps_], lambda e: e.matmul(
                        ps_[:, :qw], lhsT=kTr[m * 64:(m + 1) * 64, kt * 128:(kt + 1) * 128], rhs=qTr[m * 64:(m + 1) * 64, q0:q0 + qw], start=True, stop=True))
                    return ps_
                pend = [qk(0)]
                if nk > 1:
                    pend.append(qk(1))
                for ki, kt in enumerate(ktiles):
                    ps_ = pend.pop(0)
                    if ki + 2 < nk:
                        pend.append(qk(ki + 2))
                    pt = PT[pt_i % 4]; pt_i += 1
                    p.op("act", [ps_], [pt], lambda e: e.activation(out=pt[:, :qw], in_=ps_[:, :qw], func=AF.Exp, scale=0.125))
                    p.op("pe", [vsb, pt], [po], lambda e: e.matmul(po[:, :qw], lhsT=vsb[:, kt, :], rhs=pt[:, :qw], start=(ki == 0), stop=(ki == nk - 1)))
                    if ki == 0:
                        p.op("dve", [pt], [ac], lambda e: e.tensor_copy(out=ac[:, :qw], in_=pt[:, :qw]))
                    else:
                        p.op("dve", [pt, ac], [ac], lambda e: e.tensor_tensor(out=ac[:, :qw], in0=ac[:, :qw], in1=pt[:, :qw], op=ALU.add))
                p.op("pe", [onesf, ac], [pd], lambda e: e.matmul(pd[:, :qw], lhsT=onesf[:], rhs=ac[:, :qw], start=True, stop=True))
            e0, e1, e2, e3 = ep
            p.op("dve", [pD[0]], [e0], lambda e: e.reciprocal(out=e0[:, :qw], in_=pD[0][:, :qw]))
            p.op("dve", [pO[0], e0], [e0], lambda e: e.tensor_tensor(out=e0[:, :qw], in0=pO[0][:, :qw], in1=e0[:, :qw], op=ALU.mult))
            p.op("dve", [pD[1]], [e1], lambda e: e.reciprocal(out=e1[:, :qw], in_=pD[1][:, :qw]))
            p.op("dve", [pO[1], e1], [e1], lambda e: e.tensor_tensor(out=e1[:, :qw], in0=pO[1][:, :qw], in1=e1[:, :qw], op=ALU.mult))
            p.op("dve", [e0, e1, neg_lam], [e2], lambda e: e.scalar_tensor_tensor(
                out=e2[:, :qw], in0=e1[:, :qw], scalar=neg_lam[:, 5:6], in1=e0[:, :qw], op0=ALU.mult, op1=ALU.add))
            p.op("pool", [e2], [sqb], lambda e: e.tensor_tensor(out=sqb[:, :qw], in0=e2[:, :qw], in1=e2[:, :qw], op=ALU.mult))
            p.op("pe", [onesb, sqb], [pR], lambda e: e.matmul(pR[:, :qw], lhsT=onesb[:], rhs=sqb[:, :qw], start=True, stop=True))
            p.op("dve", [pR], [e3], lambda e: e.tensor_scalar(out=e3[:, :qw], in0=pR[:, :qw], scalar1=1.0 / 128, scalar2=EPS, op0=ALU.mult, op1=ALU.add))
            p.op("act", [e3], [e3], lambda e: e.activation(out=e3[:, :qw], in_=e3[:, :qw], func=AF.Ln))
            p.op("act", [e3], [e3], lambda e: e.activation(out=e3[:, :qw], in_=e3[:, :qw], func=AF.Exp, scale=-0.5))
            o_ = ost[bi % 2]
            p.op("dve", [e2, e3, dn], [o_], lambda e: e.scalar_tensor_tensor(
                out=o_[:, :qw], in0=e2[:, :qw], scalar=dn[:, 0:1], in1=e3[:, :qw], op0=ALU.mult, op1=ALU.mult))
            p.dma("sp", C.mixT[10 + h], C.mixT[10 + h][:, q0:q0 + qw], o_, o_[:, :qw])
    p.barrier()
    p.release(mk)


INV_DT = F32


def gdn_windows(d):
    lat = [(4 + 8 * i, 8) for i in range(8)]
    if d == 0:
        return [(0, 4)] + lat
    return [(0, 4)] + lat[::-1]


def stage_GDN(p, C, l, dbg=99):
    mk = p.mark()
    C.oTd = getattr(C, "oTd", None) or [[p.dram(f"oTd{d}_{h}", [128, TA], F32) for h in range(GH)] for d in range(2)]
    idf = p.sb("idG", [128, 128]); p.dma("sp", idf, idf[:], C.ident, C.ident[:])
    tri = p.sb("triG", [64, 2, 64]); p.dma("sp", tri, tri[:], C.tri, C.tri.ap.rearrange("d p i -> p d i"))
    nm = p.sb("nmG", [64, 4, 64]); p.dma("act", nm, nm[:], C.nmask, C.nmask.ap.rearrange("d p i -> p d i"))
    st01 = p.sb("st01G", [64, 2, 64])
    for d in range(2):
        p.op("dve", [tri, idf], [st01], lambda e: e.tensor_tensor(out=st01[:, d, :], in0=tri[:, d, :], in1=idf[:64, :64], op=ALU.subtract))
    ones = p.sb("onesG", [128, 128]); p.op("pool", [], [ones], lambda e: e.memset(ones[:], 1.0))
    cw = p.sb("cwG", [128, 18, 3]); p.dma("sp", cw, cw[:], C.gconv, C.gconv.ap[l])
    gn = p.sb("gnG", [128, 1]); p.dma("sp", gn, gn[:], C.gnorm, C.gnorm.ap[l])
    if dbg == -3:
        C.dbg = [(st01, st01[:].rearrange("p a i -> p (a i)"), [64, 128]), (nm, nm[:].rearrange("p a i -> p (a i)"), [64, 256])]
        return
    ba = p.sb("baG", [64, NCH, 24]); p.dma("sp", ba, ba[:], C.ba, C.ba[:])
    alog = p.sb("alogG", [64, 12]); p.dma("act", alog, alog[:], C.galog, C.galog.ap[l])
    dtb = p.sb("dtbG", [64, 12]); p.dma("act", dtb, dtb[:], C.gdtb, C.gdtb.ap[l])
    NG = NCH * GH
    bsig = p.sb("bsigG", [64, 2, NCH, GH]); gval = p.sb("gvalG", [64, 2, NCH, GH])
    gc = p.sb("gcG", [64, 2, NCH, GH]); gtot = p.sb("gtotG", [64, 2, NCH, GH])
    egc = p.sb("egcG", [64, 2, NCH, GH]); ekd = p.sb("ekdG", [64, 2, NCH, GH]); egl = p.sb("eglG", [128, 2, NCH, GH])
    pp = [p.ps(f"ppG{i}", [128, 512]) for i in range(2)]
    psA = [p.ps(f"psA{h}", [128, 512]) for h in range(GH)]
    ppi = [0]

    def nextpp():
        t = pp[ppi[0] % 2]; ppi[0] += 1
        return t
    p.op("act", [alog], [alog], lambda e: e.activation(out=alog[:], in_=alog[:], func=AF.Exp))
    for d in range(2):
        bsl = ba[:, :, d * 6:(d + 1) * 6]
        asl = ba[:, :, 12 + d * 6:12 + (d + 1) * 6]
        p.op("act", [ba], [bsig], lambda e: e.activation(out=bsig[:, d], in_=bsl, func=AF.Sigmoid))
        p.op("dve", [ba, dtb], [gval], lambda e: e.tensor_tensor(out=gval[:, d], in0=asl, in1=dtb[:, d * 6:(d + 1) * 6].unsqueeze(1).to_broadcast([64, NCH, GH]), op=ALU.add))
        p.op("act", [gval], [gval], lambda e: e.activation(out=gval[:, d], in_=gval[:, d], func=AF.Exp))
        p.op("dve", [gval], [gval], lambda e: e.tensor_scalar(out=gval[:, d], in0=gval[:, d], scalar1=1.0, scalar2=None, op0=ALU.add))
        p.op("act", [gval], [gval], lambda e: e.activation(out=gval[:, d], in_=gval[:, d], func=AF.Ln))
        p.op("dve", [gval, alog], [gval], lambda e: e.scalar_tensor_tensor(
            out=gval[:, d], in0=gval[:, d], scalar=-1.0, in1=alog[:, d * 6:(d + 1) * 6].unsqueeze(1).to_broadcast([64, NCH, GH]), op0=ALU.mult, op1=ALU.mult))
        if dbg == -2:
            C.dbg = [(gval, gval[:].rearrange("p a n h -> p (a n h)"), [64, 2 * NG]), (bsig, bsig[:].rearrange("p a n h -> p (a n h)"), [64, 2 * NG])]
            return
        t = nextpp()
        p.op("pe", [tri, gval], [t], lambda e: e.matmul(t[:64, :NG], lhsT=tri[:, d, :], rhs=gval[:, d].rearrange("p n h -> p (n h)"), start=True, stop=True))
        p.op("dve", [t], [gc], lambda e: e.tensor_copy(out=gc[:, d].rearrange("p n h -> p (n h)"), in_=t[:64, :NG]))
        if dbg == -1:
            C.dbg = [(gc, gc[:].rearrange("p a n h -> p (a n h)"), [64, 2 * NG])]
            return
        t2 = nextpp()
        p.op("pe", [ones, gval], [t2], lambda e: e.matmul(t2[:, :NG], lhsT=ones[:64, :], rhs=gval[:, d].rearrange("p n h -> p (n h)"), start=True, stop=True))
        p.op("dve", [t2], [gtot], lambda e: e.tensor_copy(out=gtot[:, d].rearrange("p n h -> p (n h)"), in_=t2[:64, :NG]))
        p.op("act", [t2], [egl], lambda e: e.activation(out=egl[:, d].rearrange("p n h -> p (n h)"), in_=t2[:, :NG], func=AF.Exp))
        p.op("act", [gc], [egc], lambda e: e.activation(out=egc[:, d], in_=gc[:, d], func=AF.Exp))
        p.op("dve", [gtot, gc], [ekd], lambda e: e.tensor_tensor(out=ekd[:, d], in0=gtot[:, d], in1=gc[:, d], op=ALU.subtract))
        p.op("act", [ekd], [ekd], lambda e: e.activation(out=ekd[:, d], in_=ekd[:, d], func=AF.Exp))
    if dbg < 1:
        C.dbg = [(gval, gval[:].rearrange("p a n h -> p (a n h)"), [64, 2 * NG]), (gc, gc[:].rearrange("p a n h -> p (a n h)"), [64, 2 * NG])]
        return
    H = []
    for h in range(GH):
        hb = Ctx()
        hb.wT = p.sb(f"wT{h}", [128, 8, 64], BF16); hb.u = p.sb(f"u{h}", [64, 8, 128])
        hb.attnT = p.sb(f"attnT{h}", [64, 8, 64], BF16); hb.kdec = p.sb(f"kdec{h}", [64, 8, 128], BF16)
        hb.qdT = p.sb(f"qdT{h}", [128, 8, 64], BF16); hb.oT = p.sb(f"oTw{h}", [128, 8, 64])
        hb.S = p.sb(f"S{h}", [128, 128]); hb.Sb = p.sb(f"Sb{h}", [128, 128], BF16)
        hb.vn = p.sb(f"vn{h}", [64, 128], BF16)
        hb.ps1 = psA[h].view((slice(0, 64), slice(0, 128)))
        hb.psO = psA[h].view((slice(None), slice(128, 192)))
        hb.psS = psA[h].view((slice(None), slice(256, 384)))
        H.append(hb)
    xrs = [p.sb(f"xrG{j}", [128, 514]) for j in range(3)]; cy = p.sb("cyG", [128, 3, 512]); sq = p.sb("sqG", [128, 512])
    rs = p.sb("rsG", [128, 512]); qn = p.sb("qnG", [128, 512]); kn = p.sb("knG", [128, 512])
    ktok = p.sb("ktokG", [64, 8, 128]); vtok = p.sb("vtokG", [64, 8, 128])
    Gm = p.sb("GmG", [64, 8, 64]); Bm = p.sb("BmG", [64, 8, 64])
    diff = p.sb("diffG", [64, 8, 64]); DT = p.sb("DTG", [64, 8, 64]); Ds2 = p.sb("Ds2G", [64, 8, 64])
    bbc = p.sb("bbcG", [64, 8, 64]); mkk = p.sb("mkkG", [64, 8, 64])
    LT = p.sb("LTG", [64, 8, 64]); Lm = p.sb("LmG", [64, 8, 64])
    Xa = [p.sb(f"XaG{i}", [64, 8, 64], INV_DT) for i in range(2)]; Xb = [p.sb(f"XbG{i}", [64, 8, 64], INV_DT) for i in range(2)]
    Rr = [p.sb(f"RG{i}", [64, 8, 64]) for i in range(2)]
    Rb = [p.sb(f"RbG{i}", [64, 8, 64], INV_DT) for i in range(2)]
    LTb = p.sb("LTbG", [64, 8, 64], INV_DT); Lmb = p.sb("LmbG", [64, 8, 64], INV_DT)
    kbs = [p.sb(f"kbG{i}", [64, 8, 128]) for i in range(2)]; vbs = [p.sb(f"vbG{i}", [64, 8, 128]) for i in range(2)]
    psC = psA[:4]; ci = [0]
    SEG = [(0, TC), (TC, TA)]

    def bcn(ap2, nc_, w):
        return ap2.unsqueeze(2).to_broadcast([64, nc_, w])

    def front_gen(h, d, c0, nc_, par):
        hb = H[h]
        W = nc_ * 64; t0 = c0 * 64
        s0, s1 = SEG[0] if c0 < 4 else SEG[1]
        lo = 1 if t0 == s0 else 0
        hi = 1 if t0 + W == s1 else 0
        for j, ch in enumerate((h, 6 + h, 12 + h)):
            xr = xrs[j]
            if lo or hi:
                p.op("pool", [], [xr], lambda e: e.memset(xr[:], 0.0))
            p.dma("sp" if j != 1 else "act", xr, xr[:, lo:W + 2 - hi], C.uT[ch], C.uT[ch][:, t0 - 1 + lo:t0 + W + 1 - hi])
        for j, ch in enumerate((h, 6 + h, 12 + h)):
            xr = xrs[j]
            p.op("act", [xr, cw], [cy], lambda e: e.mul(out=cy[:, j, :W], in_=xr[:, 1:W + 1], mul=cw[:, ch, 1:2]))
            p.op("dve", [xr, cw, cy], [cy], lambda e: e.scalar_tensor_tensor(
                out=cy[:, j, :W], in0=xr[:, 0:W], scalar=cw[:, ch, 0:1], in1=cy[:, j, :W], op0=ALU.mult, op1=ALU.add))
            p.op("dve", [xr, cw, cy], [cy], lambda e: e.scalar_tensor_tensor(
                out=cy[:, j, :W], in0=xr[:, 2:W + 2], scalar=cw[:, ch, 2:3], in1=cy[:, j, :W], op0=ALU.mult, op1=ALU.add))
            p.op("act", [cy], [cy], lambda e: e.activation(out=cy[:, j, :W], in_=cy[:, j, :W], func=AF.Silu))
        yield 0
        for j, dst, scl in ((0, qn, 128.0 ** -0.5), (1, kn, 1.0)):
            yield 0
            p.op("pool", [cy], [sq], lambda e: e.tensor_tensor(out=sq[:, :W], in0=cy[:, j, :W], in1=cy[:, j, :W], op=ALU.mult))
            t = nextpp()
            p.op("pe", [ones, sq], [t], lambda e: e.matmul(t[:, :W], lhsT=ones[:], rhs=sq[:, :W], start=True, stop=True))
            p.op("dve", [t], [rs], lambda e: e.tensor_scalar(out=rs[:, :W], in0=t[:, :W], scalar1=EPS, scalar2=None, op0=ALU.add))
            p.op("act", [rs], [rs], lambda e: e.activation(out=rs[:, :W], in_=rs[:, :W], func=AF.Ln))
            p.op("act", [rs], [rs], lambda e: e.activation(out=rs[:, :W], in_=rs[:, :W], func=AF.Exp, scale=-0.5))
            p.op("dve", [cy, rs], [dst], lambda e: e.scalar_tensor_tensor(
                out=dst[:, :W], in0=cy[:, j, :W], scalar=scl, in1=rs[:, :W], op0=ALU.mult, op1=ALU.mult))
        yield 0
        for src_t, src_ap, dst in ((kn, lambda n: kn[:, n * 64:(n + 1) * 64], ktok), (cy, lambda n: cy[:, 2, n * 64:(n + 1) * 64], vtok)):
            for g in range(0, nc_, 4):
                t = nextpp()
                tv = t[:64, :].rearrange("p (n d) -> p n d", d=128)
                for n in range(g, min(g + 4, nc_)):
                    p.op("pe", [src_t, idf], [t], lambda e: e.transpose(out=tv[:, n - g, :], in_=src_ap(n), identity=idf[:]))
                ne = min(4, nc_ - g)
                p.op("act", [t], [dst], lambda e: e.copy(out=dst[:, g:g + ne, :], in_=tv[:, :ne, :]))
        yield 0
        g_ = gval[:, d, c0:c0 + nc_, h]; b_ = bsig[:, d, c0:c0 + nc_, h]; gc_ = gc[:, d, c0:c0 + nc_, h]
        p.op("dve", [gval, tri], [Gm], lambda e: e.tensor_tensor(out=Gm[:, :nc_, :], in0=bcn(g_, nc_, 64), in1=tri[:, d, :].unsqueeze(1).to_broadcast([64, nc_, 64]), op=ALU.mult))
        p.op("pool", [bsig, idf], [Bm], lambda e: e.tensor_tensor(out=Bm[:, :nc_, :], in0=bcn(b_, nc_, 64), in1=idf[:64, :64].unsqueeze(1).to_broadcast([64, nc_, 64]), op=ALU.mult))
        t = nextpp()
        p.op("pe", [ones, Gm], [t], lambda e: e.matmul(t[:, :W], lhsT=ones[:64, :], rhs=Gm[:, :nc_, :].rearrange("p n i -> p (n i)"), start=True, stop=True))
        tv3 = t[:64, :W].rearrange("p (n i) -> p n i", i=64)
        p.op("dve", [t, gc], [diff], lambda e: e.tensor_tensor(out=diff[:, :nc_, :], in0=tv3, in1=bcn(gc_, nc_, 64), op=ALU.subtract))
        p.op("act", [t], [rs], lambda e: e.activation(out=rs[:, :W], in_=t[:, :W], func=AF.Exp))
        p.op("dve", [qn, rs], [hb.qdT], lambda e: e.tensor_tensor(out=hb.qdT[:, :nc_, :].rearrange("p n i -> p (n i)"), in0=qn[:, :W], in1=rs[:, :W], op=ALU.mult))
        t = nextpp()
        p.op("pe", [ones, Bm], [t], lambda e: e.matmul(t[:64, :W], lhsT=ones[:64, :64], rhs=Bm[:, :nc_, :].rearrange("p n i -> p (n i)"), start=True, stop=True))
        p.op("act", [t], [bbc], lambda e: e.copy(out=bbc[:, :nc_, :].rearrange("p n i -> p (n i)"), in_=t[:64, :W]))
        yield 0
        p.op("dve", [diff, nm], [DT], lambda e: e.tensor_tensor(out=DT[:, :nc_, :], in0=diff[:, :nc_, :], in1=nm[:, 2 * d, :].unsqueeze(1).to_broadcast([64, nc_, 64]), op=ALU.add))
        p.op("act", [DT], [DT], lambda e: e.activation(out=DT[:, :nc_, :], in_=DT[:, :nc_, :], func=AF.Exp))
        p.op("dve", [diff, nm], [Ds2], lambda e: e.scalar_tensor_tensor(
            out=Ds2[:, :nc_, :], in0=diff[:, :nc_, :], scalar=-1.0, in1=nm[:, 2 * (1 - d) + 1, :].unsqueeze(1).to_broadcast([64, nc_, 64]), op0=ALU.mult, op1=ALU.add))
        p.op("act", [Ds2], [Ds2], lambda e: e.activation(out=Ds2[:, :nc_, :], in_=Ds2[:, :nc_, :], func=AF.Exp))
        yield 0
        t = nextpp(); tv3 = t[:64, :W].rearrange("p (n i) -> p n i", i=64)
        for n in range(nc_):
            p.op("pe", [kn], [t], lambda e: e.matmul(tv3[:, n, :], lhsT=kn[:, n * 64:(n + 1) * 64], rhs=kn[:, n * 64:(n + 1) * 64], start=True, stop=True))
        p.op("act", [t], [mkk], lambda e: e.copy(out=mkk[:, :nc_, :], in_=tv3))
        t = nextpp(); tq3 = t[:64, :W].rearrange("p (n i) -> p n i", i=64)
        for n in range(nc_):
            p.op("pe", [kn, qn], [t], lambda e: e.matmul(tq3[:, n, :], lhsT=kn[:, n * 64:(n + 1) * 64], rhs=qn[:, n * 64:(n + 1) * 64], start=True, stop=True))
        p.op("dve", [t, DT], [hb.attnT], lambda e: e.tensor_tensor(out=hb.attnT[:, :nc_, :], in0=tq3, in1=DT[:, :nc_, :], op=ALU.mult))
        yield 0
        p.op("dve", [mkk, DT], [LT], lambda e: e.tensor_tensor(out=LT[:, :nc_, :], in0=mkk[:, :nc_, :], in1=DT[:, :nc_, :], op=ALU.mult))
        p.op("pool", [LT, st01], [LT], lambda e: e.tensor_tensor(out=LT[:, :nc_, :], in0=LT[:, :nc_, :], in1=st01[:, d, :].unsqueeze(1).to_broadcast([64, nc_, 64]), op=ALU.mult))
        p.op("dve", [LT, bbc], [LT], lambda e: e.tensor_tensor(out=LT[:, :nc_, :], in0=LT[:, :nc_, :], in1=bbc[:, :nc_, :], op=ALU.mult))
        p.op("pool", [mkk, Ds2], [Lm], lambda e: e.tensor_tensor(out=Lm[:, :nc_, :], in0=mkk[:, :nc_, :], in1=Ds2[:, :nc_, :], op=ALU.mult))
        p.op("dve", [Lm, bsig], [Lm], lambda e: e.tensor_tensor(out=Lm[:, :nc_, :], in0=Lm[:, :nc_, :], in1=bcn(b_, nc_, 64), op=ALU.mult))
        kb = kbs[par]; vb = vbs[par]
        p.op("pool", [ktok, bsig], [kb], lambda e: e.tensor_tensor(out=kb[:, :nc_, :], in0=ktok[:, :nc_, :], in1=bcn(b_, nc_, 128), op=ALU.mult))
        p.op("dve", [kb, egc], [kb], lambda e: e.tensor_tensor(out=kb[:, :nc_, :], in0=kb[:, :nc_, :], in1=bcn(egc[:, d, c0:c0 + nc_, h], nc_, 128), op=ALU.mult))
        p.op("pool", [vtok, bsig], [vb], lambda e: e.tensor_tensor(out=vb[:, :nc_, :], in0=vtok[:, :nc_, :], in1=bcn(b_, nc_, 128), op=ALU.mult))
        p.op("dve", [ktok, ekd], [hb.kdec], lambda e: e.tensor_tensor(out=hb.kdec[:, :nc_, :], in0=ktok[:, :nc_, :], in1=bcn(ekd[:, d, c0:c0 + nc_, h], nc_, 128), op=ALU.mult))
        yield 0
        yield 1
        R = Rr[0]
        p.op("dve", [LT, idf], [R], lambda e: e.scalar_tensor_tensor(
            out=R[:, :nc_, :], in0=LT[:, :nc_, :], scalar=-1.0, in1=idf[:64, :64].unsqueeze(1).to_broadcast([64, nc_, 64]), op0=ALU.mult, op1=ALU.add))
        p.op("act", [LT], [LTb], lambda e: e.copy(out=LTb[:, :nc_, :], in_=LT[:, :nc_, :]))
        p.op("act", [Lm], [Lmb], lambda e: e.copy(out=Lmb[:, :nc_, :], in_=Lm[:, :nc_, :]))
        p.op("pool", [R], [Rb[0]], lambda e: e.tensor_copy(out=Rb[0][:, :nc_, :], in_=R[:, :nc_, :]))

    def chain_gen(nc_):
        W = nc_ * 64
        R = Rr[0]; Rbc = Rb[0]
        X, XT = LTb, Lmb
        for it in range(5):
            Xn, XTn = Xa[it % 2], Xb[it % 2]
            t = psC[ci[0] % 4]; ci[0] += 1
            t3 = t[:64, :W].rearrange("p (n i) -> p n i", i=64)
            for n in range(nc_):
                p.op("pe", [X, XT], [t], lambda e: e.matmul(t3[:, n, :], lhsT=XT[:, n, :], rhs=X[:, n, :], start=True, stop=True))
            tb_ = psC[ci[0] % 4]; ci[0] += 1
            t3b = tb_[:64, :W].rearrange("p (n i) -> p n i", i=64)
            for n in range(nc_):
                p.op("pe", [X, XT], [tb_], lambda e: e.matmul(t3b[:, n, :], lhsT=X[:, n, :], rhs=XT[:, n, :], start=True, stop=True))
            yield 0
            p.op("act", [t], [Xn], lambda e: e.copy(out=Xn[:, :nc_, :], in_=t3))
            p.op("act", [tb_], [XTn], lambda e: e.copy(out=XTn[:, :nc_, :], in_=t3b))
            tc_ = psC[ci[0] % 4]; ci[0] += 1
            t3c = tc_[:64, :W].rearrange("p (n i) -> p n i", i=64)
            for n in range(nc_):
                p.op("pe", [XTn, Rbc], [tc_], lambda e: e.matmul(t3c[:, n, :], lhsT=XTn[:, n, :], rhs=Rbc[:, n, :], start=True, stop=True))
            yield 0
            Rn = Rr[(it + 1) % 2]
            p.op("dve", [tc_, R], [Rn], lambda e: e.tensor_tensor(out=Rn[:, :nc_, :], in0=t3c, in1=R[:, :nc_, :], op=ALU.add))
            if it < 4:
                Rbn = Rb[(it + 1) % 2]
                p.op("act", [Rn], [Rbn], lambda e: e.copy(out=Rbn[:, :nc_, :], in_=Rn[:, :nc_, :]))
                Rbc = Rbn
            R = Rn; X, XT = Xn, XTn

    def tail(h, nc_, par):
        hb = H[h]
        W = nc_ * 64
        kb = kbs[par]; vb = vbs[par]
        AinvT = Rr[1]
        t = psC[ci[0] % 4]; ci[0] += 1
        tw3 = t[:, :W].rearrange("p (n i) -> p n i", i=64)
        for n in range(nc_):
            p.op("pe", [kb, AinvT], [t], lambda e: e.matmul(tw3[:, n, :], lhsT=kb[:, n, :], rhs=AinvT[:, n, :], start=True, stop=True))
        p.op("act", [t], [hb.wT], lambda e: e.copy(out=hb.wT[:, :nc_, :], in_=tw3))
        for g in range(0, nc_, 4):
            t = psC[ci[0] % 4]; ci[0] += 1
            tu3 = t[:64, :].rearrange("p (n d) -> p n d", d=128)
            for n in range(g, min(g + 4, nc_)):
                p.op("pe", [AinvT, vb], [t], lambda e: e.matmul(tu3[:, n - g, :], lhsT=AinvT[:, n, :], rhs=vb[:, n, :], start=True, stop=True))
            ne = min(4, nc_ - g)
            p.op("dve", [t], [hb.u], lambda e: e.tensor_copy(out=hb.u[:, g:g + ne, :], in_=tu3[:, :ne, :]))

    def run_window(d, c0, nc_):
        def drain(g, until_final=False):
            for v in g:
                if until_final and v == 1:
                    return False
            return True
        fr = front_gen(0, d, c0, nc_, 0)
        drain(fr)
        for i in range(GH):
            ch = chain_gen(nc_)
            nf = front_gen(i + 1, d, c0, nc_, (i + 1) % 2) if i + 1 < GH else None
            nf_done = nf is None
            ch_done = False
            while not ch_done:
                try:
                    next(ch)
                except StopIteration:
                    ch_done = True
                if not nf_done:
                    try:
                        for _ in range(1):
                            v = next(nf)
                            if v == 1:
                                nf_done = True
                                break
                    except StopIteration:
                        nf_done = True; nf = None
            tail(i, nc_, i % 2)
            if nf is not None:
                drain(nf)

    def scan_step(h, d, c0, n):
        hb = H[h]
        cidx = c0 + n
        p.op("pe", [hb.wT, hb.Sb], [hb.ps1], lambda e: e.matmul(hb.ps1[:], lhsT=hb.wT[:, n, :], rhs=hb.Sb[:], start=True, stop=True))
        p.op("dve", [hb.u, hb.ps1], [hb.vn], lambda e: e.tensor_tensor(out=hb.vn[:], in0=hb.u[:, n, :], in1=hb.ps1[:], op=ALU.subtract))
        p.op("pe", [hb.Sb, hb.qdT], [hb.psO], lambda e: e.matmul(hb.psO[:], lhsT=hb.Sb[:], rhs=hb.qdT[:, n, :], start=True, stop=False))
        p.op("pe", [hb.vn, hb.attnT], [hb.psO], lambda e: e.matmul(hb.psO[:], lhsT=hb.vn[:], rhs=hb.attnT[:, n, :], start=False, stop=True))
        p.op("pe", [hb.kdec, hb.vn], [hb.psS], lambda e: e.matmul(hb.psS[:], lhsT=hb.kdec[:, n, :], rhs=hb.vn[:], start=True, stop=True))
        p.op("dve", [hb.S, egl, hb.psS], [hb.S], lambda e: e.scalar_tensor_tensor(
            out=hb.S[:], in0=hb.S[:], scalar=egl[:, d, cidx, h:h + 1], in1=hb.psS[:], op0=ALU.mult, op1=ALU.add))
        p.op("act", [hb.S], [hb.Sb], lambda e: e.copy(out=hb.Sb[:], in_=hb.S[:]))
        p.op("act", [hb.psO], [hb.oT], lambda e: e.copy(out=hb.oT[:, n, :], in_=hb.psO[:]))

    if dbg < 2:
        run_window(0, 4, 8)
        hb = H[0]
        C.dbg = [(hb.u, hb.u[:].rearrange("p n d -> p (n d)"), [64, 1024]), (Rr[1], Rr[1][:].rearrange("p n d -> p (n d)"), [64, 512]),
                 (LT, LT[:].rearrange("p n d -> p (n d)"), [64, 512]), (ktok, ktok[:].rearrange("p n d -> p (n d)"), [64, 1024])]
        return
    for d in range(2):
        for h in range(GH):
            hb = H[h]
            p.op("pool", [], [hb.S], lambda e: e.memset(hb.S[:], 0.0))
            p.op("pool", [], [hb.Sb], lambda e: e.memset(hb.Sb[:], 0.0))
        for (c0, nc_) in gdn_windows(d):
            run_window(d, c0, nc_)
            p.barrier()
            order = range(nc_) if d == 0 else range(nc_ - 1, -1, -1)
            for n in order:
                for h in range(GH):
                    scan_step(h, d, c0, n)
            for h in range(GH):
                hb = H[h]
                p.dma("sp" if h % 2 else "act", C.oTd[d][h], C.oTd[d][h][:, c0 * 64:(c0 + nc_) * 64], hb.oT, hb.oT[:, :nc_, :].rearrange("p n i -> p (n i)"))
            p.barrier()
    p.barrier()
    p.release(mk)
    mk = p.mark()
    gn2 = p.sb("gn2G", [128, 1]); p.dma("sp", gn2, gn2[:], C.gnorm, C.gnorm.ap[l])
    ones2 = p.sb("ones2G", [128, 128]); p.op("pool", [], [ones2], lambda e: e.memset(ones2[:], 1.0))
    of = [p.sb(f"ofG{i}", [128, TA]) for i in range(2)]; ob_ = [p.sb(f"obG{i}", [128, TA]) for i in range(2)]
    zz = [p.sb(f"zzG{i}", [128, TA]) for i in range(2)]
    sq2 = [p.sb(f"sq2G{i}", [128, 512]) for i in range(2)]; r2 = [p.sb(f"r2G{i}", [128, 512]) for i in range(2)]
    om = [p.sb(f"omG{i}", [128, TA], BF16) for i in range(2)]
    pq = [p.ps(f"pqG{i}", [128, 512]) for i in range(2)]
    for h in range(GH):
        a, b_, z_, o_ = of[h % 2], ob_[h % 2], zz[h % 2], om[h % 2]
        p.dma("sp", a, a[:], C.oTd[0][h], C.oTd[0][h][:])
        p.dma("act", b_, b_[:], C.oTd[1][h], C.oTd[1][h][:])
        p.dma("sp", z_, z_[:], C.uT[18 + h], C.uT[18 + h][:])
        p.op("pool", [a, b_], [a], lambda e: e.tensor_tensor(out=a[:], in0=a[:], in1=b_[:], op=ALU.add))
        p.op("act", [z_], [z_], lambda e: e.activation(out=z_[:], in_=z_[:], func=AF.Silu))
        for blk in range(9):
            c0 = blk * 512; w = min(512, TA - c0)
            s_, r_ = sq2[blk % 2], r2[blk % 2]; t = pq[blk % 2]
            p.op("pool", [a], [s_], lambda e: e.tensor_tensor(out=s_[:, :w], in0=a[:, c0:c0 + w], in1=a[:, c0:c0 + w], op=ALU.mult))
            p.op("pe", [ones2, s_], [t], lambda e: e.matmul(t[:, :w], lhsT=ones2[:], rhs=s_[:, :w], start=True, stop=True))
            p.op("dve", [t], [r_], lambda e: e.tensor_scalar(out=r_[:, :w], in0=t[:, :w], scalar1=1.0 / 128, scalar2=EPS, op0=ALU.mult, op1=ALU.add))
            p.op("act", [r_], [r_], lambda e: e.activation(out=r_[:, :w], in_=r_[:, :w], func=AF.Ln))
            p.op("act", [r_], [r_], lambda e: e.activation(out=r_[:, :w], in_=r_[:, :w], func=AF.Exp, scale=-0.5))
            p.op("dve", [a, gn2, r_], [r_], lambda e: e.scalar_tensor_tensor(
                out=r_[:, :w], in0=a[:, c0:c0 + w], scalar=gn2[:, 0:1], in1=r_[:, :w], op0=ALU.mult, op1=ALU.mult))
            p.op("dve", [r_, z_], [o_], lambda e: e.tensor_tensor(out=o_[:, c0:c0 + w], in0=r_[:, :w], in1=z_[:, c0:c0 + w], op=ALU.mult))
        p.dma("sp", C.mixT[h], C.mixT[h][:], o_, o_[:])
    p.barrier()
    p.release(mk)


def declare_scratch2(p, C):
    C.h2 = p.dram("h2_s", [TA, D], BF16)
    C.h2v = [C.h2.view((slice(i * 128, (i + 1) * 128), slice(None))) for i in range(NT)]
    C.affT = p.dram("affT_s", [NE, TA], F32)
    C.acc = p.dram("acc_s", [TA, D], F32)


def stage_D(p, C, l, with_ctx=True):
    mk = p.mark()
    wo = p.sb("woD", [128, 16, D], BF16)
    wov = [wo.view((slice(None), slice(q * 4, (q + 1) * 4), slice(None))) for q in range(4)]
    for q in range(4):
        p.dma("pool", wov[q], wov[q][:], C.w_out, C.w_out.ap[l, q * 512:(q + 1) * 512, :].rearrange("(k p) n -> p k n", p=128))
    wr = p.sb("wrD", [128, 16, NE], BF16)
    p.dma("pool", wr, wr[:], C.w_r, C.w_r.ap[l].rearrange("(k p) e -> p k e", p=128))
    idf = p.sb("idfD", [128, 128]); idb = p.sb("idbD", [128, 128], BF16)
    p.dma("sp", idf, idf[:], C.ident, C.ident[:])
    p.op("dve", [idf], [idb], lambda e: e.tensor_copy(out=idb[:], in_=idf[:]))
    ones16 = p.sb("ones16D", [NE, NE]); p.op("pool", [], [ones16], lambda e: e.memset(ones16[:], 1.0))
    expT = p.sb("expTD", [NE, TA])
    g2 = p.sb("g2D", [128, D]); gs = p.sb("gsD", [128, D]); sh = p.sb("shD", [128, D]); n2 = p.sb("n2D", [128, D])
    mx = p.sb("mxD", [128, 16, 1024], BF16)
    mxv = [mx.view((slice(None), c, slice(None))) for c in range(16)]
    hb = [p.sb(f"hbD{i}", [128, D]) for i in range(2)]
    yb = [p.sb(f"ybD{i}", [128, D]) for i in range(2)]
    ab = [p.sb(f"abD{i}", [128, D], BF16) for i in range(2)]
    h2T = [p.sb(f"h2TD{i}", [128, 16, 128], BF16) for i in range(2)]
    ss = [p.sb(f"ssD{i}", [128, 2]) for i in range(2)]
    tmp = [p.sb(f"tmpD{i}", [128, 512]) for i in range(2)]
    pu = [p.ps(f"puD{i}", [128, 512]) for i in range(4)]
    ptr = [p.ps(f"ptrD{i}", [128, 4, 128], BF16) for i in range(2)]
    pr = [p.ps(f"prD{i}", [NE, 128]) for i in range(2)]
    bc_load(p, "sp", n2, n2[:], C.norm2, C.norm2.ap[l:l + 1, :])
    groups = [(0, [0, 1])] + [(1, list(range(2 + 8 * g, 10 + 8 * g))) for g in range(4)]
    pc = 0; tc_ = 0; tcount = 0
    pending = [None]
    last_kind = None
    for kind, tiles in groups:
        if kind == 0 and not with_ctx:
            continue
        if kind != last_kind:
            last_kind = kind
            bc_load(p, "sp", g2, g2[:], C.mod, modrow(C, l, kind, 2))
            bc_load(p, "act", gs, gs[:], C.mod, modrow(C, l, kind, 4))
            bc_load(p, "sp", sh, sh[:], C.mod, modrow(C, l, kind, 3))
            p.op("dve", [gs, n2], [gs], lambda e: e.scalar_tensor_tensor(out=gs[:], in0=gs[:], scalar=1.0, in1=n2[:], op0=ALU.add, op1=ALU.mult))
        g0 = tiles[0] * 128; gw = len(tiles) * 128
        for c in range(16):
            p.dma("sp" if c % 2 else "act", mxv[c], mxv[c][:, :gw], C.mixT[c], C.mixT[c][:, g0:g0 + gw])
        for tj, ti in enumerate(tiles):
            ht = hb[tcount % 2]; y_ = yb[tcount % 2]; a_ = ab[tcount % 2]; s_ = ss[tcount % 2]; hT = h2T[tcount % 2]
            tcount += 1
            p.dma("sp", ht, ht[:], C.hv[ti], C.hv[ti][:])
            for k in range(16):
                for nb in range(4):
                    p.op("pe", [mxv[k], wov[k // 4]], [pu[nb]], lambda e: e.matmul(
                        pu[nb][:], lhsT=mx[:, k, tj * 128:(tj + 1) * 128], rhs=wo[:, k, nb * 512:(nb + 1) * 512], start=(k == 0), stop=(k == 15)))
            if pending[0] is not None:
                pending[0](); pending[0] = None
            for nb in range(4):
                t_ = tmp[nb % 2]
                p.op("dve", [pu[nb], g2], [t_], lambda e: e.tensor_tensor(out=t_[:], in0=pu[nb][:], in1=g2[:, nb * 512:(nb + 1) * 512], op=ALU.mult))
                p.op("pool", [t_, ht], [ht], lambda e: e.tensor_tensor(out=ht[:, nb * 512:(nb + 1) * 512], in0=ht[:, nb * 512:(nb + 1) * 512], in1=t_[:], op=ALU.add))
            p.dma("sp", C.hv[ti], C.hv[ti][:], ht, ht[:])
            p.op("pool", [], [s_], lambda e: e.memset(s_[:], 0.0))
            p.op("act", [ht], [y_, s_], lambda e: e.activation(out=y_[:], in_=ht[:], func=AF.Square, accum_out=s_[:, 0:1]))
            p.op("dve", [s_], [s_], lambda e: e.tensor_scalar(out=s_[:, 1:2], in0=s_[:, 0:1], scalar1=1.0 / D, scalar2=EPS, op0=ALU.mult, op1=ALU.add))
            p.op("act", [s_], [s_], lambda e: e.sqrt(out=s_[:, 1:2], in_=s_[:, 1:2]))
            p.op("dve", [s_], [s_], lambda e: e.reciprocal(out=s_[:, 1:2], in_=s_[:, 1:2]))
            p.op("dve", [ht, s_, gs], [y_], lambda e: e.scalar_tensor_tensor(out=y_[:], in0=ht[:], scalar=s_[:, 1:2], in1=gs[:], op0=ALU.mult, op1=ALU.mult))
            p.op("pool", [y_, sh], [a_], lambda e: e.tensor_tensor(out=a_[:], in0=y_[:], in1=sh[:], op=ALU.add))
            p.dma("act", C.h2v[ti], C.h2v[ti][:], a_, a_[:])

            def tail(a_=a_, hT=hT, ti=ti, pq=pr[tcount % 2]):
                nonlocal tc_
                for k4 in range(4):
                    pt = ptr[tc_ % 2]; tc_ += 1
                    for kk in range(4):
                        k = k4 * 4 + kk
                        p.op("pe", [a_, idb], [pt], lambda e: e.transpose(out=pt[:, kk, :], in_=a_[:, k * 128:(k + 1) * 128], identity=idb[:]))
                    p.op("act", [pt], [hT], lambda e: e.copy(out=hT[:, k4 * 4:(k4 + 1) * 4, :], in_=pt[:]))
                for k in range(16):
                    p.op("pe", [wr, hT], [pq], lambda e: e.matmul(pq[:], lhsT=wr[:, k, :], rhs=hT[:, k, :], start=(k == 0), stop=(k == 15)))
                p.op("act", [pq], [expT], lambda e: e.activation(out=expT[:, ti * 128:(ti + 1) * 128], in_=pq[:], func=AF.Exp))
            pending[0] = tail
    if pending[0] is not None:
        pending[0](); pending[0] = None
    t0_ = 0 if with_ctx else TC
    blk = t0_
    while blk < TA:
        w = min(512, TA - blk)
        pp = pu[pc % 4]; pc += 1
        t_ = tmp[pc % 2]
        p.op("pe", [ones16, expT], [pp], lambda e: e.matmul(pp[:NE, :w], lhsT=ones16[:], rhs=expT[:, blk:blk + w], start=True, stop=True))
        p.op("dve", [pp], [t_], lambda e: e.reciprocal(out=t_[:NE, :w], in_=pp[:NE, :w]))
        p.op("dve", [t_, expT], [expT], lambda e: e.tensor_tensor(out=expT[:, blk:blk + w], in0=expT[:, blk:blk + w], in1=t_[:NE, :w], op=ALU.mult))
        blk += w
    p.dma("sp", C.affT, C.affT[:, t0_:], expT, expT[:, t0_:])
    p.barrier()
    p.release(mk)


def stage_E(p, C, l, with_ctx=True):
    segs = [(TC, TL, 512)] + ([(0, TC, 32)] if with_ctx else [])
    nslots = sum(s[2] for s in segs)
    mk = p.mark()
    idf = p.sb("idfE", [128, 128]); idb = p.sb("idbE", [128, 128], BF16)
    p.dma("sp", idf, idf[:], C.ident, C.ident[:])
    p.op("dve", [idf], [idb], lambda e: e.tensor_copy(out=idb[:], in_=idf[:]))
    idxT = p.sb("idxTE", [128, 5, NE], I32); gateT = p.sb("gateTE", [128, 5, NE])
    mk0 = p.mark()
    zt = p.sb("ztE", [128, D]); p.op("pool", [], [zt], lambda e: e.memset(zt[:], 0.0))
    for i in range(NT):
        p.dma("sp" if i % 2 else "act", C.acc, C.acc.ap[i * 128:(i + 1) * 128, :], zt, zt[:])
    aff = p.sb("affE", [NE, TA]); work = p.sb("workE", [NE, TL])
    vals = p.sb("valsE", [NE, 544]); idxu = p.sb("idxuE", [NE, 544], U32); idxf = p.sb("idxfE", [NE, 544])
    ptk = p.ps("ptkE", [128, 2, NE])
    p.dma("sp", aff, aff[:], C.affT, C.affT[:])
    so = 0
    slot_tiles = []
    for (s0, n, cap) in segs:
        p.op("dve", [aff], [work], lambda e: e.tensor_copy(out=work[:, :n], in_=aff[:, s0:s0 + n]))
        for r in range(cap // 8):
            c0 = so + r * 8
            p.op("dve", [work], [vals], lambda e: e.max(out=vals[:, c0:c0 + 8], in_=work[:, :n]))
            p.op("dve", [vals, work], [idxu], lambda e: e.max_index(out=idxu[:, c0:c0 + 8], in_max=vals[:, c0:c0 + 8], in_values=work[:, :n]))
            p.op("dve", [vals, work], [work], lambda e: e.match_replace(out=work[:, :n], in_to_replace=vals[:, c0:c0 + 8], in_values=work[:, :n], imm_value=-1.0))
        p.op("dve", [idxu], [idxf], lambda e: e.tensor_copy(out=idxf[:, so:so + cap], in_=idxu[:, so:so + cap]))
        p.op("dve", [idxf], [idxf], lambda e: e.tensor_scalar(out=idxf[:, so:so + cap], in0=idxf[:, so:so + cap], scalar1=float(s0), scalar2=None, op0=ALU.add))
        for st in range((cap + 127) // 128):
            rows = min(128, cap - st * 128)
            ti = len(slot_tiles)
            c0 = so + st * 128
            p.op("pe", [idxf, idf], [ptk], lambda e: e.transpose(out=ptk[:rows, 0, :], in_=idxf[:, c0:c0 + rows], identity=idf[:NE, :NE]))
            p.op("pe", [vals, idf], [ptk], lambda e: e.transpose(out=ptk[:rows, 1, :], in_=vals[:, c0:c0 + rows], identity=idf[:NE, :NE]))
            p.op("dve", [ptk], [idxT], lambda e: e.tensor_copy(out=idxT[:rows, ti, :], in_=ptk[:rows, 0, :]))
            p.op("dve", [ptk], [gateT], lambda e: e.tensor_copy(out=gateT[:rows, ti, :], in_=ptk[:rows, 1, :]))
            slot_tiles.append((c0, rows, ti))
        so += cap
    p.barrier()
    p.release(mk0)
    gu = [[p.sb(f"guE{i}{j}", [128, 16, 512], BF16) for j in range(2)] for i in range(2)]
    wd = p.sb("wdE", [128, 8, D], BF16)
    wdv = [wd.view((slice(None), slice(q * 4, (q + 1) * 4), slice(None))) for q in range(2)]
    xsT = p.sb("xsTE", [128, 16, 544], BF16); hidT = p.sb("hidTE", [128, 8, 544], BF16)
    xs = [p.sb(f"xsE{i}", [128, D], BF16) for i in range(2)]
    yt = [p.sb(f"ytE{i}", [128, D]) for i in range(2)]
    sg = [p.sb(f"sgE{i}", [128, 512]) for i in range(2)]
    ptr = [p.ps(f"ptrE{i}", [128, 4, 128], BF16) for i in range(2)]
    X6 = [p.ps(f"pxE{i}", [128, 512]) for i in range(6)]
    blocks = [(0, 272), (272, 272)] if with_ctx else [(0, 512)]
    gi = 0; xi = 0; tci = 0; yi = 0; si = 0
    for e_ in range(NE):
        for (c0, rows, ti) in slot_tiles:
            x_ = xs[xi % 2]; xi += 1
            p.dma_indirect_gather(x_, x_[:rows, :], C.h2, C.h2.ap[:, :], idxT, idxT[:rows, ti, e_:e_ + 1])
            for k4 in range(4):
                pt = ptr[tci % 2]; tci += 1
                for kk in range(4):
                    k = k4 * 4 + kk
                    p.op("pe", [x_, idb], [pt], lambda e: e.transpose(out=pt[:, kk, :rows], in_=x_[:rows, k * 128:(k + 1) * 128], identity=idb[:rows, :rows]))
                p.op("act", [pt], [xsT], lambda e: e.copy(out=xsT[:, k4 * 4:(k4 + 1) * 4, c0:c0 + rows], in_=pt[:, :, :rows]))
        for half in range(2):
            g_, u_ = gu[gi % 2]; gi += 1
            for hh in range(2):
                p.dma("pool", g_, g_[:, hh * 8:(hh + 1) * 8, :], C.w_eg, C.w_eg.ap[l, e_, hh * 1024:(hh + 1) * 1024, half * 512:(half + 1) * 512].rearrange("(k p) n -> p k n", p=128))
                p.dma("pool", u_, u_[:, hh * 8:(hh + 1) * 8, :], C.w_eu, C.w_eu.ap[l, e_, hh * 1024:(hh + 1) * 1024, half * 512:(half + 1) * 512].rearrange("(k p) n -> p k n", p=128))
            for fcc in range(4):
                fc = half * 4 + fcc
                for (b0, bw) in blocks:
                    pG, pU = X6[2 * (si % 3)], X6[2 * (si % 3) + 1]; s_ = sg[si % 2]; si += 1
                    for k in range(16):
                        p.op("pe", [g_, xsT], [pG], lambda e: e.matmul(pG[:, :bw], lhsT=g_[:, k, fcc * 128:(fcc + 1) * 128], rhs=xsT[:, k, b0:b0 + bw], start=(k == 0), stop=(k == 15)))
                        p.op("pe", [u_, xsT], [pU], lambda e: e.matmul(pU[:, :bw], lhsT=u_[:, k, fcc * 128:(fcc + 1) * 128], rhs=xsT[:, k, b0:b0 + bw], start=(k == 0), stop=(k == 15)))
                    p.op("act", [pG], [s_], lambda e: e.activation(out=s_[:, :bw], in_=pG[:, :bw], func=AF.Silu))
                    p.op("dve", [s_, pU], [hidT], lambda e: e.tensor_tensor(out=hidT[:, fc, b0:b0 + bw], in0=s_[:, :bw], in1=pU[:, :bw], op=ALU.mult))
        for q in range(2):
            p.dma("pool", wdv[q], wdv[q][:], C.w_ed, C.w_ed.ap[l, e_, q * 512:(q + 1) * 512, :].rearrange("(k p) n -> p k n", p=128))
        for (c0, rows, ti) in slot_tiles:
            y_ = yt[yi % 2]; yi += 1
            for n2 in range(2):
                pA, pB = X6[2 * (si % 3)], X6[2 * (si % 3) + 1]; si += 1
                for fc in range(8):
                    for j, pb in enumerate((pA, pB)):
                        nb = n2 * 2 + j
                        p.op("pe", [hidT, wdv[fc // 4]], [pb], lambda e: e.matmul(
                            pb[:rows, :], lhsT=hidT[:, fc, c0:c0 + rows], rhs=wd[:, fc, nb * 512:(nb + 1) * 512], start=(fc == 0), stop=(fc == 7)))
                nb = n2 * 2
                p.op("act", [pA, gateT], [y_], lambda e: e.mul(out=y_[:rows, nb * 512:(nb + 1) * 512], in_=pA[:rows, :], mul=gateT[:rows, ti, e_:e_ + 1]))
                p.op("dve", [pB, gateT], [y_], lambda e: e.tensor_scalar(
                    out=y_[:rows, (nb + 1) * 512:(nb + 2) * 512], in0=pB[:rows, :], scalar1=gateT[:rows, ti, e_:e_ + 1], scalar2=None, op0=ALU.mult))
            p.dma_indirect_scatter_add(C.acc, C.acc.ap[:, :], y_, y_[:rows, :], idxT, idxT[:rows, ti, e_:e_ + 1])
    p.barrier()
    p.release(mk)


def stage_F(p, C, l, with_ctx=True, final=False):
    mk = p.mark()
    g5 = p.sb("g5F", [128, 2, D])
    for kind in range(2):
        bc_load(p, "sp", g5, g5[:, kind, :], C.mod, modrow(C, l, kind, 5))
    if final:
        fg = p.sb("fgF", [128, D]); bc_load(p, "act", fg, fg[:], C.fnorm, C.fnorm.ap[0:1, :])
    hb = [p.sb(f"hbF{i}", [128, D]) for i in range(2)]; ac = [p.sb(f"acF{i}", [128, D]) for i in range(2)]
    ss = [p.sb(f"ssF{i}", [128, 2]) for i in range(2)]
    for ti in range(NT):
        kind = 0 if ti < 2 else 1
        if kind == 0 and (final or not with_ctx):
            continue
        ht = hb[ti % 2]; a_ = ac[ti % 2]; s_ = ss[ti % 2]
        p.dma("sp", ht, ht[:], C.hv[ti], C.hv[ti][:])
        p.dma("act", a_, a_[:], C.acc, C.acc.ap[ti * 128:(ti + 1) * 128, :])
        p.op("pool", [a_, g5], [a_], lambda e: e.tensor_tensor(out=a_[:], in0=a_[:], in1=g5[:, kind, :], op=ALU.mult))
        p.op("dve", [ht, a_], [ht], lambda e: e.tensor_tensor(out=ht[:], in0=ht[:], in1=a_[:], op=ALU.add))
        if not final:
            p.dma("sp", C.hv[ti], C.hv[ti][:], ht, ht[:])
        else:
            p.op("pool", [], [s_], lambda e: e.memset(s_[:], 0.0))
            p.op("act", [ht], [a_, s_], lambda e: e.activation(out=a_[:], in_=ht[:], func=AF.Square, accum_out=s_[:, 0:1]))
            p.op("dve", [s_], [s_], lambda e: e.tensor_scalar(out=s_[:, 1:2], in0=s_[:, 0:1], scalar1=1.0 / D, scalar2=EPS, op0=ALU.mult, op1=ALU.add))
            p.op("act", [s_], [s_], lambda e: e.sqrt(out=s_[:, 1:2], in_=s_[:, 1:2]))
            p.op("dve", [s_], [s_], lambda e: e.reciprocal(out=s_[:, 1:2], in_=s_[:, 1:2]))
            p.op("dve", [ht, s_, fg], [a_], lambda e: e.scalar_tensor_tensor(out=a_[:], in0=ht[:], scalar=s_[:, 1:2], in1=fg[:], op0=ALU.mult, op1=ALU.mult))
            r0 = ti * 128 - TC
            p.dma("sp", C.out, C.out.ap[r0:r0 + 128, :], a_, a_[:])
    p.barrier()
    p.release(mk)


def host_consts():
    ident = np.eye(128, dtype=np.float32)
    rows = TL // 64
    row = np.repeat(np.arange(rows, dtype=np.float32), 64)
    col = np.tile(np.arange(64, dtype=np.float32), rows)
    inv = (10000.0 ** (-np.arange(16, dtype=np.float32) / 16)).astype(np.float32)
    ang = np.stack([row[:, None] * inv, col[:, None] * inv], 1).astype(np.float32)
    cosA = np.cos(ang); sinA = np.sin(ang)
    cosT = np.zeros((128, TL), np.float32); sinT = np.zeros((128, TL), np.float32)
    rot = np.zeros((128, 128), np.float32)
    for m in range(128):
        base = (m // 64) * 64; q = m % 64
        axis = q // 32; sel = (q % 32) // 16; pair = q % 16
        cosT[m] = cosA[:, axis, pair]
        sinT[m] = sinA[:, axis, pair] * (-1.0 if sel == 0 else 1.0)
        src = base + (q + 16 if sel == 0 else q - 16)
        rot[src, m] = 1.0
    idx = np.arange(64)
    tri = np.stack([(idx[:, None] <= idx[None, :]), (idx[:, None] >= idx[None, :])], 0).astype(np.float32)
    j = idx[:, None]; i = idx[None, :]
    nm = np.stack([np.where(i >= j, 0.0, NEG), np.where(i > j, 0.0, NEG), np.where(i <= j, 0.0, NEG), np.where(i < j, 0.0, NEG)], 0).astype(np.float32)
    return dict(ident=ident, cosT=np.ascontiguousarray(cosT), sinT=np.ascontiguousarray(sinT), rotm=rot, tri=tri, nmask=nm)


def make_inputs(inp, b, depth=DEPTH, l0=0):
    ls = slice(l0, l0 + depth)
    m = dict(host_consts())
    m["x"] = inp["x"][b]; m["ctx"] = inp["ctx"][b]
    sc = np.stack([inp["c"][b], inp["c_ctx"]], 0)
    m["cT"] = np.ascontiguousarray(sc.reshape(2, 16, 128).transpose(2, 1, 0))
    m["w_mod"] = inp["w_mod"][ls]
    m["b_mod"] = np.ascontiguousarray(np.broadcast_to(inp["b_mod"][ls][:, None, :], (depth, 2, 6 * D)))
    m["norm1"] = inp["norm1_g"][ls]; m["norm2"] = inp["norm2_g"][ls]
    m["w_in"] = inp["w_in"][ls]
    gc = inp["gdn_conv_w"][ls]
    m["gconv"] = np.ascontiguousarray(gc.reshape(depth, 3, 18, 128).transpose(0, 3, 2, 1))
    m["galog"] = np.ascontiguousarray(np.broadcast_to(inp["gdn_a_log"][ls].reshape(depth, 1, 12), (depth, 64, 12)))
    m["gdtb"] = np.ascontiguousarray(np.broadcast_to(inp["gdn_dt_bias"][ls].reshape(depth, 1, 12), (depth, 64, 12)))
    m["gnorm"] = np.ascontiguousarray(inp["gdn_norm_g"][ls].reshape(depth, 128, 1))
    sc_w = inp["sc_conv_w"][ls]
    m["sconv"] = np.ascontiguousarray(sc_w.reshape(depth, 3, 4, 128).transpose(0, 3, 2, 1))
    m["dlam"] = np.ascontiguousarray(np.broadcast_to(inp["diff_lambda"][ls][:, None], (depth, 128, 4, 64)))
    m["dnorm"] = np.ascontiguousarray(inp["diff_norm_g"][ls].reshape(depth, 128, 1))
    m["w_out"] = inp["w_out"][ls]; m["w_r"] = inp["w_router"][ls]
    m["w_eg"] = inp["w_e_gate"][ls]; m["w_eu"] = inp["w_e_up"][ls]; m["w_ed"] = inp["w_e_down"][ls]
    m["fnorm"] = inp["final_norm_g"].reshape(1, D)
    return m


def build_program(depth=DEPTH, debug_dump=False):
    nc = bass.Bass("TRN2", target_bir_lowering=False)
    p = P(nc)
    C = declare_io(p, depth)
    declare_scratch(p, C)
    declare_scratch2(p, C)
    stage_load(p, C)
    stage_M(p, C)
    outs = [C.out]
    for l in range(depth):
        last = (l == depth - 1) and not debug_dump
        with_ctx = not last
        stage_A(p, C, l)
        stage_SC(p, C, l, with_ctx)
        stage_ATT(p, C, l, with_ctx)
        stage_GDN(p, C, l)
        stage_D(p, C, l, with_ctx)
        stage_E(p, C, l, with_ctx)
        stage_F(p, C, l, with_ctx, final=False)
    if debug_dump:
        outs.append(dump(p, C.h, C.h[:], "d_h", [TA, D]))
        outs.append(dump(p, C.acc, C.acc[:], "d_acc", [TA, D]))
        outs.append(dump(p, C.affT, C.affT[:], "d_affT", [NE, TA]))
    stage_final(p, C)
    p.finish(outs)
    p.close()
    return nc, p


def stage_final(p, C):
    mk = p.mark()
    fg = p.sb("fgZ", [128, D]); bc_load(p, "act", fg, fg[:], C.fnorm, C.fnorm.ap[0:1, :])
    hb = [p.sb(f"hbZ{i}", [128, D]) for i in range(2)]; ac = [p.sb(f"acZ{i}", [128, D]) for i in range(2)]
    ss = [p.sb(f"ssZ{i}", [128, 2]) for i in range(2)]
    for ti in range(2, NT):
        ht = hb[ti % 2]; a_ = ac[ti % 2]; s_ = ss[ti % 2]
        p.dma("sp", ht, ht[:], C.hv[ti], C.hv[ti][:])
        p.op("pool", [], [s_], lambda e: e.memset(s_[:], 0.0))
        p.op("act", [ht], [a_, s_], lambda e: e.activation(out=a_[:], in_=ht[:], func=AF.Square, accum_out=s_[:, 0:1]))
        p.op("dve", [s_], [s_], lambda e: e.tensor_scalar(out=s_[:, 1:2], in0=s_[:, 0:1], scalar1=1.0 / D, scalar2=EPS, op0=ALU.mult, op1=ALU.add))
        p.op("act", [s_], [s_], lambda e: e.sqrt(out=s_[:, 1:2], in_=s_[:, 1:2]))
        p.op("dve", [s_], [s_], lambda e: e.reciprocal(out=s_[:, 1:2], in_=s_[:, 1:2]))
        p.op("dve", [ht, s_, fg], [a_], lambda e: e.scalar_tensor_tensor(out=a_[:], in0=ht[:], scalar=s_[:, 1:2], in1=fg[:], op0=ALU.mult, op1=ALU.mult))
        r0 = ti * 128 - TC
        p.dma("act", C.out, C.out.ap[r0:r0 + 128, :], a_, a_[:])
    p.barrier()
    p.release(mk)


def kernel(**inputs):
    inp = {k: np.asarray(v) for k, v in inputs.items()}
    nc, _ = get_prog("main", lambda: build_program(DEPTH))
    maps = [make_inputs(inp, b) for b in range(2)]
    res = launch(nc, maps)
    out = np.stack([np.asarray(res[b]["out"]) for b in range(2)], 0).astype(np.float32)
    return out
```
